# Optimizing a Trainium2 kernel written in Bass

```python
import math
import jax
import jax.numpy as jnp
from jax import lax
import numpy as np


D_MODEL = 1024
BATCH = 8
SEQ = 2048
DEPTH = 2

GRID_W = 64
CTX_LEN = 256
EPS = 1e-6
N_MOD = 6

RET_HEADS = 4
RET_DK = 128
RET_DV = 128
RET_CHUNK = 128
ROPE_BASE = 10000.0
FNET_GROUPS = 4
FNET_CH = 128
DN_HEADS = 4
DN_DK = 128
DN_DV = 128
DN_CHUNK = 64
DN_CONV = 3
SGU_GROUPS = 4
SGU_CH = 128
SGU_CHUNK = 128
MOE_GROUPS = 4
MOE_EXP_PER_GROUP = 8
MOE_EXPERTS = MOE_GROUPS * MOE_EXP_PER_GROUP
MOE_TOP_K = 2
MOE_HIDDEN = 512
MOE_BLOCK = 128

RET_QK = RET_HEADS * RET_DK
RET_V = RET_HEADS * RET_DV
FNET_W = FNET_GROUPS * FNET_CH
E_K = RET_QK
E_V = 2 * RET_QK
E_G = 2 * RET_QK + RET_V
E_F = 2 * RET_QK + 2 * RET_V
EVEN_IN = E_F + FNET_W
EVEN_MIX = RET_V + FNET_W
DN_QK = DN_HEADS * DN_DK
DN_V = DN_HEADS * DN_DV
SGU_W = SGU_GROUPS * SGU_CH
O_K = DN_QK
O_V = 2 * DN_QK
O_Z = 2 * DN_QK + DN_V
O_A = O_Z + DN_V
O_U = O_A + 4 * DN_HEADS
O_S = O_U + SGU_W
ODD_IN = O_S + SGU_W
ODD_MIX = DN_V + SGU_W

kernel_name = 'hybrid_prefix_diffusion_block'
F32 = jnp.float32


def rms_norm(x, w):
    xf = x.astype(F32)
    y = xf * lax.rsqrt(jnp.mean(xf * xf, -1, keepdims=True) + EPS)
    return (y * w.astype(F32)).astype(x.dtype)


def l2norm(x):
    return x * lax.rsqrt(jnp.sum(x * x, -1, keepdims=True) + EPS)


def to_heads(x, n_heads):
    b, t, _ = x.shape
    return x.reshape(b, t, n_heads, -1).transpose(0, 2, 1, 3).astype(F32)


def from_heads(x):
    b, h, t, d = x.shape
    return x.transpose(0, 2, 1, 3).reshape(b, t, h * d)


def to_chunks(x, c):
    return x.reshape(x.shape[:2] + (x.shape[2] // c, c) + x.shape[3:])


def from_chunks(x):
    return x.reshape(x.shape[:2] + (x.shape[2] * x.shape[3],) + x.shape[4:])


def _noflip(x):
    return x


def _flip(x):
    return x[:, :, ::-1]


def axial_rope(x):
    t, d = x.shape[2], x.shape[3]
    rows = t // GRID_W
    row = jnp.repeat(jnp.arange(rows), GRID_W).astype(F32)
    col = jnp.tile(jnp.arange(GRID_W), rows).astype(F32)
    n_freq = d // 4
    inv = jnp.power(ROPE_BASE, -jnp.arange(n_freq, dtype=F32) / n_freq)
    ang = jnp.concatenate([row[:, None] * inv, col[:, None] * inv], -1)
    cos, sin = jnp.cos(ang), jnp.sin(ang)
    x1, x2 = jnp.split(x, 2, -1)
    return jnp.concatenate([x1 * cos - x2 * sin, x1 * sin + x2 * cos], -1)


def _ret_states(k, v, log_gamma, s0):
    c = k.shape[3]
    pos = jnp.arange(c, dtype=F32)
    w_tail = jnp.exp(log_gamma[:, None] * (c - 1 - pos))
    kv = jnp.einsum('bhncd,bhnce->bhnde', k * w_tail[None, :, None, :, None], v)
    g_chunk = jnp.exp(log_gamma * c)[None, :, None, None]

    def step(s, kv_n):
        return g_chunk * s + kv_n, s

    s_fin, s_prev = lax.scan(step, s0, jnp.moveaxis(kv, 2, 0))
    return jnp.moveaxis(s_prev, 0, 2), s_fin


def _ret_out(q, k, v, log_gamma, s_prev):
    c = q.shape[3]
    pos = jnp.arange(c, dtype=F32)
    diff = pos[:, None] - pos[None, :]
    dmat = jnp.where(diff >= 0, jnp.exp(log_gamma[:, None, None] * jnp.maximum(diff, 0.0)), 0.0)
    scores = jnp.einsum('bhnid,bhnjd->bhnij', q, k) * dmat[None, :, None]
    inner = jnp.einsum('bhnij,bhnje->bhnie', scores, v)
    q_dec = q * jnp.exp(log_gamma[:, None] * (pos + 1.0))[None, :, None, :, None]
    return inner + jnp.einsum('bhnid,bhnde->bhnie', q_dec, s_prev)


def retention_dir(k, v, log_gamma, s0, q=None):
    kc, vc = to_chunks(k, RET_CHUNK), to_chunks(v, RET_CHUNK)
    s_prev, s_fin = _ret_states(kc, vc, log_gamma, s0)
    out = None if q is None else from_chunks(_ret_out(to_chunks(q, RET_CHUNK), kc, vc, log_gamma, s_prev))
    return out, s_fin


def group_norm_gate(o, gate, w):
    mu = jnp.mean(o, -1, keepdims=True)
    var = jnp.mean(jnp.square(o - mu), -1, keepdims=True)
    y = from_heads((o - mu) * lax.rsqrt(var + EPS)) * w.astype(F32)
    return (y * jax.nn.silu(gate.astype(F32))).astype(gate.dtype)


def fourier_mix(f):
    b, t, _ = f.shape
    z = jnp.fft.fft2(f.reshape(b, t, FNET_GROUPS, FNET_CH).astype(F32), axes=(1, 3), norm='ortho')
    return jnp.real(z).reshape(b, t, FNET_W).astype(f.dtype)


def short_conv(x, w):
    k = w.shape[0]
    y = lax.conv_general_dilated(x, w[:, None, :].astype(x.dtype), window_strides=(1,),
                                 padding=[((k - 1) // 2, k // 2)],
                                 dimension_numbers=('NWC', 'WIO', 'NWC'),
                                 feature_group_count=x.shape[-1])
    return jax.nn.silu(y)


def dn_gates(ab, d, a_log, dt_bias):
    b, t, _ = ab.shape
    a = ab[..., :2 * DN_HEADS].reshape(b, t, 2, DN_HEADS)[:, :, d].astype(F32)
    bt = ab[..., 2 * DN_HEADS:].reshape(b, t, 2, DN_HEADS)[:, :, d].astype(F32)
    beta = jax.nn.sigmoid(bt)
    glog = -jnp.exp(a_log[d].astype(F32)) * jax.nn.softplus(a + dt_bias[d].astype(F32))
    return beta.transpose(0, 2, 1), glog.transpose(0, 2, 1)


def _delta_prep(k, v, beta, glog):
    c = k.shape[3]
    g = jnp.cumsum(glog, -1)
    idx = jnp.arange(c)
    lower = idx[:, None] >= idx[None, :]
    strict = idx[:, None] > idx[None, :]
    diff = g[..., :, None] - g[..., None, :]
    decay = jnp.where(lower, jnp.exp(jnp.where(lower, diff, 0.0)), 0.0)
    kk = jnp.einsum('bhnid,bhnjd->bhnij', k, k)
    a_mat = jnp.where(strict, beta[..., :, None] * kk * decay, 0.0) + jnp.eye(c, dtype=F32)
    rhs = jnp.concatenate([v * beta[..., None], k * (beta * jnp.exp(g))[..., None]], -1)
    sol = lax.linalg.triangular_solve(a_mat, rhs, left_side=True, lower=True)
    dv = v.shape[-1]
    u, w = sol[..., :dv], sol[..., dv:]
    k_tail = k * jnp.exp(g[..., -1:] - g)[..., None]
    return u, w, k_tail, g, decay


def _delta_states(u, w, k_tail, g, s0):
    g_last = jnp.exp(g[..., -1])

    def step(s, xs):
        u_n, w_n, kt_n, gl_n = xs
        v_new = u_n - jnp.einsum('bhck,bhkv->bhcv', w_n, s)
        return s * gl_n[..., None, None] + jnp.einsum('bhck,bhcv->bhkv', kt_n, v_new), s

    xs = (jnp.moveaxis(u, 2, 0), jnp.moveaxis(w, 2, 0), jnp.moveaxis(k_tail, 2, 0), jnp.moveaxis(g_last, 2, 0))
    s_fin, s_prev = lax.scan(step, s0, xs)
    return jnp.moveaxis(s_prev, 0, 2), s_fin


def _delta_out(q, k, u, w, g, decay, s_prev):
    v_new = u - jnp.einsum('bhnck,bhnkv->bhncv', w, s_prev)
    attn = jnp.einsum('bhnik,bhnjk->bhnij', q, k) * decay
    inter = jnp.einsum('bhnik,bhnkv->bhniv', q * jnp.exp(g)[..., None], s_prev)
    return inter + jnp.einsum('bhnij,bhnjv->bhniv', attn, v_new)


def delta_dir(k, v, beta, glog, s0, q=None):
    kc = to_chunks(k, DN_CHUNK)
    u, w, k_tail, g, decay = _delta_prep(kc, to_chunks(v, DN_CHUNK), to_chunks(beta, DN_CHUNK), to_chunks(glog, DN_CHUNK))
    s_prev, s_fin = _delta_states(u, w, k_tail, g, s0)
    out = None if q is None else from_chunks(_delta_out(to_chunks(q, DN_CHUNK), kc, u, w, g, decay, s_prev))
    return out, s_fin


def gated_head_norm(o, z, w):
    y = o * lax.rsqrt(jnp.mean(o * o, -1, keepdims=True) + EPS) * w.astype(F32)
    return (from_heads(y) * jax.nn.silu(z.astype(F32))).astype(z.dtype)


def spatial_gating(u, v, w_s, b_s):
    b, t, _ = u.shape
    u = jax.nn.gelu(u, approximate=False)
    v = jax.nn.gelu(v.astype(F32), approximate=False).reshape(b, t // SGU_CHUNK, SGU_CHUNK, SGU_GROUPS, SGU_CH)
    mu = jnp.mean(v, -1, keepdims=True)
    var = jnp.mean(jnp.square(v - mu), -1, keepdims=True)
    v = (v - mu) * lax.rsqrt(var + EPS)
    v = jnp.einsum('gpq,bnqgc->bnpgc', w_s.astype(F32), v) + b_s.astype(F32).T[None, None, :, :, None]
    return (u.astype(F32) * v.reshape(b, t, SGU_W)).astype(u.dtype)


def even_mixer(h_ctx, h_lat, w_in, decay_logit, gn_w, w_out, need_ctx):
    log_gamma = jax.nn.log_sigmoid(decay_logit.astype(F32))
    scale = RET_DK ** -0.5
    lat = h_lat @ w_in
    q_l = axial_rope(to_heads(lat[..., :E_K], RET_HEADS)) * scale
    k_l = axial_rope(to_heads(lat[..., E_K:E_V], RET_HEADS))
    v_l = to_heads(lat[..., E_V:E_G], RET_HEADS)
    if need_ctx:
        ctx = h_ctx @ w_in
        q_c = to_heads(ctx[..., :E_K], RET_HEADS) * scale
        kv_c = ctx[..., E_K:E_G]
    else:
        kv_c = h_ctx @ w_in[:, E_K:E_G]
    k_c = to_heads(kv_c[..., :RET_QK], RET_HEADS)
    v_c = to_heads(kv_c[..., RET_QK:], RET_HEADS)
    s_zero = jnp.zeros((h_lat.shape[0], RET_HEADS, RET_DK, RET_DV), F32)
    o_l, o_c = 0.0, 0.0
    for d, fl in enumerate((_noflip, _flip)):
        oc, s_c = retention_dir(fl(k_c), fl(v_c), log_gamma[d], s_zero, fl(q_c) if need_ctx else None)
        ol, _ = retention_dir(fl(k_l), fl(v_l), log_gamma[d], s_c, fl(q_l))
        o_l = o_l + fl(ol)
        if need_ctx:
            o_c = o_c + fl(oc)
    y_l = jnp.concatenate([group_norm_gate(o_l, lat[..., E_G:E_F], gn_w), fourier_mix(lat[..., E_F:])], -1) @ w_out
    y_c = None
    if need_ctx:
        y_c = jnp.concatenate([group_norm_gate(o_c, ctx[..., E_G:E_F], gn_w), fourier_mix(ctx[..., E_F:])], -1) @ w_out
    return y_c, y_l


def odd_mixer(h_ctx, h_lat, w_in, conv_w, a_log, dt_bias, norm_w, sp_w, sp_b, w_out, need_ctx):
    scale = DN_DK ** -0.5
    lat = h_lat @ w_in
    qkv_l = short_conv(lat[..., :O_Z], conv_w)
    q_l = l2norm(to_heads(qkv_l[..., :O_K], DN_HEADS)) * scale
    k_l = l2norm(to_heads(qkv_l[..., O_K:O_V], DN_HEADS))
    v_l = to_heads(qkv_l[..., O_V:], DN_HEADS)
    ab_l = lat[..., O_A:O_U]
    if need_ctx:
        ctx = h_ctx @ w_in
        qkv_c = short_conv(ctx[..., :O_Z], conv_w)
        q_c = l2norm(to_heads(qkv_c[..., :O_K], DN_HEADS)) * scale
        kv_c = qkv_c[..., O_K:]
        ab_c = ctx[..., O_A:O_U]
    else:
        kv_c = short_conv(h_ctx @ w_in[:, O_K:O_Z], conv_w[:, O_K:O_Z])
        ab_c = h_ctx @ w_in[:, O_A:O_U]
    k_c = l2norm(to_heads(kv_c[..., :DN_QK], DN_HEADS))
    v_c = to_heads(kv_c[..., DN_QK:], DN_HEADS)
    s_zero = jnp.zeros((h_lat.shape[0], DN_HEADS, DN_DK, DN_DV), F32)
    o_l, o_c = 0.0, 0.0
    for d, fl in enumerate((_noflip, _flip)):
        beta_c, glog_c = dn_gates(ab_c, d, a_log, dt_bias)
        beta_l, glog_l = dn_gates(ab_l, d, a_log, dt_bias)
        oc, s_c = delta_dir(fl(k_c), fl(v_c), fl(beta_c), fl(glog_c), s_zero, fl(q_c) if need_ctx else None)
        ol, _ = delta_dir(fl(k_l), fl(v_l), fl(beta_l), fl(glog_l), s_c, fl(q_l))
        o_l = o_l + fl(ol)
        if need_ctx:
            o_c = o_c + fl(oc)
    y_l = jnp.concatenate([gated_head_norm(o_l, lat[..., O_Z:O_A], norm_w),
                           spatial_gating(lat[..., O_U:O_S], lat[..., O_S:], sp_w, sp_b)], -1) @ w_out
    y_c = None
    if need_ctx:
        y_c = jnp.concatenate([gated_head_norm(o_c, ctx[..., O_Z:O_A], norm_w),
                               spatial_gating(ctx[..., O_U:O_S], ctx[..., O_S:], sp_w, sp_b)], -1) @ w_out
    return y_c, y_l


def hier_moe(h, wg, bg, we, be, w_gate, w_up, w_down):
    t, d = h.shape
    logit_g = (h @ wg).astype(F32) + bg.astype(F32)
    grp = jnp.argmax(logit_g, -1)
    p_grp = jnp.take_along_axis(jax.nn.softmax(logit_g, -1), grp[:, None], -1)[:, 0]
    logit_e = ((h @ we).astype(F32) + be.astype(F32)).reshape(t, MOE_GROUPS, MOE_EXP_PER_GROUP)
    logit_in = jnp.take_along_axis(logit_e, grp[:, None, None], 1)[:, 0]
    top_l, top_i = lax.top_k(logit_in, MOE_TOP_K)
    gate = p_grp[:, None] * jax.nn.softmax(top_l, -1)
    expert = grp[:, None] * MOE_EXP_PER_GROUP + top_i
    n_assign = t * MOE_TOP_K
    e_flat = expert.reshape(-1)
    g_flat = gate.reshape(-1)
    tok = jnp.repeat(jnp.arange(t), MOE_TOP_K)
    order = jnp.argsort(e_flat)
    e_s, tok_s, g_s = e_flat[order], tok[order], g_flat[order]
    counts = jnp.bincount(e_flat, length=MOE_EXPERTS)
    starts = jnp.cumsum(counts) - counts
    padded = (counts + MOE_BLOCK - 1) // MOE_BLOCK * MOE_BLOCK
    p_ends = jnp.cumsum(padded)
    dest = p_ends[e_s] - padded[e_s] + jnp.arange(n_assign) - starts[e_s]
    n_blocks = -(-(n_assign + MOE_EXPERTS * (MOE_BLOCK - 1)) // MOE_BLOCK)
    rows = jnp.zeros((n_blocks * MOE_BLOCK, d), h.dtype).at[dest].set(h[tok_s])
    blk_exp = jnp.minimum(jnp.searchsorted(p_ends, jnp.arange(n_blocks) * MOE_BLOCK, side='right'), MOE_EXPERTS - 1)

    def run_block(args):
        xb, e = args
        return (jax.nn.silu(xb @ w_gate[e]) * (xb @ w_up[e])) @ w_down[e]

    y = lax.map(run_block, (rows.reshape(n_blocks, MOE_BLOCK, d), blk_exp)).reshape(-1, d)
    contrib = y[dest].astype(F32) * g_s[:, None]
    return jnp.zeros((t, d), F32).at[tok_s].add(contrib).astype(h.dtype)


def setup_inputs(seed: int = 0) -> dict:
    key = jax.random.key(seed)
    ks = iter(jax.random.split(key, 40))
    n_even = (DEPTH + 1) // 2
    n_odd = DEPTH // 2

    def nrm(shape, scale=1.0):
        return jax.random.normal(next(ks), shape, F32) * scale

    def gain(shape):
        return 1.0 + nrm(shape, 0.02)

    x = nrm((BATCH, SEQ, D_MODEL))
    c = nrm((BATCH, D_MODEL))
    ctx = nrm((BATCH, CTX_LEN, D_MODEL))
    c_ctx = nrm((D_MODEL,))
    norm1_w = gain((DEPTH, D_MODEL))
    norm2_w = gain((DEPTH, D_MODEL))
    ada_w = nrm((DEPTH, D_MODEL, N_MOD * D_MODEL), 0.5 * D_MODEL ** -0.5)
    ada_b = nrm((DEPTH, N_MOD * D_MODEL), 0.01)
    even_w_in = nrm((n_even, D_MODEL, EVEN_IN), D_MODEL ** -0.5)
    ret_base = jnp.log(jnp.power(2.0, 5.0 + jnp.arange(RET_HEADS, dtype=F32)) - 1.0)
    ret_decay_logit = ret_base + nrm((n_even, 2, RET_HEADS), 0.1)
    ret_gn_w = gain((n_even, RET_V))
    even_w_out = nrm((n_even, EVEN_MIX, D_MODEL), EVEN_MIX ** -0.5)
    odd_w_in = nrm((n_odd, D_MODEL, ODD_IN), D_MODEL ** -0.5)
    dn_conv_w = nrm((n_odd, DN_CONV, O_Z), DN_CONV ** -0.5)
    dn_a_log = jnp.log(jax.random.uniform(next(ks), (n_odd, 2, DN_HEADS), F32, 1.0, 16.0))
    dt = jnp.exp(jax.random.uniform(next(ks), (n_odd, 2, DN_HEADS), F32, math.log(1e-3), math.log(1e-1)))
    dn_dt_bias = dt + jnp.log(-jnp.expm1(-dt))
    dn_norm_w = gain((n_odd, DN_DV))
    sgu_w = nrm((n_odd, SGU_GROUPS, SGU_CHUNK, SGU_CHUNK), SGU_CHUNK ** -0.5)
    sgu_b = gain((n_odd, SGU_GROUPS, SGU_CHUNK))
    odd_w_out = nrm((n_odd, ODD_MIX, D_MODEL), ODD_MIX ** -0.5)
    router_g_w = nrm((DEPTH, D_MODEL, MOE_GROUPS), D_MODEL ** -0.5)
    router_g_b = nrm((DEPTH, MOE_GROUPS), 0.01)
    router_e_w = nrm((DEPTH, D_MODEL, MOE_EXPERTS), D_MODEL ** -0.5)
    router_e_b = nrm((DEPTH, MOE_EXPERTS), 0.01)
    moe_w_gate = nrm((DEPTH, MOE_EXPERTS, D_MODEL, MOE_HIDDEN), D_MODEL ** -0.5)
    moe_w_up = nrm((DEPTH, MOE_EXPERTS, D_MODEL, MOE_HIDDEN), D_MODEL ** -0.5)
    moe_w_down = nrm((DEPTH, MOE_EXPERTS, MOE_HIDDEN, D_MODEL), MOE_HIDDEN ** -0.5)
    final_norm_w = gain((D_MODEL,))
    return {'x': x, 'c': c, 'ctx': ctx, 'c_ctx': c_ctx, 'norm1_w': norm1_w, 'norm2_w': norm2_w,
            'ada_w': ada_w, 'ada_b': ada_b, 'even_w_in': even_w_in, 'ret_decay_logit': ret_decay_logit,
            'ret_gn_w': ret_gn_w, 'even_w_out': even_w_out, 'odd_w_in': odd_w_in, 'dn_conv_w': dn_conv_w,
            'dn_a_log': dn_a_log, 'dn_dt_bias': dn_dt_bias, 'dn_norm_w': dn_norm_w, 'sgu_w': sgu_w,
            'sgu_b': sgu_b, 'odd_w_out': odd_w_out, 'router_g_w': router_g_w, 'router_g_b': router_g_b,
            'router_e_w': router_e_w, 'router_e_b': router_e_b, 'moe_w_gate': moe_w_gate,
            'moe_w_up': moe_w_up, 'moe_w_down': moe_w_down, 'final_norm_w': final_norm_w}


def reference(x, c, ctx, c_ctx, norm1_w, norm2_w, ada_w, ada_b, even_w_in, ret_decay_logit, ret_gn_w,
              even_w_out, odd_w_in, dn_conv_w, dn_a_log, dn_dt_bias, dn_norm_w, sgu_w, sgu_b, odd_w_out,
              router_g_w, router_g_b, router_e_w, router_e_b, moe_w_gate, moe_w_up, moe_w_down, final_norm_w):
    b, s, d = x.shape
    x_lat, x_ctx = x, ctx
    for layer in range(DEPTH):
        last = layer == DEPTH - 1
        mod_l = (jax.nn.silu(c) @ ada_w[layer] + ada_b[layer])[:, None, :]
        mod_c = jax.nn.silu(c_ctx) @ ada_w[layer] + ada_b[layer]
        sh1_l, sc1_l, g1_l, sh2_l, sc2_l, g2_l = jnp.split(mod_l, N_MOD, -1)
        sh1_c, sc1_c, g1_c, sh2_c, sc2_c, g2_c = jnp.split(mod_c, N_MOD, -1)
        h_l = rms_norm(x_lat, norm1_w[layer]) * (1.0 + sc1_l) + sh1_l
        h_c = rms_norm(x_ctx, norm1_w[layer]) * (1.0 + sc1_c) + sh1_c
        if layer % 2 == 0:
            i = layer // 2
            y_c, y_l = even_mixer(h_c, h_l, even_w_in[i], ret_decay_logit[i], ret_gn_w[i], even_w_out[i], not last)
        else:
            i = layer // 2
            y_c, y_l = odd_mixer(h_c, h_l, odd_w_in[i], dn_conv_w[i], dn_a_log[i], dn_dt_bias[i], dn_norm_w[i],
                                 sgu_w[i], sgu_b[i], odd_w_out[i], not last)
        x_lat = x_lat + g1_l * y_l
        h2_l = rms_norm(x_lat, norm2_w[layer]) * (1.0 + sc2_l) + sh2_l
        moe_args = (router_g_w[layer], router_g_b[layer], router_e_w[layer], router_e_b[layer],
                    moe_w_gate[layer], moe_w_up[layer], moe_w_down[layer])
        if last:
            x_lat = x_lat + g2_l * hier_moe(h2_l.reshape(-1, d), *moe_args).reshape(b, s, d)
        else:
            x_ctx = x_ctx + g1_c * y_c
            h2_c = rms_norm(x_ctx, norm2_w[layer]) * (1.0 + sc2_c) + sh2_c
            n_c = h2_c.shape[0] * h2_c.shape[1]
            out = hier_moe(jnp.concatenate([h2_c.reshape(-1, d), h2_l.reshape(-1, d)], 0), *moe_args)
            x_ctx = x_ctx + g2_c * out[:n_c].reshape(x_ctx.shape)
            x_lat = x_lat + g2_l * out[n_c:].reshape(b, s, d)
    return rms_norm(x_lat, final_norm_w)
```

```python
import numpy as np
import ml_dtypes
from contextlib import ExitStack
import concourse.bass as bass
import concourse.mybir as mybir
from concourse.bass_utils import run_bass_kernel_spmd

F32 = mybir.dt.float32
BF16 = mybir.dt.bfloat16
I32 = mybir.dt.int32
AF = mybir.ActivationFunctionType
ALU = mybir.AluOpType
AX = mybir.AxisListType


class View:
    def __init__(self, tile, ap):
        self.tile = tile
        self.ap = ap


class Tile:
    def __init__(self, name, handle, space):
        self.name = name
        self.h = handle
        self.space = space
        self.w = {}
        self.r = {}
        self.pw = {}
        self.dsem = {}

    def __getitem__(self, idx):
        return View(self, self.h[idx])


def _merge(d, tok):
    k, sem, val = tok
    if k not in d or d[k][1] < val:
        d[k] = (sem, val)


class KB:
    def __init__(self, nc, st):
        self.nc = nc
        self.st = st
        self.base_st = st
        self.all_recs = []
        self.sem_pool = {'sw': [], 'hw': []}
        self.alloc_log = []
        self.engs = {'pe': nc.tensor, 'act': nc.scalar, 'dve': nc.vector, 'pool': nc.gpsimd, 'sp': nc.sync}
        self.prog = {e: [] for e in self.engs}
        self.esem = {e: st.enter_context(nc.semaphore("es_" + e)) for e in self.engs}
        self.ecnt = {e: 0 for e in self.engs}
        self.seen = {e: {} for e in self.engs}
        self.nsem = len(self.engs)
        self.out_tokens = []
        self.ninst = 0

    def sb(self, name, shape, dt):
        self.uid = getattr(self, 'uid', 0) + 1
        name = "s%d_%s" % (self.uid, name)
        h = self.st.enter_context(self.nc.sbuf_tensor(name, list(shape), dt))
        t = Tile(name, h, 'sb')
        self.alloc_log.append(t)
        return t

    def ps(self, name, shape, dt):
        self.uid = getattr(self, 'uid', 0) + 1
        name = "p%d_%s" % (self.uid, name)
        h = self.st.enter_context(self.nc.psum_tensor(name, list(shape), dt))
        return Tile(name, h, 'ps')

    def dram(self, name, shape, dt, kind="Internal"):
        h = self.nc.dram_tensor(name, list(shape), dt, kind=kind)
        return Tile(name, h.ap(), 'dram')

    def _deps(self, eng, reads, writes, pwrites):
        deps = {}
        for v in reads:
            t = v.tile
            if t.pw:
                t.w = t.pw
                t.pw = {}
                t.r = {}
            for k, (sem, val) in t.w.items():
                _merge(deps, (k, sem, val))
            if t.space == 'ps':
                for k, (sem, val) in t.r.items():
                    if k != 'e_' + eng:
                        _merge(deps, (k, sem, val))
        for v in writes:
            t = v.tile
            if t.pw:
                t.w = t.pw
                t.pw = {}
                t.r = {}
            for d in (t.w, t.r):
                for k, (sem, val) in d.items():
                    _merge(deps, (k, sem, val))
        for v in pwrites:
            t = v.tile
            for d in (t.w, t.r):
                for k, (sem, val) in d.items():
                    _merge(deps, (k, sem, val))
        seen = self.seen[eng]
        for k, (sem, val) in deps.items():
            if eng == 'pe' and k == 'e_pe':
                continue
            if seen.get(k, 0) >= val:
                continue
            seen[k] = val
            self.prog[eng].append(('w', sem, val))

    def _commit(self, tok, reads, writes, pwrites):
        for v in reads:
            _merge(v.tile.r, tok)
        for v in writes:
            v.tile.w = {}
            v.tile.r = {}
            _merge(v.tile.w, tok)
        for v in pwrites:
            _merge(v.tile.pw, tok)

    def op(self, eng, fn, reads=(), writes=(), pwrites=()):
        reads = [v for v in reads if isinstance(v, View)]
        self._deps(eng, reads, writes, pwrites)
        self.ecnt[eng] += 1
        tok = ('e_' + eng, self.esem[eng], self.ecnt[eng])
        self.prog[eng].append(('i', fn, self.esem[eng], 1))
        self._commit(tok, reads, writes, pwrites)
        self.ninst += 1
        return tok

    def dma(self, out, in_, eng='sp', part=False, is_output=False, **kw):
        sbt = out.tile if out.tile.space == 'sb' else in_.tile
        kind = 'sw' if eng == 'pool' else 'hw'
        if kind not in sbt.dsem:
            if self.sem_pool[kind]:
                sbt.dsem[kind] = self.sem_pool[kind].pop()
            else:
                sem = self.base_st.enter_context(self.nc.semaphore("ds_%d" % len(self.all_recs)))
                sbt.dsem[kind] = {'sem': sem, 'cnt': 0, 'key': 'd%d' % len(self.all_recs)}
                self.all_recs.append(sbt.dsem[kind])
                self.nsem += 1
        reads = [in_]
        writes = [] if part else [out]
        pwrites = [out] if part else []
        self._deps(eng, reads, writes, pwrites)
        rec = sbt.dsem[kind]
        rec['cnt'] += 16
        tok = (rec['key'], rec['sem'], rec['cnt'])
        oap, iap = out.ap, in_.ap
        self.prog[eng].append(('i', lambda e: e.dma_start(out=oap, in_=iap, **kw), rec['sem'], 16))
        self._commit(tok, reads, writes, pwrites)
        if is_output:
            self.out_tokens.append(tok)
        self.ninst += 1
        return tok

    def dma_custom(self, eng, fn, sbt, reads, writes, pwrites=(), is_output=False):
        kind = 'sw' if eng == 'pool' else 'hw'
        if kind not in sbt.dsem:
            if self.sem_pool[kind]:
                sbt.dsem[kind] = self.sem_pool[kind].pop()
            else:
                sem = self.base_st.enter_context(self.nc.semaphore("ds_%d" % len(self.all_recs)))
                sbt.dsem[kind] = {'sem': sem, 'cnt': 0, 'key': 'd%d' % len(self.all_recs)}
                self.all_recs.append(sbt.dsem[kind])
        reads = list(reads)
        writes = list(writes)
        pwrites = list(pwrites)
        self._deps(eng, reads, writes, pwrites)
        rec = sbt.dsem[kind]
        rec['cnt'] += 16
        tok = (rec['key'], rec['sem'], rec['cnt'])
        self.prog[eng].append(('i', fn, rec['sem'], 16))
        self._commit(tok, reads, writes, pwrites)
        if is_output:
            self.out_tokens.append(tok)
        return tok

    def mm(self, out, lhsT, rhs, start=True, stop=True, **kw):
        o, l, r = out.ap, lhsT.ap, rhs.ap
        fn = lambda e: e.matmul(o, l, r, start=start, stop=stop, **kw)
        if start:
            return self.op('pe', fn, [lhsT, rhs], [out])
        return self.op('pe', fn, [lhsT, rhs], [], [out])

    def transpose(self, out, in_, ident):
        o, i, d = out.ap, in_.ap, ident.ap
        return self.op('pe', lambda e: e.transpose(o, i, d), [in_, ident], [out])

    def act(self, out, in_, func, bias=None, scale=None, accum=None, part=False, eng='act'):
        o, i = out.ap, in_.ap
        kw = {}
        reads = [in_]
        if bias is not None:
            if isinstance(bias, View):
                kw['bias'] = bias.ap
                reads.append(bias)
            else:
                kw['bias'] = bias
        if scale is not None:
            if isinstance(scale, View):
                kw['scale'] = scale.ap
                reads.append(scale)
            else:
                kw['scale'] = scale
        ws = [] if part else [out]
        pws = [out] if part else []
        if accum is not None:
            kw['accum_out'] = accum.ap
            ws = ws + [accum]
        return self.op('act', lambda e: e.activation(o, i, func, **kw), reads, ws, pws)

    def tt(self, out, a, b, op, eng='dve', part=False):
        o, x, y = out.ap, a.ap, b.ap
        ws = [] if part else [out]
        pws = [out] if part else []
        return self.op(eng, lambda e: e.tensor_tensor(o, x, y, op), [a, b], ws, pws)

    def ts(self, out, a, s1, s2, op0, op1=None, eng='dve', part=False, accum=None):
        o, x = out.ap, a.ap
        reads = [a]
        v1 = s1.ap if isinstance(s1, View) else s1
        v2 = s2.ap if isinstance(s2, View) else s2
        if isinstance(s1, View):
            reads.append(s1)
        if isinstance(s2, View):
            reads.append(s2)
        ws = [] if part else [out]
        pws = [out] if part else []
        kw = {}
        if accum is not None:
            kw['accum_out'] = accum.ap
            ws = ws + [accum]
        if op1 is None:
            return self.op(eng, lambda e: e.tensor_scalar(o, x, v1, None, op0, **kw), reads, ws, pws)
        return self.op(eng, lambda e: e.tensor_scalar(o, x, v1, v2, op0, op1, **kw), reads, ws, pws)

    def stt(self, out, a, s, b, op0, op1, eng='dve', part=False):
        o, x, y = out.ap, a.ap, b.ap
        reads = [a, b]
        sv = s.ap if isinstance(s, View) else s
        if isinstance(s, View):
            reads.append(s)
        ws = [] if part else [out]
        pws = [out] if part else []
        return self.op(eng, lambda e: e.scalar_tensor_tensor(o, x, sv, y, op0, op1), reads, ws, pws)

    def copy(self, out, in_, eng='dve', part=False):
        o, i = out.ap, in_.ap
        ws = [] if part else [out]
        pws = [out] if part else []
        if eng == 'act':
            return self.op('act', lambda e: e.copy(o, i), [in_], ws, pws)
        return self.op(eng, lambda e: e.tensor_copy(o, i), [in_], ws, pws)

    def reduce(self, out, in_, op, axis=AX.X, eng='dve'):
        o, i = out.ap, in_.ap
        return self.op(eng, lambda e: e.tensor_reduce(o, i, axis, op), [in_], [out])

    def memset(self, out, val, eng='dve'):
        o = out.ap
        return self.op(eng, lambda e: e.memset(o, val), [], [out])

    def recip(self, out, in_):
        o, i = out.ap, in_.ap
        return self.op('dve', lambda e: e.reciprocal(o, i), [in_], [out])

    def barrier(self):
        for eng in self.engs:
            seen = self.seen[eng]
            for e2 in self.engs:
                if self.ecnt[e2] > 0 and seen.get('e_' + e2, 0) < self.ecnt[e2] and not (eng == 'pe' and e2 == 'pe'):
                    seen['e_' + e2] = self.ecnt[e2]
                    self.prog[eng].append(('w', self.esem[e2], self.ecnt[e2]))
            for rec in self.all_recs:
                k = rec['key']
                if rec['cnt'] > 0 and seen.get(k, 0) < rec['cnt']:
                    seen[k] = rec['cnt']
                    self.prog[eng].append(('w', rec['sem'], rec['cnt']))

    def phase(self):
        return _Phase(self)

    def emit(self):
        nc = self.nc
        for (k, sem, val) in self.out_tokens:
            if self.seen['sp'].get(k, 0) < val:
                self.seen['sp'][k] = val
                self.prog['sp'].append(('w', sem, val))
        with nc.Block() as block:
            def run(e, lst):
                for it in lst:
                    if it[0] == 'w':
                        e.wait_ge(it[1], it[2])
                    else:
                        it[1](e).then_inc(it[2], it[3])

            @block.tensor
            def _(e):
                run(e, self.prog['pe'])

            @block.scalar
            def _(e):
                run(e, self.prog['act'])

            @block.vector
            def _(e):
                run(e, self.prog['dve'])

            @block.gpsimd
            def _(e):
                run(e, self.prog['pool'])

            @block.sync
            def _(e):
                run(e, self.prog['sp'])


class _Phase:
    def __init__(self, kb):
        self.kb = kb

    def __enter__(self):
        self.old = self.kb.st
        self.s = ExitStack()
        self.s.__enter__()
        self.kb.st = self.s
        self.mark = len(self.kb.alloc_log)
        return self

    def __exit__(self, *a):
        if a[0] is None:
            self.kb.barrier()
            for t in self.kb.alloc_log[self.mark:]:
                for kind, rec in t.dsem.items():
                    self.kb.sem_pool[kind].append(rec)
                t.dsem = {}
            del self.kb.alloc_log[self.mark:]
        self.s.__exit__(*a)
        self.kb.st = self.old
        return False


def bc(v, shape):
    return View(v.tile, v.ap.to_broadcast(list(shape)))


def rr(v, pat, **kw):
    return View(v.tile, v.ap.rearrange(pat, **kw))

import math

D = 1024
NT = 18
NTC = 2
NTL = 16
TT = NT * 128
EPS = 1e-6
SCALE = 128 ** -0.5
TBLK = [(0, 512), (512, 512), (1024, 512), (1536, 512), (2048, 256)]


def host_consts():
    c = {}
    c['ident'] = np.eye(128).astype(ml_dtypes.bfloat16)
    c['identf'] = np.eye(128).astype(np.float32)
    t = np.arange(2048)
    row = (t // 64).astype(np.float64)
    col = (t % 64).astype(np.float64)
    inv = 10000.0 ** (-np.arange(32, dtype=np.float64) / 32)
    ang = np.concatenate([row[:, None] * inv, col[:, None] * inv], -1)
    cs, sn = np.cos(ang), np.sin(ang)

    def lay(a):
        return np.ascontiguousarray(a.reshape(16, 128, 64).transpose(1, 0, 2)).astype(np.float32)
    c['cos_q'] = lay(cs * SCALE)
    c['sin_q'] = lay(sn * SCALE)
    c['cos_k'] = lay(cs)
    c['sin_k'] = lay(sn)
    p = np.arange(128)
    i = p[None, :]
    j = p[:, None]
    c['dpos'] = np.maximum(i - j, 0).astype(np.float32)
    c['indf'] = (i >= j).astype(np.float32)
    c['dneg'] = np.maximum(j - i, 0).astype(np.float32)
    c['indb'] = (j >= i).astype(np.float32)
    c['ptab'] = np.stack([127 - p, p, p + 1, 128 - p], 1).astype(np.float32)
    ch = np.arange(128)
    phi = 2 * np.pi * np.outer(ch, ch) / 128
    c['dft_c'] = np.concatenate([np.cos(phi), np.sin(phi)], 1) / np.sqrt(128)
    c['dft_c'] = c['dft_c'].astype(ml_dtypes.bfloat16)
    tl = np.arange(2048)
    th = 2 * np.pi * ((np.outer(tl, tl)) % 2048) / 2048
    c['dft_tl_c'] = (np.cos(th) / np.sqrt(2048)).astype(ml_dtypes.bfloat16)
    c['dft_tl_s'] = (-np.sin(th) / np.sqrt(2048)).astype(ml_dtypes.bfloat16)
    tc = np.arange(256)
    th = 2 * np.pi * ((np.outer(tc, tc)) % 256) / 256
    c['dft_tc_c'] = (np.cos(th) / np.sqrt(256)).astype(ml_dtypes.bfloat16)
    c['dft_tc_s'] = (-np.sin(th) / np.sqrt(256)).astype(ml_dtypes.bfloat16)
    c['lowS'] = (j > i).astype(np.float32)
    c['upS'] = (j < i).astype(np.float32)
    c['onesf'] = np.ones((128, 128), np.float32)
    c['thr'] = np.tile((256.0 * np.arange(18))[None, :], (128, 1)).astype(np.float32)
    c['bvals'] = np.tile(np.arange(50.0)[None, :], (128, 1)).astype(np.float32)
    c['pcol'] = np.arange(128.0).reshape(128, 1).astype(np.float32)
    return c


CONST_SPECS = {
    'ident': ([128, 128], BF16), 'identf': ([128, 128], F32),
    'cos_q': ([128, 16, 64], F32), 'sin_q': ([128, 16, 64], F32),
    'cos_k': ([128, 16, 64], F32), 'sin_k': ([128, 16, 64], F32),
    'dpos': ([128, 128], F32), 'indf': ([128, 128], F32), 'dneg': ([128, 128], F32), 'indb': ([128, 128], F32),
    'ptab': ([128, 4], F32), 'dft_c': ([128, 256], BF16),
    'dft_tl_c': ([2048, 2048], BF16), 'dft_tl_s': ([2048, 2048], BF16),
    'dft_tc_c': ([256, 256], BF16), 'dft_tc_s': ([256, 256], BF16),
}

IN_SPECS = {
    'x_all': ([TT, D], F32), 'c2t': ([128, 16], F32),
    'norm1_w': ([2, D], F32), 'norm2_w': ([2, D], F32),
    'ada_w': ([2, D, 6 * D], F32), 'ada_b': ([2, 6 * D], F32),
    'even_w_in': ([1, D, 2560], F32), 'ret_decay_logit': ([1, 8], F32), 'ret_gn_w': ([1, 512], F32),
    'even_w_out': ([1, D, D], F32),
    'router_w': ([2, D, 36], F32), 'router_b': ([2, 36], F32),
    'moe_w_gate': ([2, 32, D, 512], F32), 'moe_w_up': ([2, 32, D, 512], F32), 'moe_w_down': ([2, 32, 512, D], F32),
    'final_norm_w': ([D], F32),
}


SPARSE = True


class Ctx:
    pass


class LazyInputs(dict):
    def __init__(self, kb):
        super().__init__()
        self.kb = kb

    def __missing__(self, k):
        shp, dt = IN_SPECS[k] if k in IN_SPECS else CONST_SPECS[k]
        t = self.kb.dram(k, shp, dt, kind="ExternalInput")
        self[k] = t
        return t


def build(stop_after=None, dbg=()):
    nc = bass.Bass("TRN2", target_bir_lowering=False)
    st = ExitStack()
    with st:
        kb = KB(nc, st)
        g = Ctx()
        g.kb = kb
        g.I = LazyInputs(kb)
        g.out = kb.dram("out", [2048, D], F32, kind="ExternalOutput")
        g.X = kb.dram("Xs", [TT, D], F32)
        g.dbg = {}
        g.regcache = {}
        g.dbg_want = dbg
        g.pT = [kb.ps("pT%d" % i, [128, 1024], BF16) for i in range(2)]
        g.pA = [kb.ps("pA%d" % i, [128, 512], F32) for i in range(6)]
        g.pTi = 0
        g.pAi = 0
        g.ident = kb.sb("ident", [128, 128], BF16)
        kb.dma(g.ident[:], g.I['ident'][:])
        g.identf = kb.sb("identf", [128, 128], F32)
        kb.dma(g.identf[:], g.I['identf'][:])
        g.stop_after = stop_after
        layer0(g)
        if stop_after == 'mix0':
            finish_debug(g)
        else:
            (moe_sparse if SPARSE else moe_stage)(g, 0, False)
            if stop_after == 'l0':
                finish_debug(g)
            else:
                layer1(g)
                if stop_after == 'mix1':
                    finish_debug(g)
                else:
                    (moe_sparse if SPARSE else moe_stage)(g, 1, True)
        kb.emit()
        nc._used_inputs = list(g.I.keys())
        nc._dbg = list(g.dbg.keys())
    return nc


def nextA(g):
    g.pAi = (g.pAi + 1) % len(g.pA)
    return g.pA[g.pAi]


def nextT(g):
    g.pTi = (g.pTi + 1) % len(g.pT)
    return g.pT[g.pTi]


def dump(g, name, view, shape, dt=F32):
    if name not in g.dbg_want:
        return
    kb = g.kb
    d = kb.dram("dbg_" + name, shape, dt, kind="ExternalOutput")
    g.dbg[name] = d
    kb.dma(d[:], view, is_output=True)


def finish_debug(g):
    kb = g.kb
    with kb.phase():
        t = kb.sb("fin_t", [128, D], F32)
        dc = kb.dram("dbg_Xc", [256, D], F32, kind="ExternalOutput")
        for n in range(NTC):
            kb.dma(t[:], g.X[n * 128:(n + 1) * 128, :])
            kb.dma(dc[n * 128:(n + 1) * 128, :], t[:], is_output=True)
        for n in range(NTL):
            kb.dma(t[:], g.X[(n + 2) * 128:(n + 3) * 128, :])
            kb.dma(g.out[n * 128:(n + 1) * 128, :], t[:], is_output=True)


def bcast_row(kb, dst, dram_view):
    v = View(dram_view.tile, dram_view.ap.partition_broadcast(128))
    kb.dma(dst, v)


def compute_mod(g, L, half, modt):
    kb = g.kb
    with kb.phase():
        c2 = kb.sb("c2", [128, 16], F32)
        c2s = kb.sb("c2s", [128, 16], BF16)
        crep = kb.sb("crep", [128, 16, 128], BF16)
        kb.dma(c2[:], g.I['c2t'][:])
        kb.act(c2s[:], c2[:], AF.Silu)
        for j in range(16):
            kb.copy(crep[:, j, :], bc(c2s[:, j:j + 1], [128, 128]), part=True)
        wb = [kb.sb("adaw%d" % i, [128, 8, 512], BF16) for i in range(2)]
        bb = [kb.sb("adab%d" % i, [128, 512], F32) for i in range(2)]
        nw = kb.sb("nw", [128, D], F32)
        nwname = 'norm1_w' if half == 0 else 'norm2_w'
        bcast_row(kb, nw[:], g.I[nwname][L, :])
        for blk in range(6):
            c0 = half * 3072 + blk * 512
            w = wb[blk % 2]
            b = bb[blk % 2]
            kb.dma(w[:], rr(g.I['ada_w'][L, :, c0:c0 + 512], "(k p) c -> p k c", p=128), eng='pool')
            bcast_row(kb, b[:], g.I['ada_b'][L, c0:c0 + 512])
            for r in range(2):
                ps = nextA(g)
                for k in range(8):
                    kb.mm(ps[:, :], crep[:, r * 8 + k, :], w[:, k, :], start=(k == 0), stop=(k == 7))
                kb.tt(modt[r][:, blk * 512:(blk + 1) * 512], ps[:, :], b[:], ALU.add, part=True)
        for r in range(2):
            kb.stt(modt[r][:, 1024:2048], modt[r][:, 1024:2048], 1.0, nw[:], ALU.add, ALU.mult)


def norm_mod_tile(g, xt, modr, hb, ss, tmp):
    kb = g.kb
    if isinstance(ss, list):
        g.nmi = getattr(g, 'nmi', 0) + 1
        ss = ss[g.nmi % len(ss)]
        tmp = tmp[g.nmi % len(tmp)]
    kb.memset(ss[:, 0:1], 0.0)
    kb.act(tmp[:], xt, AF.Square, accum=ss[:, 0:1])
    kb.ts(ss[:, 1:2], ss[:, 0:1], 1.0 / D, EPS, ALU.mult, ALU.add)
    kb.act(ss[:, 2:3], ss[:, 1:2], AF.Sqrt)
    kb.recip(ss[:, 3:4], ss[:, 2:3])
    kb.stt(tmp[:], xt, ss[:, 3:4], modr[:, 1024:2048], ALU.mult, ALU.mult)
    kb.tt(hb, tmp[:], modr[:, 0:1024], ALU.add)


def transpose_into(g, dst3, src_bf, nchunk, eng='act'):
    kb = g.kb
    pt = nextT(g)
    for k in range(nchunk):
        kb.transpose(pt[:, k * 128:(k + 1) * 128], View(src_bf.tile, src_bf.ap[:, k * 128:(k + 1) * 128]), g.ident[:])
    kb.copy(dst3, rr(pt[:, 0:nchunk * 128], "p (k t) -> p k t", k=nchunk), eng=eng, part=True)


def layer0(g):
    kb = g.kb
    I = g.I
    L = 0
    with kb.phase():
        mixT = kb.sb("mixT", [128, 8, TT], BF16)
        SGs = kb.dram("SGs", [TT, 512], BF16)
        FTs = kb.dram("FTs", [4, 128, TT], BF16)
        modt = [kb.sb("modt%d" % r, [128, 3072], F32) for r in range(2)]
        compute_mod(g, L, 0, modt)
        with kb.phase():
            qT = kb.sb("qT", [128, 4, TT], BF16)
            kT = kb.sb("kT", [128, 4, TT], BF16)
            k_tm = kb.sb("k_tm", [128, NT, 512], BF16)
            v_tm = kb.sb("v_tm", [128, NT, 512], BF16)
            with kb.phase():
                hT = mixT
                sgt = [kb.sb("sgt%d" % i, [128, 512], BF16) for i in range(2)]
                xt = [kb.sb("xt%d" % i, [128, D], F32) for i in range(2)]
                tmp = [kb.sb("tmp_%d" % i, [128, D], F32) for i in range(2)]
                hb_l = [kb.sb("hb_%d" % i, [128, D], BF16) for i in range(2)]
                ss = [kb.sb("ss_%d" % i, [128, 4], F32) for i in range(2)]
                def mk_norm(n):
                    def gen(s):
                        kb.dma(xt[s][:], I['x_all'][n * 128:(n + 1) * 128, :])
                        yield
                        yield from norm_mod_gen(g, xt[s][:], modt[0 if n >= NTC else 1], hb_l[s][:], ss[s], tmp[s])
                        yield from transpose_gen(g, s, hT[:, :, n * 128:(n + 1) * 128], hb_l[s][:], 8)
                    return gen
                run_chains([mk_norm(n) for n in range(NT)], 2)
                wb = [kb.sb("winb%d" % i, [128, 8, 512], BF16) for i in range(2)]
                rope_c = kb.sb("rope_c", [128, 16, 64], F32)
                rope_s = kb.sb("rope_s", [128, 16, 64], F32)
                pf2 = [kb.sb("pf%d" % i, [128, 512], F32) for i in range(2)]
                ra2 = [kb.sb("ra%d" % i, [128, 4, 64], F32) for i in range(4)]
                rb2 = [kb.sb("rb%d" % i, [128, 4, 64], F32) for i in range(4)]
                pb2 = [kb.sb("pb%d" % i, [128, 512], BF16) for i in range(2)]
                for cb in range(5):
                    w = wb[cb % 2]
                    kb.dma(w[:], rr(I['even_w_in'][0, :, cb * 512:(cb + 1) * 512], "(k p) c -> p k c", p=128), eng='pool')
                    if cb < 2:
                        kb.dma(rope_c[:], I['cos_q' if cb == 0 else 'cos_k'][:])
                        kb.dma(rope_s[:], I['sin_q' if cb == 0 else 'sin_k'][:])
                    if cb < 4:
                        def mk_proj(n, cb=cb, w=w):
                            def gen(s):
                                ps = nextA_s(g, s)
                                for k in range(8):
                                    kb.mm(ps[:, :], hT[:, k, n * 128:(n + 1) * 128], w[:, k, :], start=(k == 0), stop=(k == 7))
                                yield
                                if cb in (0, 1):
                                    pf, pb = pf2[s], pb2[s]
                                    ra, rb = ra2[s * 2], rb2[s * 2]
                                    ra_, rb_ = ra2[s * 2 + 1], rb2[s * 2 + 1]
                                    dstb = pb[:] if cb == 0 else k_tm[:, n, :]
                                    if n >= NTC:
                                        nl = n - NTC
                                        kb.copy(pf[:], ps[:, :], eng='act')
                                        yield
                                        p3 = rr(pf[:], "p (h e) -> p h e", h=4)
                                        x1 = View(pf, p3.ap[:, :, 0:64])
                                        x2 = View(pf, p3.ap[:, :, 64:128])
                                        cosb = bc(rope_c[:, nl:nl + 1, :], [128, 4, 64])
                                        sinb = bc(rope_s[:, nl:nl + 1, :], [128, 4, 64])
                                        d3 = rr(dstb, "p (h e) -> p h e", h=4)
                                        o1 = View(dstb.tile, d3.ap[:, :, 0:64])
                                        o2 = View(dstb.tile, d3.ap[:, :, 64:128])
                                        kb.tt(ra[:], x1, cosb, ALU.mult)
                                        kb.tt(ra_[:], x1, sinb, ALU.mult, eng='pool')
                                        yield
                                        kb.tt(rb[:], x2, sinb, ALU.mult)
                                        kb.tt(rb_[:], x2, cosb, ALU.mult, eng='pool')
                                        yield
                                        kb.tt(o1, ra[:], rb[:], ALU.subtract, part=True)
                                        kb.tt(o2, ra_[:], rb_[:], ALU.add, part=True, eng='pool')
                                        yield
                                    else:
                                        if cb == 0:
                                            kb.act(dstb, ps[:, :], AF.Copy, scale=SCALE)
                                        else:
                                            kb.copy(dstb, ps[:, :], eng='act')
                                        yield
                                    dT = qT if cb == 0 else kT
                                    yield from transpose_gen(g, s, dT[:, :, n * 128:(n + 1) * 128], dstb, 4, eng='dve')
                                elif cb == 2:
                                    kb.copy(v_tm[:, n, :], ps[:, :], eng='act', part=True)
                                    yield
                                else:
                                    kb.act(sgt[s][:], ps[:, :], AF.Silu)
                                    yield
                                    kb.dma(SGs[n * 128:(n + 1) * 128, :], sgt[s][:], part=True)
                                    yield
                            return gen
                        run_chains([mk_proj(n) for n in range(NT)], 2)
                    else:
                        for gi in range(4):
                            for (t0, tl) in TBLK:
                                ps = nextA(g)
                                for k in range(8):
                                    kb.mm(ps[:, 0:tl], w[:, k, gi * 128:(gi + 1) * 128], hT[:, k, t0:t0 + tl], start=(k == 0), stop=(k == 7))
                                ft_ = sgt[(gi + t0 // 512) % 2]
                                kb.copy(ft_[:, 0:tl], ps[:, 0:tl], eng='act')
                                kb.dma(FTs[gi, :, t0:t0 + tl], ft_[:, 0:tl], part=True)
            dump(g, 'qT', qT[:, 0, 256:768], [128, 512], BF16)
            dump(g, 'v_tm', v_tm[:, 2, :], [128, 512], BF16)
            retention(g, qT, kT, k_tm, v_tm, SGs, mixT)
        fourier(g, FTs, mixT)
        dump(g, 'mixT_r', mixT[:, 0, 256:768], [128, 512], BF16)
        dump(g, 'mixT_f', mixT[:, 4, 256:768], [128, 512], BF16)
        with kb.phase():
            wo = kb.sb("wo", [128, 8, D], BF16)
            kb.dma(wo[:], rr(I['even_w_out'][0, :, :], "(k p) c -> p k c", p=128), eng='pool')
            xt = [kb.sb("xt4_%d" % i, [128, D], F32) for i in range(2)]
            yt_l = [kb.sb("yt4_%d" % i, [128, D], F32) for i in range(2)]
            def _mk(n):
                def gen(s):
                    x_ = xt[s]
                    kb.dma(x_[:], I['x_all'][n * 128:(n + 1) * 128, :])
                    yield
                    mr = modt[0 if n >= NTC else 1]
                    for dh in range(2):
                        ps = nextA_s(g, s)
                        for k in range(8):
                            kb.mm(ps[:, :], mixT[:, k, n * 128:(n + 1) * 128], wo[:, k, dh * 512:(dh + 1) * 512], start=(k == 0), stop=(k == 7))
                        kb.tt(yt_l[s][:, dh * 512:(dh + 1) * 512], ps[:, :], mr[:, 2048 + dh * 512:2048 + (dh + 1) * 512], ALU.mult, part=True)
                        yield
                    kb.tt(x_[:], x_[:], yt_l[s][:], ALU.add)
                    yield
                    kb.dma(g.X[n * 128:(n + 1) * 128, :], x_[:])
                    yield
                    yield
                return gen
            run_chains([_mk(n) for n in range(NT)], 2)


def retention(g, qT, kT, k_tm, v_tm, SGs, mixT):
    kb = g.kb
    I = g.I
    with kb.phase():
        dl = kb.sb("dl", [128, 8], F32)
        lg = kb.sb("lg", [128, 8], F32)
        bcast_row(kb, dl[:], I['ret_decay_logit'][0, :])
        kb.act(lg[:], dl[:], AF.Exp, scale=-1.0)
        kb.ts(lg[:], lg[:], 1.0, None, ALU.add)
        kb.act(lg[:], lg[:], AF.Ln)
        kb.ts(lg[:], lg[:], -1.0, None, ALU.mult)
        consts = {nm: kb.sb("rc_" + nm, [128, 128], F32) for nm in ('dpos', 'indf', 'dneg', 'indb')}
        for nm in consts:
            kb.dma(consts[nm][:], I[nm][:])
        ptab = kb.sb("ptab", [128, 4], F32)
        kb.dma(ptab[:], I['ptab'][:])
        MT = kb.sb("MT", [128, 4, 128], F32)
        mtmp = kb.sb("mtmp", [128, 128], F32)
        pv = kb.sb("pv", [128, 4, 4], F32)
        g128 = kb.sb("g128", [128, 2, 512], F32)
        for h in range(4):
            kb.act(MT[:, h, :], consts['dpos'][:], AF.Exp, scale=lg[:, h:h + 1], part=True)
            kb.tt(MT[:, h, :], MT[:, h, :], consts['indf'][:], ALU.mult, part=True)
            kb.act(mtmp[:], consts['dneg'][:], AF.Exp, scale=lg[:, 4 + h:5 + h])
            kb.tt(mtmp[:], mtmp[:], consts['indb'][:], ALU.mult)
            kb.tt(MT[:, h, :], MT[:, h, :], mtmp[:], ALU.add, part=True)
            kb.act(pv[:, 0, h:h + 1], ptab[:, 0:1], AF.Exp, scale=lg[:, h:h + 1], part=True)
            kb.act(pv[:, 1, h:h + 1], ptab[:, 1:2], AF.Exp, scale=lg[:, 4 + h:5 + h], part=True)
            kb.act(pv[:, 2, h:h + 1], ptab[:, 2:3], AF.Exp, scale=lg[:, h:h + 1], part=True)
            kb.act(pv[:, 3, h:h + 1], ptab[:, 3:4], AF.Exp, scale=lg[:, 4 + h:5 + h], part=True)
            for d in range(2):
                kb.act(mtmp[:, 0:1], lg[:, d * 4 + h:d * 4 + h + 1], AF.Exp, scale=128.0)
                kb.copy(g128[:, d, h * 128:(h + 1) * 128], bc(mtmp[:, 0:1], [128, 128]), part=True)
        S_all = [kb.sb("S_all%d" % d, [128, NT, 512], BF16) for d in range(2)]
        S = kb.sb("S_run", [128, 512], F32)
        S2 = kb.sb("S_tmp", [128, 512], F32)
        vt = kb.sb("vtail", [128, 512], BF16)
        for d in range(2):
            order = list(range(NT)) if d == 0 else [1, 0] + list(range(NT - 1, 1, -1))
            kb.memset(S[:], 0.0)
            for idx, n in enumerate(order):
                kb.copy(S_all[d][:, n, :], S[:], eng='act', part=True)
                if idx == NT - 1:
                    break
                for h in range(4):
                    kb.ts(vt[:, h * 128:(h + 1) * 128], v_tm[:, n, h * 128:(h + 1) * 128], pv[:, d, h:h + 1], None, ALU.mult, part=True)
                ps = nextA(g)
                for h in range(4):
                    kb.mm(ps[:, h * 128:(h + 1) * 128], k_tm[:, n, h * 128:(h + 1) * 128], vt[:, h * 128:(h + 1) * 128], start=True, stop=True)
                kb.tt(S2[:], S[:], g128[:, d, :], ALU.mult)
                kb.tt(S[:], S2[:], ps[:, :], ALU.add)
        A_l = [kb.sb("A_bf%d" % i, [128, 4, 128], BF16) for i in range(2)]
        o_l = [kb.sb("o_ret%d" % i, [128, 4, 128], F32) for i in range(2)]
        o2_l = [kb.sb("o_ret2_%d" % i, [128, 4, 128], F32) for i in range(2)]
        sq_l = [kb.sb("o_sq%d" % i, [128, 4, 128], F32) for i in range(2)]
        st_l = [kb.sb("gn_st%d" % i, [128, 6, 4], F32) for i in range(2)]
        yb_l = [kb.sb("y_ret%d" % i, [128, 512], BF16) for i in range(2)]
        gnw = kb.sb("gnw", [128, 512], F32)
        sgl = [kb.sb("sgl%d" % i, [128, 512], BF16) for i in range(2)]
        bcast_row(kb, gnw[:], I['ret_gn_w'][0, :])
        def _mk(n):
            def gen(s):
                tsl = slice(n * 128, (n + 1) * 128)
                A, o, o2, sq, st_, yb = A_l[s], o_l[s], o2_l[s], sq_l[s], st_l[s], yb_l[s]
                kb.dma(sgl[s][:], SGs[tsl, :])
                yield
                pS = nextA_s(g, s)
                for h in range(4):
                    kb.mm(pS[:, h * 128:(h + 1) * 128], kT[:, h, tsl], qT[:, h, tsl], start=True, stop=True)
                kb.tt(A[:], rr(pS[:, :], "p (h i) -> p h i", h=4), MT[:], ALU.mult)
                yield
                pI = nextA_s(g, s)
                pF = nextA_s(g, s)
                pB = nextA_s(g, s)
                for h in range(4):
                    hs = slice(h * 128, (h + 1) * 128)
                    kb.mm(pI[:, hs], A[:, h, :], v_tm[:, n, hs], start=True, stop=True)
                    kb.mm(pF[:, hs], qT[:, h, tsl], S_all[0][:, n, hs], start=True, stop=True)
                    kb.mm(pB[:, hs], qT[:, h, tsl], S_all[1][:, n, hs], start=True, stop=True)
                kb.copy(rr(o2[:], "p h e -> p (h e)"), pI[:, :], eng='act')
                yield
                for h in range(4):
                    hs = slice(h * 128, (h + 1) * 128)
                    kb.stt(o2[:, h, :], pF[:, hs], pv[:, 2, h:h + 1], o2[:, h, :], ALU.mult, ALU.add, part=True)
                    yield
                for h in range(4):
                    hs = slice(h * 128, (h + 1) * 128)
                    kb.stt(o[:, h, :], pB[:, hs], pv[:, 3, h:h + 1], o2[:, h, :], ALU.mult, ALU.add, part=True)
                    yield
                if n == 2:
                    dump(g, 'o_ret', o[:], [128, 4, 128], F32)
                kb.reduce(st_[:, 0, :], o[:], ALU.add)
                yield
                kb.tt(sq[:], o[:], o[:], ALU.mult)
                yield
                kb.reduce(st_[:, 1, :], sq[:], ALU.add)
                yield
                kb.ts(st_[:, 2, :], st_[:, 0, :], 1.0 / 128, None, ALU.mult)
                yield
                kb.tt(st_[:, 3, :], st_[:, 2, :], st_[:, 2, :], ALU.mult)
                yield
                kb.stt(st_[:, 4, :], st_[:, 1, :], 1.0 / 128, st_[:, 3, :], ALU.mult, ALU.subtract)
                yield
                kb.ts(st_[:, 4, :], st_[:, 4, :], EPS, None, ALU.add)
                yield
                kb.act(st_[:, 5, :], st_[:, 4, :], AF.Sqrt)
                yield
                kb.recip(st_[:, 3, :], st_[:, 5, :])
                yield
                for h in range(4):
                    kb.ts(sq[:, h, :], o[:, h, :], st_[:, 2, h:h + 1], st_[:, 3, h:h + 1], ALU.subtract, ALU.mult, part=True)
                    yield
                sqf = rr(sq[:], "p h e -> p (h e)")
                kb.tt(sqf, sqf, gnw[:], ALU.mult)
                yield
                kb.tt(yb[:], sqf, sgl[s][:], ALU.mult)
                yield
                yield from transpose_gen(g, s, mixT[:, 0:4, tsl], yb[:], 4, eng='act')
                yield
            return gen
        run_chains([_mk(n) for n in range(NT)], 2)


def fourier(g, FTs, mixT):
    kb = g.kb
    I = g.I
    with kb.phase():
        cs = kb.sb("dftc", [128, 256], BF16)
        kb.dma(cs[:], I['dft_c'][:])
        fc = kb.sb("fc", [128, NT, 512], BF16)
        fs = kb.sb("fs", [128, NT, 512], BF16)
        ftl = [kb.sb("ftl%d" % i, [128, 4, 128], BF16) for i in range(2)]
        for n in range(NT):
            tsl = slice(n * 128, (n + 1) * 128)
            fT = ftl[n % 2]
            kb.dma(fT[:], rr(FTs[:, :, tsl], "g c t -> c g t"))
            for gp in range(2):
                ps = nextA(g)
                for gg in range(2):
                    gi = gp * 2 + gg
                    kb.mm(ps[:, gg * 256:(gg + 1) * 256], fT[:, gi, :], cs[:], start=True, stop=True)
                p3 = rr(ps[:, :], "p (g two c) -> p g two c", g=2, two=2)
                kb.copy(rr(fc[:, n, gp * 256:(gp + 1) * 256], "p (g c) -> p g c", g=2), View(ps, p3.ap[:, :, 0, :]), eng='act', part=True)
                kb.copy(rr(fs[:, n, gp * 256:(gp + 1) * 256], "p (g c) -> p g c", g=2), View(ps, p3.ap[:, :, 1, :]), eng='dve', part=True)
        tcc = kb.sb("tcc", [128, 2, 256], BF16)
        tcs = kb.sb("tcs", [128, 2, 256], BF16)
        kb.dma(tcc[:], rr(I['dft_tc_c'][:, :], "(k p) c -> p k c", p=128))
        kb.dma(tcs[:], rr(I['dft_tc_s'][:, :], "(k p) c -> p k c", p=128))
        for gi in range(4):
            gs = slice(gi * 128, (gi + 1) * 128)
            ps = nextA(g)
            for tc in range(2):
                kb.mm(ps[:, 0:256], fc[:, tc, gs], tcc[:, tc, :], start=(tc == 0), stop=False)
                kb.mm(ps[:, 0:256], fs[:, tc, gs], tcs[:, tc, :], start=False, stop=(tc == 1))
            kb.copy(mixT[:, 4 + gi, 0:256], ps[:, 0:256], eng='act', part=True)
        tlc = kb.sb("tlc", [128, 16, 512], BF16)
        tls = kb.sb("tls", [128, 16, 512], BF16)
        for tb in range(4):
            kb.dma(tlc[:], rr(I['dft_tl_c'][:, tb * 512:(tb + 1) * 512], "(k p) c -> p k c", p=128))
            kb.dma(tls[:], rr(I['dft_tl_s'][:, tb * 512:(tb + 1) * 512], "(k p) c -> p k c", p=128))
            for gi in range(4):
                gs = slice(gi * 128, (gi + 1) * 128)
                ps = nextA(g)
                for tc in range(16):
                    kb.mm(ps[:, :], fc[:, 2 + tc, gs], tlc[:, tc, :], start=(tc == 0), stop=False)
                    kb.mm(ps[:, :], fs[:, 2 + tc, gs], tls[:, tc, :], start=False, stop=(tc == 15))
                kb.copy(mixT[:, 4 + gi, 256 + tb * 512:256 + (tb + 1) * 512], ps[:, :], eng='act', part=True)


def prep_shared(inputs):
    sh = {}
    f = lambda a: np.ascontiguousarray(np.asarray(a, dtype=np.float32))
    for k in ('norm1_w', 'norm2_w', 'ada_w', 'ada_b', 'even_w_in', 'ret_gn_w', 'even_w_out',
              'moe_w_gate', 'moe_w_up', 'moe_w_down', 'final_norm_w'):
        if k in inputs:
            sh[k] = f(inputs[k])
    sh['ret_decay_logit'] = f(inputs['ret_decay_logit']).reshape(1, 8)
    if 'odd_w_in' in inputs:
        sh['odd_w_in'] = f(inputs['odd_w_in'])
        sh['odd_w_out'] = f(inputs['odd_w_out'])
        sh['cw'] = f(np.asarray(inputs['dn_conv_w'])[0].reshape(3, 12, 128).transpose(2, 1, 0))
        sh['dn_a_log'] = f(inputs['dn_a_log']).reshape(1, 8)
        sh['dn_dt_bias'] = f(inputs['dn_dt_bias']).reshape(1, 8)
        sh['dn_norm_w'] = f(inputs['dn_norm_w'])
        sh['wsT'] = f(np.asarray(inputs['sgu_w'])[0].transpose(2, 0, 1))
        sh['sgb'] = f(np.asarray(inputs['sgu_b'])[0].T)
    sh['router_w'] = f(np.concatenate([inputs['router_g_w'], inputs['router_e_w']], -1))
    sh['router_b'] = f(np.concatenate([inputs['router_g_b'], inputs['router_e_b']], -1))
    sh.update(host_consts())
    return sh


def prep_core(inputs, sh, b, used):
    m = {}
    for k in used:
        if k == 'x_all':
            m[k] = np.ascontiguousarray(np.concatenate([inputs['ctx'][b], inputs['x'][b]], 0).astype(np.float32))
        elif k == 'c2t':
            c2 = np.stack([inputs['c'][b], inputs['c_ctx']], 0).astype(np.float32)
            m[k] = np.ascontiguousarray(c2.reshape(2, 8, 128).transpose(2, 0, 1).reshape(128, 16))
        else:
            m[k] = sh[k]
    return m


def ubc(v, axis, shape):
    return View(v.tile, v.ap.unsqueeze(axis).to_broadcast(list(shape)))


def moe_stage(g, L, last):
    kb = g.kb
    I = g.I
    tiles = list(range(NTC, NT)) if last else list(range(NT))
    blocks = [(256, 512), (768, 512), (1280, 512), (1792, 512)] if last else \
             [(0, 512), (512, 512), (1024, 512), (1536, 512), (2048, 256)]
    with kb.phase():
        modt = [kb.sb("modm%d" % r, [128, 3072], F32) for r in range(2)]
        compute_mod(g, L, 1, modt)
        h2T = kb.sb("h2T", [128, 8, TT], BF16)
        Gall = kb.sb("Gall", [128, NT, 32], F32)
        acc = {n: kb.sb("acc%d" % n, [128, D], F32) for n in tiles}
        with kb.phase():
            rw = kb.sb("rw", [128, 8, 36], F32)
            kb.dma(rw[:], rr(I['router_w'][L, :, :], "(k p) c -> p k c", p=128))
            rb = kb.sb("rbias", [128, 36], F32)
            bcast_row(kb, rb[:], I['router_b'][L, :])
            xt = [kb.sb("xm%d" % i, [128, D], F32) for i in range(2)]
            tmp = kb.sb("tmpm", [128, D], F32)
            hf = kb.sb("hf", [128, D], F32)
            hb = kb.sb("hbm", [128, D], BF16)
            hTf = kb.sb("hTf", [128, 8, 128], F32)
            ss = kb.sb("ssm", [128, 4], F32)
            lg = kb.sb("lgt", [128, 36], F32)
            s8 = kb.sb("s8", [128, 16], F32)
            oh4 = kb.sb("oh4", [128, 4], F32)
            t48 = kb.sb("t48", [128, 4, 8], F32)
            sel = kb.sb("sel8", [128, 8], F32)
            msk = kb.sb("msk8", [128, 8], F32)
            oh1 = kb.sb("oh1", [128, 8], F32)
            oh2 = kb.sb("oh2", [128, 8], F32)
            G8 = kb.sb("G8", [128, 8], F32)
            e4 = kb.sb("e4", [128, 4], F32)
            for n in tiles:
                x_ = xt[n % 2]
                kb.dma(x_[:], g.X[n * 128:(n + 1) * 128, :])
                norm_mod_tile(g, x_[:], modt[0 if n >= NTC else 1], hf[:], ss, tmp)
                kb.copy(hb[:], hf[:], eng='act')
                transpose_into(g, h2T[:, :, n * 128:(n + 1) * 128], hb[:], 8)
                for half in range(2):
                    pt = nextA(g)
                    for k in range(4):
                        kk = half * 4 + k
                        kb.transpose(pt[:, k * 128:(k + 1) * 128], hf[:, kk * 128:(kk + 1) * 128], g.identf[:])
                    kb.copy(hTf[:, half * 4:(half + 1) * 4, :], rr(pt[:, :], "p (k t) -> p k t", k=4), eng='act', part=True)
                ps = nextA(g)
                for k in range(8):
                    kb.mm(ps[:, 0:36], hTf[:, k, :], rw[:, k, :], start=(k == 0), stop=(k == 7))
                kb.tt(lg[:], ps[:, 0:36], rb[:], ALU.add)
                kb.reduce(s8[:, 0:1], lg[:, 0:4], ALU.max)
                kb.ts(oh4[:], lg[:, 0:4], s8[:, 0:1], None, ALU.is_ge)
                kb.ts(s8[:, 1:2], s8[:, 0:1], -1.0, None, ALU.mult)
                kb.memset(s8[:, 2:3], 0.0)
                kb.act(e4[:], lg[:, 0:4], AF.Exp, bias=s8[:, 1:2], accum=s8[:, 2:3])
                kb.recip(s8[:, 3:4], s8[:, 2:3])
                kb.tt(t48[:], rr(lg[:, 4:36], "p (g e) -> p g e", g=4), ubc(oh4[:], 2, [128, 4, 8]), ALU.mult)
                kb.reduce(sel[:], rr(t48[:], "p g e -> p e g"), ALU.add)
                kb.reduce(s8[:, 4:5], sel[:], ALU.max)
                kb.ts(oh1[:], sel[:], s8[:, 4:5], None, ALU.is_ge)
                kb.stt(msk[:], oh1[:], -1e30, sel[:], ALU.mult, ALU.add)
                kb.reduce(s8[:, 5:6], msk[:], ALU.max)
                kb.ts(oh2[:], msk[:], s8[:, 5:6], None, ALU.is_ge)
                kb.tt(s8[:, 6:7], s8[:, 5:6], s8[:, 4:5], ALU.subtract)
                kb.act(s8[:, 7:8], s8[:, 6:7], AF.Exp)
                kb.ts(s8[:, 8:9], s8[:, 7:8], 1.0, None, ALU.add)
                kb.recip(s8[:, 9:10], s8[:, 8:9])
                kb.tt(s8[:, 10:11], s8[:, 9:10], s8[:, 3:4], ALU.mult)
                kb.tt(s8[:, 11:12], s8[:, 10:11], s8[:, 7:8], ALU.mult)
                kb.ts(G8[:], oh1[:], s8[:, 10:11], None, ALU.mult)
                kb.stt(G8[:], oh2[:], s8[:, 11:12], G8[:], ALU.mult, ALU.add)
                kb.tt(rr(Gall[:, n, :], "p (g e) -> p g e", g=4), ubc(G8[:], 1, [128, 4, 8]), ubc(oh4[:], 2, [128, 4, 8]), ALU.mult, part=True)
                kb.memset(acc[n][:], 0.0, eng='pool')
        dump(g, 'Gall', Gall[:, 2, :], [128, 32], F32)
        with kb.phase():
            wg = [kb.sb("wg%d" % i, [128, 8, 512], BF16) for i in range(2)]
            wu = [kb.sb("wu%d" % i, [128, 8, 512], BF16) for i in range(2)]
            wd = [kb.sb("wd%d" % i, [128, 4, D], BF16) for i in range(2)]
            actT = [kb.sb("actT%d" % i, [128, 4, 512], BF16) for i in range(2)]
            sgm = [kb.sb("sgm%d" % i, [128, 512], BF16) for i in range(2)]
            cnt = 0
            for e in range(32):
                wg_, wu_, wd_ = wg[e % 2], wu[e % 2], wd[e % 2]
                kb.dma(wg_[:], rr(I['moe_w_gate'][L, e, :, :], "(k p) c -> p k c", p=128), eng='pool')
                kb.dma(wu_[:], rr(I['moe_w_up'][L, e, :, :], "(k p) c -> p k c", p=128), eng='pool')
                kb.dma(wd_[:], rr(I['moe_w_down'][L, e, :, :], "(k p) c -> p k c", p=128), eng='pool')
                for bi, (t0, tl) in enumerate(blocks):
                    aT = actT[bi % 2]
                    for hc in range(4):
                        hs = slice(hc * 128, (hc + 1) * 128)
                        pg_ = nextA(g)
                        for k in range(8):
                            kb.mm(pg_[:, 0:tl], wg_[:, k, hs], h2T[:, k, t0:t0 + tl], start=(k == 0), stop=(k == 7))
                        pu_ = nextA(g)
                        for k in range(8):
                            kb.mm(pu_[:, 0:tl], wu_[:, k, hs], h2T[:, k, t0:t0 + tl], start=(k == 0), stop=(k == 7))
                        sg_ = sgm[cnt % 2]
                        cnt += 1
                        kb.act(sg_[:, 0:tl], pg_[:, 0:tl], AF.Silu)
                        kb.tt(aT[:, hc, 0:tl], sg_[:, 0:tl], pu_[:, 0:tl], ALU.mult, part=True)
                    for tsi in range(tl // 128):
                        n = t0 // 128 + tsi
                        for dh in range(2):
                            py = nextA(g)
                            for hc in range(4):
                                kb.mm(py[:, :], aT[:, hc, tsi * 128:(tsi + 1) * 128], wd_[:, hc, dh * 512:(dh + 1) * 512], start=(hc == 0), stop=(hc == 3))
                            a_ = acc[n][:, dh * 512:(dh + 1) * 512]
                            kb.stt(a_, py[:, :], Gall[:, n, e:e + 1], a_, ALU.mult, ALU.add)
        with kb.phase():
            xt = [kb.sb("xf%d" % i, [128, D], F32) for i in range(2)]
            tmp = kb.sb("tmpf", [128, D], F32)
            ss = kb.sb("ssf", [128, 4], F32)
            if last:
                fnw = kb.sb("fnw", [128, D], F32)
                bcast_row(kb, fnw[:], I['final_norm_w'][:])
            for n in tiles:
                x_ = xt[n % 2]
                kb.dma(x_[:], g.X[n * 128:(n + 1) * 128, :])
                mr = modt[0 if n >= NTC else 1]
                kb.tt(tmp[:], acc[n][:], mr[:, 2048:3072], ALU.mult)
                kb.tt(x_[:], x_[:], tmp[:], ALU.add)
                if not last:
                    kb.dma(g.X[n * 128:(n + 1) * 128, :], x_[:])
                else:
                    kb.memset(ss[:, 0:1], 0.0)
                    kb.act(tmp[:], x_[:], AF.Square, accum=ss[:, 0:1])
                    kb.ts(ss[:, 1:2], ss[:, 0:1], 1.0 / D, EPS, ALU.mult, ALU.add)
                    kb.act(ss[:, 2:3], ss[:, 1:2], AF.Sqrt)
                    kb.recip(ss[:, 3:4], ss[:, 2:3])
                    kb.stt(x_[:], x_[:], ss[:, 3:4], fnw[:], ALU.mult, ALU.mult)
                    kb.dma(g.out[(n - NTC) * 128:(n - NTC + 1) * 128, :], x_[:], is_output=True)


IN_SPECS.update({
    'odd_w_in': ([1, D, 3088], F32), 'cw': ([128, 12, 3], F32), 'dn_a_log': ([1, 8], F32), 'dn_dt_bias': ([1, 8], F32),
    'dn_norm_w': ([1, 128], F32), 'wsT': ([128, 4, 128], F32), 'sgb': ([128, 4], F32), 'odd_w_out': ([1, D, D], F32),
})
CONST_SPECS.update({'lowS': ([128, 128], F32), 'upS': ([128, 128], F32), 'onesf': ([128, 128], F32)})


def layer1(g):
    kb = g.kb
    I = g.I
    L = 1
    with kb.phase():
        MIXs = kb.dram("MIXs1", [8, 128, TT], BF16)
        g1k = kb.sb("g1k", [128, D], F32)
        SGs = kb.dram("SGs1", [TT, 512], BF16)
        with kb.phase():
            qT = kb.sb("qT1", [128, 4, TT], BF16)
            kT = kb.sb("kT1", [128, 4, TT], BF16)
            k_tm = kb.sb("k_tm1", [128, NT, 512], BF16)
            v_tm = kb.sb("v_tm1", [128, NT, 512], BF16)
            glog = kb.sb("glog", [128, NT, 8], F32)
            beta = kb.sb("beta", [128, NT, 8], F32)
            hph = kb.phase()
            hph.__enter__()
            hT = kb.sb("hT1", [128, 8, TT], BF16)
            modt = [kb.sb("modt1_%d" % r, [128, 3072], F32) for r in range(2)]
            compute_mod(g, L, 0, modt)
            kb.copy(g1k[:], modt[0][:, 2048:3072], eng='pool')
            with kb.phase():
                xt = [kb.sb("xt1_%d" % i, [128, D], F32) for i in range(2)]
                tmp = [kb.sb("tmp1_%d" % i, [128, D], F32) for i in range(2)]
                hb_l = [kb.sb("hb1_%d" % i, [128, D], BF16) for i in range(2)]
                ss = [kb.sb("ss1_%d" % i, [128, 4], F32) for i in range(2)]
                def _mk(n):
                    def gen(s):
                        x_ = xt[s]
                        kb.dma(x_[:], g.X[n * 128:(n + 1) * 128, :])
                        yield
                        hb = hb_l[s]
                        yield from norm_mod_gen(g, x_[:], modt[0 if n >= NTC else 1], hb[:], ss[s], tmp[s])
                        yield from transpose_gen(g, s, hT[:, :, n * 128:(n + 1) * 128], hb[:], 8)
                        yield
                    return gen
                run_chains([_mk(n) for n in range(NT)], 2)
            with kb.phase():
                wb = [kb.sb("w1b%d" % i, [128, 8, 512], BF16) for i in range(2)]
                cw = kb.sb("cw", [128, 12, 3], F32)
                kb.dma(cw[:], I['cw'][:])
                onesf = kb.sb("onesf", [128, 128], F32)
                kb.dma(onesf[:], I['onesf'][:])
                raw2 = [kb.sb("raw%d" % i, [128, TT], F32) for i in range(2)]
                y2 = [kb.sb("yconv%d" % i, [128, TT], F32) for i in range(2)]
                sq = kb.sb("sqc", [128, TT], F32)
                vb = kb.sb("vbf", [128, TT], BF16)
                for cb in range(3):
                    w = wb[cb % 2]
                    kb.dma(w[:], rr(I['odd_w_in'][0, :, cb * 512:(cb + 1) * 512], "(k p) c -> p k c", p=128), eng='pool')
                    for hh in range(4):
                        ch = cb * 4 + hh
                        raw, y = raw2[ch % 2], y2[ch % 2]
                        for (t0, tl) in TBLK:
                            ps = nextA(g)
                            for k in range(8):
                                kb.mm(ps[:, 0:tl], w[:, k, hh * 128:(hh + 1) * 128], hT[:, k, t0:t0 + tl], start=(k == 0), stop=(k == 7))
                            kb.copy(raw[:, t0:t0 + tl], ps[:, 0:tl], eng='act', part=True)
                        kb.ts(y[:], raw[:], cw[:, ch, 1:2], None, ALU.mult)
                        for (a, b_) in ((1, 256), (257, TT)):
                            kb.stt(y[:, a:b_], raw[:, a - 1:b_ - 1], cw[:, ch, 0:1], y[:, a:b_], ALU.mult, ALU.add)
                        for (a, b_) in ((0, 255), (256, TT - 1)):
                            kb.stt(y[:, a:b_], raw[:, a + 1:b_ + 1], cw[:, ch, 2:3], y[:, a:b_], ALU.mult, ALU.add)
                        kb.act(y[:], y[:], AF.Silu)
                        if cb < 2:
                            kb.tt(sq[:], y[:], y[:], ALU.mult)
                            for (t0, tl) in TBLK:
                                ps = nextA(g)
                                kb.mm(ps[:, 0:tl], onesf[:], sq[:, t0:t0 + tl], start=True, stop=True)
                                kb.ts(raw[:, t0:t0 + tl], ps[:, 0:tl], EPS, None, ALU.add, part=True)
                            kb.act(raw[:], raw[:], AF.Sqrt)
                            kb.recip(raw[:], raw[:])
                            dstT = qT if cb == 0 else kT
                            if cb == 0:
                                kb.stt(dstT[:, hh, :], y[:], SCALE, raw[:], ALU.mult, ALU.mult, part=True)
                            else:
                                kb.tt(dstT[:, hh, :], y[:], raw[:], ALU.mult, part=True)
                        else:
                            kb.copy(vb[:], y[:], eng='act')
                        if cb >= 1:
                            srcT = kT[:, hh, :] if cb == 1 else vb[:]
                            dst_tm = k_tm if cb == 1 else v_tm
                            for n0 in range(0, NT, 6):
                                pt = nextT(g)
                                for q in range(6):
                                    n = n0 + q
                                    kb.transpose(pt[:, q * 128:(q + 1) * 128], View(srcT.tile, srcT.ap[:, n * 128:(n + 1) * 128]), g.ident[:])
                                kb.copy(dst_tm[:, n0:n0 + 6, hh * 128:(hh + 1) * 128], rr(pt[:, 0:768], "p (n e) -> p n e", n=6), eng='dve', part=True)
            with kb.phase():
                wz = kb.sb("wz", [128, 8, 512], BF16)
                wab = kb.sb("wab", [128, 8, 16], BF16)
                wu = kb.sb("wu1", [128, 8, 512], BF16)
                wsx = kb.sb("ws1", [128, 8, 512], BF16)
                kb.dma(wz[:], rr(I['odd_w_in'][0, :, 1536:2048], "(k p) c -> p k c", p=128), eng='pool')
                kb.dma(wab[:], rr(I['odd_w_in'][0, :, 2048:2064], "(k p) c -> p k c", p=128), eng='pool')
                kb.dma(wu[:], rr(I['odd_w_in'][0, :, 2064:2576], "(k p) c -> p k c", p=128), eng='pool')
                kb.dma(wsx[:], rr(I['odd_w_in'][0, :, 2576:3088], "(k p) c -> p k c", p=128), eng='pool')
                alog = kb.sb("alog", [128, 8], F32)
                dtb = kb.sb("dtb", [128, 8], F32)
                bcast_row(kb, alog[:], I['dn_a_log'][0, :])
                bcast_row(kb, dtb[:], I['dn_dt_bias'][0, :])
                kb.act(alog[:], alog[:], AF.Exp)
                kb.ts(alog[:], alog[:], -1.0, None, ALU.mult)
                wsT = kb.sb("wsT", [128, 4, 128], F32)
                wsTb = kb.sb("wsTb", [128, 4, 128], BF16)
                kb.dma(wsT[:], I['wsT'][:])
                kb.copy(wsTb[:], wsT[:], eng='act')
                sgb = kb.sb("sgb", [128, 4], F32)
                kb.dma(sgb[:], I['sgb'][:])
                sgt = [kb.sb("sgt1_%d" % i, [128, 512], BF16) for i in range(2)]
                ab_l = [kb.sb("ab%d" % i, [128, 16], F32) for i in range(2)]
                ug_l = [kb.sb("ug%d" % i, [128, 4, 128], F32) for i in range(2)]
                sgl_l = [kb.sb("sgl1_%d" % i, [128, 4, 128], F32) for i in range(2)]
                sq2_l = [kb.sb("sq2_%d" % i, [128, 4, 128], F32) for i in range(2)]
                vn_l = [kb.sb("vn%d" % i, [128, 4, 128], BF16) for i in range(2)]
                st_l = [kb.sb("sgu_st%d" % i, [128, 6, 4], F32) for i in range(2)]
                ob_l = [kb.sb("sgu_ob%d" % i, [128, 512], BF16) for i in range(2)]
                sgo = [kb.sb("sgo%d" % i, [128, 4, 128], BF16) for i in range(2)]
                def _mk(n):
                    def gen(s):
                        tsl = slice(n * 128, (n + 1) * 128)
                        ab, ug, sgl, sq2, vn, st_, ob = ab_l[s], ug_l[s], sgl_l[s], sq2_l[s], vn_l[s], st_l[s], ob_l[s]
                        ps = nextA_s(g, s)
                        for k in range(8):
                            kb.mm(ps[:, 0:16], hT[:, k, tsl], wab[:, k, :], start=(k == 0), stop=(k == 7))
                        kb.copy(ab[:], ps[:, 0:16], eng='act')
                        yield
                        kb.act(beta[:, n, :], ab[:, 8:16], AF.Sigmoid, part=True)
                        yield
                        kb.tt(ab[:, 0:8], ab[:, 0:8], dtb[:], ALU.add)
                        yield
                        kb.act(ab[:, 0:8], ab[:, 0:8], AF.Exp)
                        yield
                        kb.act(ab[:, 0:8], ab[:, 0:8], AF.Ln, bias=1.0)
                        yield
                        kb.tt(glog[:, n, :], ab[:, 0:8], alog[:], ALU.mult, part=True)
                        yield
                        if n < NTC:
                            return
                        ps = nextA_s(g, s)
                        for k in range(8):
                            kb.mm(ps[:, :], hT[:, k, tsl], wz[:, k, :], start=(k == 0), stop=(k == 7))
                        kb.act(sgt[s][:], ps[:, :], AF.Silu)
                        yield
                        kb.dma(SGs[tsl, :], sgt[s][:], part=True)
                        yield
                        pu = nextA_s(g, s)
                        for k in range(8):
                            kb.mm(pu[:, :], hT[:, k, tsl], wu[:, k, :], start=(k == 0), stop=(k == 7))
                        kb.act(rr(ug[:], "p g c -> p (g c)"), pu[:, :], AF.Gelu)
                        yield
                        pv_ = nextA_s(g, s)
                        for k in range(8):
                            kb.mm(pv_[:, :], hT[:, k, tsl], wsx[:, k, :], start=(k == 0), stop=(k == 7))
                        kb.act(rr(sgl[:], "p g c -> p (g c)"), pv_[:, :], AF.Gelu)
                        yield
                        kb.reduce(st_[:, 0, :], sgl[:], ALU.add)
                        yield
                        kb.tt(sq2[:], sgl[:], sgl[:], ALU.mult)
                        yield
                        kb.reduce(st_[:, 1, :], sq2[:], ALU.add)
                        yield
                        kb.ts(st_[:, 2, :], st_[:, 0, :], 1.0 / 128, None, ALU.mult)
                        yield
                        kb.tt(st_[:, 3, :], st_[:, 2, :], st_[:, 2, :], ALU.mult)
                        yield
                        kb.stt(st_[:, 4, :], st_[:, 1, :], 1.0 / 128, st_[:, 3, :], ALU.mult, ALU.subtract)
                        yield
                        kb.ts(st_[:, 4, :], st_[:, 4, :], EPS, None, ALU.add)
                        yield
                        kb.act(st_[:, 5, :], st_[:, 4, :], AF.Sqrt)
                        yield
                        kb.recip(st_[:, 3, :], st_[:, 5, :])
                        yield
                        for gi in range(4):
                            kb.ts(vn[:, gi, :], sgl[:, gi, :], st_[:, 2, gi:gi + 1], st_[:, 3, gi:gi + 1], ALU.subtract, ALU.mult, part=True)
                            yield
                        pm = nextA_s(g, s)
                        for gi in range(4):
                            kb.mm(pm[:, gi * 128:(gi + 1) * 128], wsTb[:, gi, :], vn[:, gi, :], start=True, stop=True)
                        for gi in range(4):
                            kb.stt(ob[:, gi * 128:(gi + 1) * 128], pm[:, gi * 128:(gi + 1) * 128], sgb[:, gi:gi + 1], ug[:, gi, :], ALU.add, ALU.mult, part=True)
                            yield
                        so_ = sgo[s]
                        yield from transpose_gen(g, s, so_[:], ob[:], 4, eng='act')
                        kb.dma(rr(MIXs[4:8, :, tsl], "k c t -> c k t"), so_[:], part=True)
                        yield
                        yield
                    return gen
                run_chains([_mk(n) for n in range(NT)], 2)
            hph.__exit__(None, None, None)
            deltanet2(g, qT, kT, k_tm, v_tm, glog, beta, SGs, MIXs)
        with kb.phase():
            wo = kb.sb("wo1", [128, 8, D], BF16)
            kb.dma(wo[:], rr(I['odd_w_out'][0, :, :], "(k p) c -> p k c", p=128), eng='pool')
            xt = [kb.sb("xt14_%d" % i, [128, D], F32) for i in range(2)]
            yt_l = [kb.sb("yt14_%d" % i, [128, D], F32) for i in range(2)]
            mxl = [kb.sb("mxl%d" % i, [128, 8, 128], BF16) for i in range(2)]
            def _mk(n):
                def gen(s):
                    tsl = slice(n * 128, (n + 1) * 128)
                    x_ = xt[s]
                    kb.dma(x_[:], g.X[tsl, :])
                    yield
                    mx = mxl[s]
                    kb.dma(mx[:], rr(MIXs[:, :, tsl], "k c t -> c k t"))
                    yield
                    for dh in range(2):
                        ps = nextA_s(g, s)
                        for k in range(8):
                            kb.mm(ps[:, :], mx[:, k, :], wo[:, k, dh * 512:(dh + 1) * 512], start=(k == 0), stop=(k == 7))
                        kb.tt(yt_l[s][:, dh * 512:(dh + 1) * 512], ps[:, :], g1k[:, dh * 512:(dh + 1) * 512], ALU.mult, part=True)
                        yield
                    kb.tt(x_[:], x_[:], yt_l[s][:], ALU.add)
                    yield
                    kb.dma(g.X[tsl, :], x_[:])
                    yield
                    yield
                return gen
            run_chains([_mk(n) for n in range(NTC, NT)], 2)


def deltanet(g, qT, kT, k_tm, v_tm, glog, beta, SGs, mixT):
    kb = g.kb
    I = g.I
    with kb.phase():
        cm = {nm: kb.sb("dc_" + nm, [128, 128], F32) for nm in ('indf', 'indb', 'lowS', 'upS')}
        for nm in cm:
            kb.dma(cm[nm][:], I[nm][:])
        identf = g.identf
        o_acc = kb.sb("o_acc", [128, NTL, 512], F32)
        F = lambda nm: kb.sb(nm, [128, 128], F32)
        B = lambda nm: kb.sb(nm, [128, 128], BF16)
        grep, grow, t1, dec, decS, Lm, Nm, decT = F("grep"), F("grow"), F("t1"), F("dec"), F("decS"), F("Lm"), F("Nm"), F("decT")
        X = [F("X0"), F("X1")]
        Y = [F("Y0"), F("Y1")]
        P = [F("P0"), F("P1")]
        PT = [F("PT0"), F("PT1")]
        TTb, rhsV, rhsK, wTb, vnew, attnT, ktail, Sb = B("TTb"), B("rhsV"), B("rhsK"), B("wTb"), B("vnew"), B("attnT"), B("ktail"), B("Sb")
        u_sb, otmp = F("u_sb"), F("otmp")
        gcol = kb.sb("gcol", [128, 8], F32)
        eg = kb.sb("eg", [128, 8], F32)
        sc = kb.sb("dsc", [128, 8], F32)
        S = [[kb.sb("S%d_%d" % (d, h), [128, 128], F32) for h in range(4)] for d in range(2)]
        for d in range(2):
            order = list(range(NT)) if d == 0 else [1, 0] + list(range(NT - 1, 1, -1))
            mI = cm['indb'] if d == 0 else cm['indf']
            mS = cm['lowS'] if d == 0 else cm['upS']
            U = cm['indf'] if d == 0 else cm['indb']
            li = 127 if d == 0 else 0
            for h in range(4):
                kb.memset(S[d][h][:], 0.0)
            for idx, n in enumerate(order):
                tsl = slice(n * 128, (n + 1) * 128)
                is_out = n >= NTC
                last_chunk = idx == NT - 1
                pg = nextA(g)
                kb.mm(pg[:, 0:4], U[:], glog[:, n, d * 4:(d + 1) * 4], start=True, stop=True)
                kb.copy(gcol[:, 0:4], pg[:, 0:4], eng='act')
                kb.act(eg[:, 0:4], gcol[:, 0:4], AF.Exp)
                for h in range(4):
                    c = d * 4 + h
                    hs = slice(h * 128, (h + 1) * 128)
                    kb.copy(grep[:], bc(glog[:, n, c:c + 1], [128, 128]))
                    p1 = nextA(g)
                    kb.mm(p1[:, 0:128], grep[:], U[:], start=True, stop=True)
                    kb.copy(grow[:], p1[:, 0:128], eng='act')
                    kb.ts(t1[:], grow[:], gcol[:, h:h + 1], 0.0, ALU.subtract, ALU.max)
                    kb.act(t1[:], t1[:], AF.Exp, scale=-1.0)
                    kb.tt(dec[:], t1[:], mI[:], ALU.mult, eng='pool')
                    kb.tt(decS[:], t1[:], mS[:], ALU.mult)
                    p2 = nextA(g)
                    kb.mm(p2[:, 0:128], kT[:, h, tsl], kT[:, h, tsl], start=True, stop=True)
                    kb.stt(Lm[:], p2[:, 0:128], beta[:, n, c:c + 1], decS[:], ALU.mult, ALU.mult)
                    p3 = nextA(g)
                    kb.transpose(p3[:, 0:128], Lm[:], identf[:])
                    kb.transpose(p3[:, 128:256], dec[:], identf[:])
                    kb.copy(Nm[:], p3[:, 0:128], eng='act')
                    kb.copy(decT[:], p3[:, 128:256], eng='act')
                    kb.tt(P[0][:], identf[:], Lm[:], ALU.subtract)
                    kb.tt(PT[0][:], identf[:], Nm[:], ALU.subtract, eng='pool')
                    Xc, Yc = Lm, Nm
                    pi = 0
                    for m in range(6):
                        lastm = m == 5
                        pa = nextA(g)
                        kb.mm(pa[:, 0:128], Xc[:], Yc[:], start=True, stop=True)
                        if not lastm:
                            kb.mm(pa[:, 128:256], Yc[:], Xc[:], start=True, stop=True)
                        Yn, Xn = Y[m % 2], X[m % 2]
                        kb.copy(Yn[:], pa[:, 0:128], eng='act')
                        if not lastm:
                            kb.copy(Xn[:], pa[:, 128:256], eng='act')
                        pb_ = nextA(g)
                        kb.mm(pb_[:, 0:128], P[pi][:], Yn[:], start=True, stop=True)
                        if not lastm:
                            kb.mm(pb_[:, 128:256], PT[pi][:], Xn[:], start=True, stop=True)
                        kb.tt(PT[1 - pi][:], PT[pi][:], pb_[:, 0:128], ALU.add)
                        if not lastm:
                            kb.tt(P[1 - pi][:], P[pi][:], pb_[:, 128:256], ALU.add)
                        pi = 1 - pi
                        Xc, Yc = Xn, Yn
                    kb.copy(TTb[:], PT[pi][:], eng='act')
                    kb.ts(rhsV[:], v_tm[:, n, hs], beta[:, n, c:c + 1], None, ALU.mult)
                    kb.tt(sc[:, 0:1], beta[:, n, c:c + 1], eg[:, h:h + 1], ALU.mult)
                    kb.ts(rhsK[:], k_tm[:, n, hs], sc[:, 0:1], None, ALU.mult)
                    p4 = nextA(g)
                    kb.mm(p4[:, 0:128], TTb[:], rhsV[:], start=True, stop=True)
                    kb.mm(p4[:, 128:256], rhsK[:], TTb[:], start=True, stop=True)
                    kb.copy(u_sb[:], p4[:, 0:128], eng='act')
                    kb.copy(wTb[:], p4[:, 128:256], eng='act')
                    kb.copy(Sb[:], S[d][h][:], eng='act')
                    p5 = nextA(g)
                    kb.mm(p5[:, 0:128], wTb[:], Sb[:], start=True, stop=True)
                    kb.tt(vnew[:], u_sb[:], p5[:, 0:128], ALU.subtract)
                    if is_out:
                        p6 = nextA(g)
                        kb.mm(p6[:, 0:128], kT[:, h, tsl], qT[:, h, tsl], start=True, stop=True)
                        kb.tt(attnT[:], p6[:, 0:128], decT[:], ALU.mult)
                        p7 = nextA(g)
                        kb.mm(p7[:, 0:128], attnT[:], vnew[:], start=True, stop=True)
                        kb.mm(p7[:, 128:256], qT[:, h, tsl], Sb[:], start=True, stop=True)
                        kb.copy(otmp[:], p7[:, 0:128], eng='act')
                        kb.stt(otmp[:], p7[:, 128:256], eg[:, h:h + 1], otmp[:], ALU.mult, ALU.add)
                        oa = o_acc[:, n - NTC, hs]
                        if d == 0:
                            kb.copy(oa, otmp[:], eng='pool', part=True)
                        else:
                            kb.tt(oa, oa, otmp[:], ALU.add, eng='pool')
                    if not last_chunk:
                        kb.act(sc[:, 1:2], gcol[:, h:h + 1], AF.Exp, scale=-1.0, bias=grow[:, li:li + 1])
                        kb.ts(ktail[:], k_tm[:, n, hs], sc[:, 1:2], None, ALU.mult)
                        kb.act(sc[:, 2:3], grow[:, li:li + 1], AF.Exp)
                        p8 = nextA(g)
                        kb.mm(p8[:, 0:128], ktail[:], vnew[:], start=True, stop=True)
                        kb.stt(S[d][h][:], S[d][h][:], sc[:, 2:3], p8[:, 0:128], ALU.mult, ALU.add)
        nwb = kb.sb("dnw", [128, 128], F32)
        bcast_row(kb, nwb[:], I['dn_norm_w'][0, :])
        sq = kb.sb("dsq", [128, 4, 128], F32)
        st_ = kb.sb("dst", [128, 3, 4], F32)
        yb = kb.sb("dyb", [128, 512], BF16)
        zl = [kb.sb("zl%d" % i, [128, 512], BF16) for i in range(2)]
        for nl in range(NTL):
            n = nl + NTC
            tsl = slice(n * 128, (n + 1) * 128)
            kb.dma(zl[nl % 2][:], SGs[tsl, :])
            o3 = rr(o_acc[:, nl, :], "p (h e) -> p h e", h=4)
            kb.tt(sq[:], o3, o3, ALU.mult)
            kb.reduce(st_[:, 0, :], sq[:], ALU.add)
            kb.ts(st_[:, 1, :], st_[:, 0, :], 1.0 / 128, EPS, ALU.mult, ALU.add)
            kb.act(st_[:, 2, :], st_[:, 1, :], AF.Sqrt)
            kb.recip(st_[:, 1, :], st_[:, 2, :])
            for h in range(4):
                kb.stt(sq[:, h, :], View(o_acc, o3.ap[:, h, :]), st_[:, 1, h:h + 1], nwb[:], ALU.mult, ALU.mult, part=True)
            kb.tt(yb[:], rr(sq[:], "p h e -> p (h e)"), zl[nl % 2][:], ALU.mult)
            transpose_into(g, mixT[:, 0:4, tsl], yb[:], 4, eng='act')


def deltanet2(g, qT, kT, k_tm, v_tm, glog, beta, SGs, MIXs):
    kb = g.kb
    I = g.I
    with kb.phase():
        cm = {nm: kb.sb("dc_" + nm, [128, 128], F32) for nm in ('indf', 'indb', 'lowS', 'upS')}
        for nm in cm:
            kb.dma(cm[nm][:], I[nm][:])
        identf = g.identf
        o_acc = [kb.sb("o_acc%d" % i, [128, 512], F32) for i in range(NTL)]
        for i in range(NTL):
            kb.memset(o_acc[i][:], 0.0, eng='pool')

        def chain(d, h):
            c = d * 4 + h
            nm = "c%d_" % c
            F = lambda s: kb.sb(nm + s, [128, 128], F32)
            B = lambda s: kb.sb(nm + s, [128, 128], BF16)
            grep, grow, t1, dec, decS, Lm, Nm, decT = F("grep"), F("grow"), F("t1"), F("dec"), F("decS"), F("Lm"), F("Nm"), F("decT")
            X = [F("X0"), F("X1")]
            Y = [F("Y0"), F("Y1")]
            P = [F("P0"), F("P1")]
            PT = [F("PT0"), F("PT1")]
            TTb, rhsV, rhsK, wTb, vnew, attnT, ktail, Sb = B("TTb"), B("rhsV"), B("rhsK"), B("wTb"), B("vnew"), B("attnT"), B("ktail"), B("Sb")
            u_sb, otmp = t1, grep
            sc = kb.sb(nm + "sc", [128, 8], F32)
            S = F("S")
            order = list(range(NT)) if d == 0 else [1, 0] + list(range(NT - 1, 1, -1))
            mI = cm['indb'] if d == 0 else cm['indf']
            mS = cm['lowS'] if d == 0 else cm['upS']
            U = cm['indf'] if d == 0 else cm['indb']
            li = 127 if d == 0 else 0
            hs = slice(h * 128, (h + 1) * 128)
            bank = g.pA[c] if c < 6 else F32Bank(g.pT[c - 6])
            kb.memset(S[:], 0.0)
            yield
            for idx, n in enumerate(order):
                tsl = slice(n * 128, (n + 1) * 128)
                is_out = n >= NTC
                last_chunk = idx == NT - 1
                bcol = beta[:, n, c:c + 1]
                pg = bank
                kb.mm(pg[:, 0:1], U[:], glog[:, n, c:c + 1], start=True, stop=True)
                kb.copy(grep[:], bc(glog[:, n, c:c + 1], [128, 128]))
                kb.mm(pg[:, 128:256], grep[:], U[:], start=True, stop=True)
                yield
                kb.copy(sc[:, 0:1], pg[:, 0:1], eng='act')
                kb.copy(grow[:], pg[:, 128:256], eng='act')
                yield
                kb.act(sc[:, 1:2], sc[:, 0:1], AF.Exp)
                kb.ts(t1[:], grow[:], sc[:, 0:1], 0.0, ALU.subtract, ALU.max)
                yield
                kb.act(t1[:], t1[:], AF.Exp, scale=-1.0)
                yield
                kb.tt(dec[:], t1[:], mI[:], ALU.mult, eng='pool')
                kb.tt(decS[:], t1[:], mS[:], ALU.mult)
                p2 = bank
                kb.mm(p2[:, 0:128], kT[:, h, tsl], kT[:, h, tsl], start=True, stop=True)
                yield
                kb.stt(Lm[:], p2[:, 0:128], bcol, decS[:], ALU.mult, ALU.mult)
                yield
                p3 = bank
                kb.transpose(p3[:, 0:128], Lm[:], identf[:])
                kb.transpose(p3[:, 128:256], dec[:], identf[:])
                yield
                kb.copy(Nm[:], p3[:, 0:128], eng='act')
                kb.copy(decT[:], p3[:, 128:256], eng='act')
                kb.tt(P[0][:], identf[:], Lm[:], ALU.subtract)
                yield
                kb.tt(PT[0][:], identf[:], Nm[:], ALU.subtract, eng='pool')
                Xc, Yc = Lm, Nm
                pi = 0
                for m in range(6):
                    lastm = m == 5
                    pa = bank
                    kb.mm(pa[:, 0:128], Xc[:], Yc[:], start=True, stop=True)
                    if not lastm:
                        kb.mm(pa[:, 128:256], Yc[:], Xc[:], start=True, stop=True)
                    yield
                    Yn, Xn = Y[m % 2], X[m % 2]
                    kb.copy(Yn[:], pa[:, 0:128], eng='act')
                    if not lastm:
                        kb.copy(Xn[:], pa[:, 128:256], eng='dve')
                    yield
                    pb_ = bank
                    kb.mm(pb_[:, 0:128], P[pi][:], Yn[:], start=True, stop=True)
                    if not lastm:
                        kb.mm(pb_[:, 128:256], PT[pi][:], Xn[:], start=True, stop=True)
                    yield
                    kb.tt(PT[1 - pi][:], PT[pi][:], pb_[:, 0:128], ALU.add)
                    if not lastm:
                        kb.tt(P[1 - pi][:], P[pi][:], pb_[:, 128:256], ALU.add)
                    yield
                    pi = 1 - pi
                    Xc, Yc = Xn, Yn
                kb.copy(TTb[:], PT[pi][:], eng='act')
                kb.ts(rhsV[:], v_tm[:, n, hs], bcol, None, ALU.mult, eng='pool')
                kb.tt(sc[:, 2:3], bcol, sc[:, 1:2], ALU.mult)
                yield
                kb.ts(rhsK[:], k_tm[:, n, hs], sc[:, 2:3], None, ALU.mult)
                kb.copy(Sb[:], S[:], eng='act')
                yield
                p4 = bank
                kb.mm(p4[:, 0:128], TTb[:], rhsV[:], start=True, stop=True)
                kb.mm(p4[:, 128:256], rhsK[:], TTb[:], start=True, stop=True)
                yield
                kb.copy(u_sb[:], p4[:, 0:128], eng='act')
                kb.copy(wTb[:], p4[:, 128:256], eng='dve')
                yield
                p5 = bank
                kb.mm(p5[:, 0:128], wTb[:], Sb[:], start=True, stop=True)
                yield
                kb.tt(vnew[:], u_sb[:], p5[:, 0:128], ALU.subtract)
                yield
                if is_out:
                    p6 = bank
                    kb.mm(p6[:, 0:128], kT[:, h, tsl], qT[:, h, tsl], start=True, stop=True)
                    yield
                    kb.tt(attnT[:], p6[:, 0:128], decT[:], ALU.mult)
                    yield
                    p7 = bank
                    kb.mm(p7[:, 0:128], attnT[:], vnew[:], start=True, stop=True)
                    kb.mm(p7[:, 128:256], qT[:, h, tsl], Sb[:], start=True, stop=True)
                    yield
                    kb.copy(otmp[:], p7[:, 0:128], eng='act')
                    yield
                    kb.stt(otmp[:], p7[:, 128:256], sc[:, 1:2], otmp[:], ALU.mult, ALU.add)
                    yield
                    oa = o_acc[n - NTC][:, hs]
                    kb.tt(oa, oa, otmp[:], ALU.add, eng='pool')
                    yield
                if not last_chunk:
                    kb.act(sc[:, 3:4], sc[:, 0:1], AF.Exp, scale=-1.0, bias=grow[:, li:li + 1])
                    kb.act(sc[:, 4:5], grow[:, li:li + 1], AF.Exp)
                    yield
                    kb.ts(ktail[:], k_tm[:, n, hs], sc[:, 3:4], None, ALU.mult)
                    yield
                    p8 = bank
                    kb.mm(p8[:, 0:128], ktail[:], vnew[:], start=True, stop=True)
                    yield
                    kb.stt(S[:], S[:], sc[:, 4:5], p8[:, 0:128], ALU.mult, ALU.add)
                    yield

        gens = [chain(d, h) for d in range(2) for h in range(4)]
        while gens:
            for gen in list(gens):
                try:
                    next(gen)
                except StopIteration:
                    gens.remove(gen)
        nwb = kb.sb("dnw", [128, 128], F32)
        bcast_row(kb, nwb[:], I['dn_norm_w'][0, :])
        sq = kb.sb("dsq", [128, 4, 128], F32)
        st_ = kb.sb("dst", [128, 3, 4], F32)
        yb = kb.sb("dyb", [128, 512], BF16)
        zl = [kb.sb("zl%d" % i, [128, 512], BF16) for i in range(2)]
        yo = [kb.sb("dyo%d" % i, [128, 4, 128], BF16) for i in range(2)]
        for nl in range(NTL):
            n = nl + NTC
            tsl = slice(n * 128, (n + 1) * 128)
            kb.dma(zl[nl % 2][:], SGs[tsl, :])
            o3 = rr(o_acc[nl][:], "p (h e) -> p h e", h=4)
            kb.tt(sq[:], o3, o3, ALU.mult)
            kb.reduce(st_[:, 0, :], sq[:], ALU.add)
            kb.ts(st_[:, 1, :], st_[:, 0, :], 1.0 / 128, EPS, ALU.mult, ALU.add)
            kb.act(st_[:, 2, :], st_[:, 1, :], AF.Sqrt)
            kb.recip(st_[:, 1, :], st_[:, 2, :])
            for h in range(4):
                kb.stt(sq[:, h, :], View(o_acc[nl], o3.ap[:, h, :]), st_[:, 1, h:h + 1], nwb[:], ALU.mult, ALU.mult, part=True)
            kb.tt(yb[:], rr(sq[:], "p h e -> p (h e)"), zl[nl % 2][:], ALU.mult)
            transpose_into(g, yo[nl % 2][:], yb[:], 4, eng='act')
            kb.dma(rr(MIXs[0:4, :, tsl], "k c t -> c k t"), yo[nl % 2][:], part=True)


class F32Bank:
    def __init__(self, tile):
        self.tile = tile

    def __getitem__(self, idx):
        return View(self.tile, self.tile.h[:, :].bitcast(F32)[idx])


CONST_SPECS.update({'thr': ([128, 18], F32), 'bvals': ([128, 50], F32), 'pcol': ([128, 1], F32)})


def moe_sparse(g, L, last):
    kb = g.kb
    I = g.I
    tiles = list(range(NTC, NT)) if last else list(range(NT))
    NBLK = len(tiles) + 32
    BR = 256
    with kb.phase():
        modt = [kb.sb("modm%d" % r, [128, 3072], F32) for r in range(2)]
        compute_mod(g, L, 1, modt)
        h2all = kb.sb("h2all", [128, NT, D], BF16)
        Gall = kb.sb("Gall", [128, NT, 32], F32)
        kb.memset(Gall[:], 0.0, eng='pool')
        idx_i = kb.sb("idx_i", [128, 2, NT], I32)
        gts = kb.sb("gts", [128, 2, NT], F32)
        widx_i = kb.sb("widx_i", [128, NBLK], I32)
        ROWS = kb.dram("ROWS%d" % L, [NBLK * BR, D], BF16)
        YROWS = kb.dram("YROWS%d" % L, [NBLK * BR, D], F32)
        with kb.phase():
            rw = kb.sb("rw", [128, 8, 36], F32)
            kb.dma(rw[:], rr(I['router_w'][L, :, :], "(k p) c -> p k c", p=128))
            rb = kb.sb("rbias", [128, 36], F32)
            bcast_row(kb, rb[:], I['router_b'][L, :])
            xt = [kb.sb("xm%d" % i, [128, D], F32) for i in range(2)]
            tmp = [kb.sb("tmpm%d" % i, [128, D], F32) for i in range(2)]
            hf_l = [kb.sb("hf%d" % i, [128, D], F32) for i in range(2)]
            hTf = kb.sb("hTf", [128, 8, 128], F32)
            ss = [kb.sb("ssm%d" % i, [128, 4], F32) for i in range(2)]
            hTf_l = [kb.sb("hTf%d" % i, [128, 8, 128], F32) for i in range(2)]
            lgall = kb.sb("lgall", [128, NT, 36], F32)
            kb.memset(lgall[:], 0.0, eng='pool')
            s8 = kb.sb("s8", [128, 16], F32)
            oh4 = kb.sb("oh4", [128, 4], F32)
            t48 = kb.sb("t48", [128, 4, 8], F32)
            sel8 = kb.sb("sel8", [128, 8], F32)
            msk = kb.sb("msk8", [128, 8], F32)
            oh1 = kb.sb("oh1", [128, 8], F32)
            oh2 = kb.sb("oh2", [128, 8], F32)
            G8 = kb.sb("G8", [128, 8], F32)
            e4 = kb.sb("e4", [128, 4], F32)
            def _mk(n):
                def gen(s):
                    x_ = xt[s]
                    kb.dma(x_[:], g.X[n * 128:(n + 1) * 128, :])
                    yield
                    hf, hTf = hf_l[s], hTf_l[s]
                    yield from norm_mod_gen(g, x_[:], modt[0 if n >= NTC else 1], hf[:], ss[s], tmp[s])
                    kb.copy(h2all[:, n, :], hf[:], eng='act', part=True)
                    yield
                    for half in range(2):
                        pt = nextA_s(g, s)
                        for k in range(4):
                            kk = half * 4 + k
                            kb.transpose(pt[:, k * 128:(k + 1) * 128], hf[:, kk * 128:(kk + 1) * 128], g.identf[:])
                        kb.copy(hTf[:, half * 4:(half + 1) * 4, :], rr(pt[:, :], "p (k t) -> p k t", k=4), eng='act', part=True)
                        yield
                    ps = nextA_s(g, s)
                    for k in range(8):
                        kb.mm(ps[:, 0:36], hTf[:, k, :], rw[:, k, :], start=(k == 0), stop=(k == 7))
                    kb.tt(lgall[:, n, :], ps[:, 0:36], rb[:], ALU.add, part=True)
                    yield
                    yield
                return gen
            run_chains([_mk(n) for n in tiles], 2)
            N3 = lambda nm, k: kb.sb(nm, [128, NT, k], F32)
            N2 = lambda nm: kb.sb(nm, [128, NT], F32)
            lg4 = lgall[:, :, 0:4]
            m4, s4, pgr, m1, m2, dm, ee, g1_, g2_ = N2("m4"), N2("s4"), N2("pgr"), N2("m1"), N2("m2"), N2("dm"), N2("ee"), N2("g1_"), N2("g2_")
            oh4, d4, sel8, oh1, oh2, msk, G8 = N3("oh4", 4), N3("d4", 4), N3("sel8", 8), N3("oh1", 8), N3("oh2", 8), N3("msk", 8), N3("G8", 8)
            t48 = kb.sb("t48", [128, NT, 4, 8], F32)
            kb.reduce(m4[:], lg4, ALU.max)
            kb.tt(oh4[:], lg4, ubc(m4[:], 2, [128, NT, 4]), ALU.is_ge)
            kb.tt(d4[:], lg4, ubc(m4[:], 2, [128, NT, 4]), ALU.subtract)
            kb.act(d4[:], d4[:], AF.Exp)
            kb.reduce(s4[:], d4[:], ALU.add)
            kb.recip(pgr[:], s4[:])
            kb.tt(t48[:], rr(lgall[:, :, 4:36], "p n (g e) -> p n g e", g=4), ubc(oh4[:], 3, [128, NT, 4, 8]), ALU.mult)
            kb.reduce(sel8[:], rr(t48[:], "p n g e -> p n e g"), ALU.add)
            kb.reduce(m1[:], sel8[:], ALU.max)
            kb.tt(oh1[:], sel8[:], ubc(m1[:], 2, [128, NT, 8]), ALU.is_ge)
            kb.stt(msk[:], oh1[:], -1e30, sel8[:], ALU.mult, ALU.add)
            kb.reduce(m2[:], msk[:], ALU.max)
            kb.tt(oh2[:], msk[:], ubc(m2[:], 2, [128, NT, 8]), ALU.is_ge)
            kb.tt(dm[:], m2[:], m1[:], ALU.subtract)
            kb.act(ee[:], dm[:], AF.Exp)
            kb.ts(dm[:], ee[:], 1.0, None, ALU.add)
            kb.recip(g1_[:], dm[:])
            kb.tt(g1_[:], g1_[:], pgr[:], ALU.mult)
            kb.tt(g2_[:], g1_[:], ee[:], ALU.mult)
            kb.tt(G8[:], oh1[:], ubc(g1_[:], 2, [128, NT, 8]), ALU.mult)
            kb.tt(msk[:], oh2[:], ubc(g2_[:], 2, [128, NT, 8]), ALU.mult)
            kb.tt(G8[:], G8[:], msk[:], ALU.add)
            kb.tt(rr(Gall[:], "p n (g e) -> p n g e", g=4), ubc(G8[:], 2, [128, NT, 4, 8]), ubc(oh4[:], 3, [128, NT, 4, 8]), ALU.mult)
            if last:
                kb.memset(Gall[:, 0:NTC, :], 0.0)
        with kb.phase():
            cst = {nm: kb.sb("ms_" + nm, shp, F32) for nm, shp in (('upS', [128, 128]), ('onesf', [128, 128]), ('thr', [128, 18]), ('bvals', [128, 50]), ('pcol', [128, 1]))}
            for nm in cst:
                kb.dma(cst[nm][:], I[nm][:])
            A3 = lambda nm: kb.sb(nm, [128, NT, 32], F32)
            sel, rank, dest, mlo, mhi, eq = A3("sel"), A3("rank"), A3("dest"), A3("mlo"), A3("mhi"), A3("eq")
            base = kb.sb("base", [128, 32], F32)
            kb.ts(sel[:], Gall[:], 0.0, None, ALU.is_gt)
            kb.memset(rank[:], 0.0, eng='pool')
            kb.memset(base[:], 0.0)
            for n in tiles:
                ps = nextA(g)
                kb.mm(ps[:, 0:32], cst['upS'][:], sel[:, n, :], start=True, stop=True)
                kb.mm(ps[:, 32:64], cst['onesf'][:], sel[:, n, :], start=True, stop=True)
                kb.tt(rank[:, n, :], ps[:, 0:32], base[:], ALU.add, part=True)
                kb.tt(base[:], base[:], ps[:, 32:64], ALU.add)
            cmp = kb.sb("cmpk", [128, 32, 18], F32)
            nb = kb.sb("nbk", [128, 32], F32)
            kb.tt(cmp[:], ubc(base[:], 2, [128, 32, 18]), ubc(cst['thr'][:], 1, [128, 32, 18]), ALU.is_gt)
            kb.reduce(nb[:], cmp[:], ALU.add)
            cum = [kb.sb("cum%d" % i, [128, 32], F32) for i in range(2)]
            kb.copy(cum[0][:], nb[:])
            ci = 0
            for s in (1, 2, 4, 8, 16):
                kb.copy(cum[1 - ci][:], cum[ci][:])
                kb.tt(cum[1 - ci][:, s:32], cum[ci][:, s:32], cum[ci][:, 0:32 - s], ALU.add)
                ci = 1 - ci
            start = kb.sb("startb", [128, 32], F32)
            st256 = kb.sb("st256", [128, 32], F32)
            kb.tt(start[:], cum[ci][:], nb[:], ALU.subtract)
            kb.ts(st256[:], start[:], float(BR), None, ALU.mult)
            kb.tt(dest[:], rank[:], ubc(st256[:], 1, [128, NT, 32]), ALU.add)
            kb.ts(eq[:], sel[:], -1e9, 1e9, ALU.mult, ALU.add)
            kb.tt(mlo[:], dest[:], eq[:], ALU.add)
            kb.tt(mhi[:], dest[:], sel[:], ALU.mult)
            ixf = kb.sb("ixf", [128, 2, NT], F32)
            kb.reduce(ixf[:, 0, :], mlo[:], ALU.min)
            kb.reduce(ixf[:, 1, :], mhi[:], ALU.max)
            kb.copy(idx_i[:], ixf[:])
            kb.tt(eq[:], mlo[:], ubc(ixf[:, 0, :], 2, [128, NT, 32]), ALU.is_equal)
            kb.tt(eq[:], eq[:], Gall[:], ALU.mult)
            kb.reduce(gts[:, 0, :], eq[:], ALU.add)
            kb.tt(eq[:], mhi[:], ubc(ixf[:, 1, :], 2, [128, NT, 32]), ALU.is_equal)
            kb.tt(eq[:], eq[:], Gall[:], ALU.mult)
            kb.reduce(gts[:, 1, :], eq[:], ALU.add)
            cmpb = kb.sb("cmpb", [128, NBLK, 32], F32)
            ebf = kb.sb("ebf", [128, NBLK], F32)
            kb.tt(cmpb[:], ubc(start[:], 1, [128, NBLK, 32]), ubc(cst['bvals'][:, 0:NBLK], 2, [128, NBLK, 32]), ALU.is_le)
            kb.reduce(ebf[:], cmpb[:], ALU.add)
            kb.ts(ebf[:], ebf[:], -1.0 + 32.0 * L, 128.0, ALU.add, ALU.mult)
            kb.ts(ebf[:], ebf[:], cst['pcol'][:, 0:1], None, ALU.add)
            oobf = kb.sb("oobf", [128, NBLK], F32)
            kb.ts(oobf[:], cst['bvals'][:, 0:NBLK], cum[ci][:, 31:32], 1.0e7, ALU.is_ge, ALU.mult)
            kb.tt(ebf[:], ebf[:], oobf[:], ALU.add)
            kb.copy(widx_i[:], ebf[:])
            dump(g, 'idx_i', idx_i[:], [128, 2, NT], I32)
            dump(g, 'widx_i', widx_i[:], [128, NBLK], I32)
            dump(g, 'gts', gts[:], [128, 2, NT], F32)
        rows_ap = ROWS[:, :].ap
        for n in tiles:
            for j in range(2):
                iap = h2all[:, n, :].ap
                xap = idx_i[:, j, n:n + 1].ap

                def scat(e, iap=iap, xap=xap):
                    return e.indirect_dma_start(out=rows_ap, out_offset=bass.IndirectOffsetOnAxis(ap=xap, axis=0), in_=iap, in_offset=None)
                kb.dma_custom('pool', scat, sbt=h2all, reads=[h2all[:, n, :], idx_i[:]], writes=[], pwrites=[ROWS[:, :]])
        with kb.phase():
            Wg2 = I['moe_w_gate'][:, :, :, :].ap.rearrange("l e (p k) c -> (l e p) (k c)", k=8)
            Wu2 = I['moe_w_up'][:, :, :, :].ap.rearrange("l e (p k) c -> (l e p) (k c)", k=8)
            Wd2 = I['moe_w_down'][:, :, :, :].ap.rearrange("l e (p k) c -> (l e p) (k c)", k=4)
            wg = [kb.sb("wg%d" % i, [128, 8, 512], BF16) for i in range(2)]
            wu = [kb.sb("wu%d" % i, [128, 8, 512], BF16) for i in range(2)]
            wd = [kb.sb("wd%d" % i, [128, 4, D], BF16) for i in range(2)]
            xr = [kb.sb("xr%d" % i, [128, D], BF16) for i in range(4)]
            xT = [kb.sb("xT%d" % i, [128, 8, BR], BF16) for i in range(2)]
            aT = [kb.sb("aT%d" % i, [128, 4, BR], BF16) for i in range(2)]
            sgm = [kb.sb("sgm%d" % i, [128, BR], BF16) for i in range(2)]
            yo = [kb.sb("yo%d" % i, [128, D], F32) for i in range(2)]
            sgm4 = [kb.sb("sgmx%d" % i, [128, BR], BF16) for i in range(4)]
            yo4 = [kb.sb("yox%d" % i, [128, D], F32) for i in range(4)]

            def mk_block(b):
                def gen(s):
                    wg_, wu_, wd_ = wg[s], wu[s], wd[s]
                    xap = widx_i[:, b:b + 1].ap
                    for (dst, src) in ((wg_, Wg2), (wu_, Wu2), (wd_, Wd2)):
                        oap = dst[:].ap.rearrange("p k c -> p (k c)")

                        def gat(e, oap=oap, src=src, xap=xap):
                            if 'bc' not in g.regcache:
                                g.regcache['bc'] = e.to_reg(2 * 32 * 128 - 1)
                            return e.indirect_dma_start(out=oap, out_offset=None, in_=src, in_offset=bass.IndirectOffsetOnAxis(ap=xap, axis=0),
                                                        bounds_check=g.regcache['bc'], oob_is_err=False)
                        kb.dma_custom('pool', gat, sbt=dst, reads=[widx_i[:]], writes=[dst[:]])
                    xT_ = xT[s]
                    for i in range(2):
                        xr_ = xr[s * 2 + i]
                        kb.dma(xr_[:], ROWS[b * BR + i * 128:b * BR + (i + 1) * 128, :])
                    yield
                    for i in range(2):
                        xr_ = xr[s * 2 + i]
                        pt = g.pT[s]
                        x3 = xr_[:].ap.rearrange("t (p k) -> t k p", k=8)
                        for k in range(8):
                            kb.transpose(pt[:, k * 128:(k + 1) * 128], View(xr_, x3[:, k, :]), g.ident[:])
                        kb.copy(xT_[:, :, i * 128:(i + 1) * 128], rr(pt[:, :], "p (k t) -> p k t", k=8), eng=('act' if i == 0 else 'dve'), part=True)
                        yield
                    aT_ = aT[s]
                    for hc in range(4):
                        pg_ = nextA_s(g, s)
                        for k in range(8):
                            lw = View(wg_, wg_[:, k, :].ap.rearrange("p (m f) -> p f m", f=4)[:, hc, :])
                            kb.mm(pg_[:, 0:BR], lw, xT_[:, k, :], start=(k == 0), stop=(k == 7))
                        pu_ = nextA_s(g, s)
                        for k in range(8):
                            lw = View(wu_, wu_[:, k, :].ap.rearrange("p (m f) -> p f m", f=4)[:, hc, :])
                            kb.mm(pu_[:, 0:BR], lw, xT_[:, k, :], start=(k == 0), stop=(k == 7))
                        sg_ = sgm4[s * 2 + hc % 2]
                        kb.act(sg_[:], pg_[:, 0:BR], AF.Silu)
                        kb.tt(aT_[:, hc, :], sg_[:], pu_[:, 0:BR], ALU.mult, part=True)
                        yield
                    for i in range(2):
                        yo_ = yo4[s * 2 + i]
                        for dh in range(2):
                            py = nextA_s(g, s)
                            for hc in range(4):
                                kb.mm(py[:, :], aT_[:, hc, i * 128:(i + 1) * 128], wd_[:, hc, dh * 512:(dh + 1) * 512], start=(hc == 0), stop=(hc == 3))
                            kb.copy(yo_[:, dh * 512:(dh + 1) * 512], py[:, :], eng=('act' if dh == 0 else 'dve'), part=True)
                        kb.dma(YROWS[b * BR + i * 128:b * BR + (i + 1) * 128, :], yo_[:], part=True)
                        yield
                return gen
            run_chains([mk_block(b) for b in range(NBLK)], 2)
        with kb.phase():
            xt = [kb.sb("xf%d" % i, [128, D], F32) for i in range(2)]
            yl = [kb.sb("yl%d" % i, [128, D], F32) for i in range(2)]
            yh = [kb.sb("yh%d" % i, [128, D], F32) for i in range(2)]
            tmp_l = [kb.sb("tmpf%d" % i, [128, D], F32) for i in range(2)]
            ss_l = [kb.sb("ssf%d" % i, [128, 4], F32) for i in range(2)]
            yrows_ap = YROWS[:, :].ap
            if last:
                fnw = kb.sb("fnw", [128, D], F32)
                bcast_row(kb, fnw[:], I['final_norm_w'][:])
            def _mk(n):
                def gen(s):
                    x_ = xt[s]
                    kb.dma(x_[:], g.X[n * 128:(n + 1) * 128, :])
                    yield
                    bufs = (yl[s], yh[s])
                    for j in range(2):
                        oap = bufs[j][:].ap
                        xap = idx_i[:, j, n:n + 1].ap

                        def gat2(e, oap=oap, xap=xap):
                            return e.indirect_dma_start(out=oap, out_offset=None, in_=yrows_ap, in_offset=bass.IndirectOffsetOnAxis(ap=xap, axis=0))
                        kb.dma_custom('pool', gat2, sbt=bufs[j], reads=[idx_i[:], YROWS[:, :]], writes=[bufs[j][:]])
                        yield
                    mr = modt[0 if n >= NTC else 1]
                    kb.ts(tmp_l[s][:], bufs[0][:], gts[:, 0, n:n + 1], None, ALU.mult)
                    yield
                    kb.stt(tmp_l[s][:], bufs[1][:], gts[:, 1, n:n + 1], tmp_l[s][:], ALU.mult, ALU.add)
                    yield
                    kb.tt(tmp_l[s][:], tmp_l[s][:], mr[:, 2048:3072], ALU.mult)
                    yield
                    kb.tt(x_[:], x_[:], tmp_l[s][:], ALU.add)
                    yield
                    if not last:
                        kb.dma(g.X[n * 128:(n + 1) * 128, :], x_[:])
                        yield
                    else:
                        kb.memset(ss_l[s][:, 0:1], 0.0)
                        yield
                        kb.act(tmp_l[s][:], x_[:], AF.Square, accum=ss_l[s][:, 0:1])
                        yield
                        kb.ts(ss_l[s][:, 1:2], ss_l[s][:, 0:1], 1.0 / D, EPS, ALU.mult, ALU.add)
                        yield
                        kb.act(ss_l[s][:, 2:3], ss_l[s][:, 1:2], AF.Sqrt)
                        yield
                        kb.recip(ss_l[s][:, 3:4], ss_l[s][:, 2:3])
                        yield
                        kb.stt(x_[:], x_[:], ss_l[s][:, 3:4], fnw[:], ALU.mult, ALU.mult)
                        yield
                        kb.dma(g.out[(n - NTC) * 128:(n - NTC + 1) * 128, :], x_[:], is_output=True)
                        yield
                    yield
                return gen
            run_chains([_mk(n) for n in tiles], 2)


def run_chains(makers, width):
    makers = list(makers)
    nxt = 0
    active = {}
    free = list(range(width))
    while nxt < len(makers) or active:
        while free and nxt < len(makers):
            s = free.pop(0)
            active[s] = makers[nxt](s)
            nxt += 1
        for s in sorted(active):
            try:
                next(active[s])
            except StopIteration:
                del active[s]
                free.append(s)


def nextA_s(g, s, nslots=2):
    per = len(g.pA) // nslots
    d = g.__dict__.setdefault('pAs', {})
    d[s] = (d.get(s, -1) + 1) % per
    return g.pA[s * per + d[s]]


def norm_mod_gen(g, xt, modr, hb, ss, tmp):
    kb = g.kb
    kb.memset(ss[:, 0:1], 0.0)
    kb.act(tmp[:], xt, AF.Square, accum=ss[:, 0:1])
    yield
    kb.ts(ss[:, 1:2], ss[:, 0:1], 1.0 / D, EPS, ALU.mult, ALU.add)
    yield
    kb.act(ss[:, 2:3], ss[:, 1:2], AF.Sqrt)
    yield
    kb.recip(ss[:, 3:4], ss[:, 2:3])
    yield
    kb.stt(tmp[:], xt, ss[:, 3:4], modr[:, 1024:2048], ALU.mult, ALU.mult)
    yield
    kb.tt(hb, tmp[:], modr[:, 0:1024], ALU.add)
    yield


def transpose_gen(g, s, dst3, src_bf, nchunk, eng='act'):
    kb = g.kb
    pt = g.pT[s]
    for k in range(nchunk):
        kb.transpose(pt[:, k * 128:(k + 1) * 128], View(src_bf.tile, src_bf.ap[:, k * 128:(k + 1) * 128]), g.ident[:])
    yield
    kb.copy(dst3, rr(pt[:, 0:nchunk * 128], "p (k t) -> p k t", k=nchunk), eng=eng, part=True)
    yield


def kernel(**inputs):
    inputs = {k: np.asarray(v) for k, v in inputs.items()}
    nc = build()
    sh = prep_shared(inputs)
    maps = [prep_core(inputs, sh, b, nc._used_inputs) for b in range(8)]
    res = run_bass_kernel_spmd(nc, maps, core_ids=list(range(8)))
    out = np.stack([np.asarray(r["out"], dtype=np.float32) for r in res.results], 0)
    return out
```

```python
import numpy as np
import ml_dtypes
from contextlib import ExitStack
import concourse.bass as bass
import concourse.mybir as mybir
from concourse.bass_utils import run_bass_kernel_spmd

F32 = mybir.dt.float32
BF16 = mybir.dt.bfloat16
I32 = mybir.dt.int32
AF = mybir.ActivationFunctionType
ALU = mybir.AluOpType
AX = mybir.AxisListType


class View:
    def __init__(self, tile, ap):
        self.tile = tile
        self.ap = ap


class Tile:
    def __init__(self, name, handle, space):
        self.name = name
        self.h = handle
        self.space = space
        self.w = {}
        self.r = {}
        self.pw = {}
        self.dsem = {}

    def __getitem__(self, idx):
        return View(self, self.h[idx])


def _merge(d, tok):
    k, sem, val = tok
    if k not in d or d[k][1] < val:
        d[k] = (sem, val)


class KB:
    def __init__(self, nc, st):
        self.nc = nc
        self.st = st
        self.base_st = st
        self.all_recs = []
        self.sem_pool = {'sw': [], 'hw': []}
        self.alloc_log = []
        self.engs = {'pe': nc.tensor, 'act': nc.scalar, 'dve': nc.vector, 'pool': nc.gpsimd, 'sp': nc.sync}
        self.prog = {e: [] for e in self.engs}
        self.esem = {e: st.enter_context(nc.semaphore("es_" + e)) for e in self.engs}
        self.ecnt = {e: 0 for e in self.engs}
        self.seen = {e: {} for e in self.engs}
        self.nsem = len(self.engs)
        self.out_tokens = []
        self.ninst = 0

    def sb(self, name, shape, dt):
        self.uid = getattr(self, 'uid', 0) + 1
        name = "s%d_%s" % (self.uid, name)
        h = self.st.enter_context(self.nc.sbuf_tensor(name, list(shape), dt))
        t = Tile(name, h, 'sb')
        self.alloc_log.append(t)
        return t

    def ps(self, name, shape, dt):
        self.uid = getattr(self, 'uid', 0) + 1
        name = "p%d_%s" % (self.uid, name)
        h = self.st.enter_context(self.nc.psum_tensor(name, list(shape), dt))
        return Tile(name, h, 'ps')

    def dram(self, name, shape, dt, kind="Internal"):
        h = self.nc.dram_tensor(name, list(shape), dt, kind=kind)
        return Tile(name, h.ap(), 'dram')

    def _deps(self, eng, reads, writes, pwrites):
        deps = {}
        for v in reads:
            t = v.tile
            if t.pw:
                t.w = t.pw
                t.pw = {}
                t.r = {}
            for k, (sem, val) in t.w.items():
                _merge(deps, (k, sem, val))
            if t.space == 'ps':
                for k, (sem, val) in t.r.items():
                    if k != 'e_' + eng:
                        _merge(deps, (k, sem, val))
        for v in writes:
            t = v.tile
            if t.pw:
                t.w = t.pw
                t.pw = {}
                t.r = {}
            for d in (t.w, t.r):
                for k, (sem, val) in d.items():
                    _merge(deps, (k, sem, val))
        for v in pwrites:
            t = v.tile
            for d in (t.w, t.r):
                for k, (sem, val) in d.items():
                    _merge(deps, (k, sem, val))
        seen = self.seen[eng]
        for k, (sem, val) in deps.items():
            if eng == 'pe' and k == 'e_pe':
                continue
            if seen.get(k, 0) >= val:
                continue
            seen[k] = val
            self.prog[eng].append(('w', sem, val))

    def _commit(self, tok, reads, writes, pwrites):
        for v in reads:
            _merge(v.tile.r, tok)
        for v in writes:
            v.tile.w = {}
            v.tile.r = {}
            _merge(v.tile.w, tok)
        for v in pwrites:
            _merge(v.tile.pw, tok)

    def op(self, eng, fn, reads=(), writes=(), pwrites=()):
        reads = [v for v in reads if isinstance(v, View)]
        self._deps(eng, reads, writes, pwrites)
        self.ecnt[eng] += 1
        tok = ('e_' + eng, self.esem[eng], self.ecnt[eng])
        self.prog[eng].append(('i', fn, self.esem[eng], 1))
        self._commit(tok, reads, writes, pwrites)
        self.ninst += 1
        return tok

    def dma(self, out, in_, eng='sp', part=False, is_output=False, **kw):
        sbt = out.tile if out.tile.space == 'sb' else in_.tile
        kind = 'sw' if eng == 'pool' else 'hw'
        if kind not in sbt.dsem:
            if self.sem_pool[kind]:
                sbt.dsem[kind] = self.sem_pool[kind].pop()
            else:
                sem = self.base_st.enter_context(self.nc.semaphore("ds_%d" % len(self.all_recs)))
                sbt.dsem[kind] = {'sem': sem, 'cnt': 0, 'key': 'd%d' % len(self.all_recs)}
                self.all_recs.append(sbt.dsem[kind])
                self.nsem += 1
        reads = [in_]
        writes = [] if part else [out]
        pwrites = [out] if part else []
        self._deps(eng, reads, writes, pwrites)
        rec = sbt.dsem[kind]
        rec['cnt'] += 16
        tok = (rec['key'], rec['sem'], rec['cnt'])
        oap, iap = out.ap, in_.ap
        self.prog[eng].append(('i', lambda e: e.dma_start(out=oap, in_=iap, **kw), rec['sem'], 16))
        self._commit(tok, reads, writes, pwrites)
        if is_output:
            self.out_tokens.append(tok)
        self.ninst += 1
        return tok

    def dma_custom(self, eng, fn, sbt, reads, writes, pwrites=(), is_output=False):
        kind = 'sw' if eng == 'pool' else 'hw'
        if kind not in sbt.dsem:
            if self.sem_pool[kind]:
                sbt.dsem[kind] = self.sem_pool[kind].pop()
            else:
                sem = self.base_st.enter_context(self.nc.semaphore("ds_%d" % len(self.all_recs)))
                sbt.dsem[kind] = {'sem': sem, 'cnt': 0, 'key': 'd%d' % len(self.all_recs)}
                self.all_recs.append(sbt.dsem[kind])
        reads = list(reads)
        writes = list(writes)
        pwrites = list(pwrites)
        self._deps(eng, reads, writes, pwrites)
        rec = sbt.dsem[kind]
        rec['cnt'] += 16
        tok = (rec['key'], rec['sem'], rec['cnt'])
        self.prog[eng].append(('i', fn, rec['sem'], 16))
        self._commit(tok, reads, writes, pwrites)
        if is_output:
            self.out_tokens.append(tok)
        return tok

    def mm(self, out, lhsT, rhs, start=True, stop=True, **kw):
        o, l, r = out.ap, lhsT.ap, rhs.ap
        fn = lambda e: e.matmul(o, l, r, start=start, stop=stop, **kw)
        if start:
            return self.op('pe', fn, [lhsT, rhs], [out])
        return self.op('pe', fn, [lhsT, rhs], [], [out])

    def transpose(self, out, in_, ident):
        o, i, d = out.ap, in_.ap, ident.ap
        return self.op('pe', lambda e: e.transpose(o, i, d), [in_, ident], [out])

    def act(self, out, in_, func, bias=None, scale=None, accum=None, part=False, eng='act'):
        o, i = out.ap, in_.ap
        kw = {}
        reads = [in_]
        if bias is not None:
            if isinstance(bias, View):
                kw['bias'] = bias.ap
                reads.append(bias)
            else:
                kw['bias'] = bias
        if scale is not None:
            if isinstance(scale, View):
                kw['scale'] = scale.ap
                reads.append(scale)
            else:
                kw['scale'] = scale
        ws = [] if part else [out]
        pws = [out] if part else []
        if accum is not None:
            kw['accum_out'] = accum.ap
            ws = ws + [accum]
        return self.op('act', lambda e: e.activation(o, i, func, **kw), reads, ws, pws)

    def tt(self, out, a, b, op, eng='dve', part=False):
        o, x, y = out.ap, a.ap, b.ap
        ws = [] if part else [out]
        pws = [out] if part else []
        return self.op(eng, lambda e: e.tensor_tensor(o, x, y, op), [a, b], ws, pws)

    def ts(self, out, a, s1, s2, op0, op1=None, eng='dve', part=False, accum=None):
        o, x = out.ap, a.ap
        reads = [a]
        v1 = s1.ap if isinstance(s1, View) else s1
        v2 = s2.ap if isinstance(s2, View) else s2
        if isinstance(s1, View):
            reads.append(s1)
        if isinstance(s2, View):
            reads.append(s2)
        ws = [] if part else [out]
        pws = [out] if part else []
        kw = {}
        if accum is not None:
            kw['accum_out'] = accum.ap
            ws = ws + [accum]
        if op1 is None:
            return self.op(eng, lambda e: e.tensor_scalar(o, x, v1, None, op0, **kw), reads, ws, pws)
        return self.op(eng, lambda e: e.tensor_scalar(o, x, v1, v2, op0, op1, **kw), reads, ws, pws)

    def stt(self, out, a, s, b, op0, op1, eng='dve', part=False):
        o, x, y = out.ap, a.ap, b.ap
        reads = [a, b]
        sv = s.ap if isinstance(s, View) else s
        if isinstance(s, View):
            reads.append(s)
        ws = [] if part else [out]
        pws = [out] if part else []
        return self.op(eng, lambda e: e.scalar_tensor_tensor(o, x, sv, y, op0, op1), reads, ws, pws)

    def copy(self, out, in_, eng='dve', part=False):
        o, i = out.ap, in_.ap
        ws = [] if part else [out]
        pws = [out] if part else []
        if eng == 'act':
            return self.op('act', lambda e: e.copy(o, i), [in_], ws, pws)
        return self.op(eng, lambda e: e.tensor_copy(o, i), [in_], ws, pws)

    def reduce(self, out, in_, op, axis=AX.X, eng='dve'):
        o, i = out.ap, in_.ap
        return self.op(eng, lambda e: e.tensor_reduce(o, i, axis, op), [in_], [out])

    def memset(self, out, val, eng='dve'):
        o = out.ap
        return self.op(eng, lambda e: e.memset(o, val), [], [out])

    def recip(self, out, in_):
        o, i = out.ap, in_.ap
        return self.op('dve', lambda e: e.reciprocal(o, i), [in_], [out])

    def barrier(self):
        for eng in self.engs:
            seen = self.seen[eng]
            for e2 in self.engs:
                if self.ecnt[e2] > 0 and seen.get('e_' + e2, 0) < self.ecnt[e2] and not (eng == 'pe' and e2 == 'pe'):
                    seen['e_' + e2] = self.ecnt[e2]
                    self.prog[eng].append(('w', self.esem[e2], self.ecnt[e2]))
            for rec in self.all_recs:
                k = rec['key']
                if rec['cnt'] > 0 and seen.get(k, 0) < rec['cnt']:
                    seen[k] = rec['cnt']
                    self.prog[eng].append(('w', rec['sem'], rec['cnt']))

    def phase(self):
        return _Phase(self)

    def emit(self):
        nc = self.nc
        for (k, sem, val) in self.out_tokens:
            if self.seen['sp'].get(k, 0) < val:
                self.seen['sp'][k] = val
                self.prog['sp'].append(('w', sem, val))
        with nc.Block() as block:
            def run(e, lst):
                for it in lst:
                    if it[0] == 'w':
                        e.wait_ge(it[1], it[2])
                    else:
                        it[1](e).then_inc(it[2], it[3])

            @block.tensor
            def _(e):
                run(e, self.prog['pe'])

            @block.scalar
            def _(e):
                run(e, self.prog['act'])

            @block.vector
            def _(e):
                run(e, self.prog['dve'])

            @block.gpsimd
            def _(e):
                run(e, self.prog['pool'])

            @block.sync
            def _(e):
                run(e, self.prog['sp'])


class _Phase:
    def __init__(self, kb):
        self.kb = kb

    def __enter__(self):
        self.old = self.kb.st
        self.s = ExitStack()
        self.s.__enter__()
        self.kb.st = self.s
        self.mark = len(self.kb.alloc_log)
        return self

    def __exit__(self, *a):
        if a[0] is None:
            self.kb.barrier()
            for t in self.kb.alloc_log[self.mark:]:
                for kind, rec in t.dsem.items():
                    self.kb.sem_pool[kind].append(rec)
                t.dsem = {}
            del self.kb.alloc_log[self.mark:]
        self.s.__exit__(*a)
        self.kb.st = self.old
        return False


def bc(v, shape):
    return View(v.tile, v.ap.to_broadcast(list(shape)))


def rr(v, pat, **kw):
    return View(v.tile, v.ap.rearrange(pat, **kw))

import math

D = 1024
NT = 18
NTC = 2
NTL = 16
TT = NT * 128
EPS = 1e-6
SCALE = 128 ** -0.5
TBLK = [(0, 512), (512, 512), (1024, 512), (1536, 512), (2048, 256)]


def host_consts():
    c = {}
    c['ident'] = np.eye(128).astype(ml_dtypes.bfloat16)
    c['identf'] = np.eye(128).astype(np.float32)
    t = np.arange(2048)
    row = (t // 64).astype(np.float64)
    col = (t % 64).astype(np.float64)
    inv = 10000.0 ** (-np.arange(32, dtype=np.float64) / 32)
    ang = np.concatenate([row[:, None] * inv, col[:, None] * inv], -1)
    cs, sn = np.cos(ang), np.sin(ang)

    def lay(a):
        return np.ascontiguousarray(a.reshape(16, 128, 64).transpose(1, 0, 2)).astype(np.float32)
    c['cos_q'] = lay(cs * SCALE)
    c['sin_q'] = lay(sn * SCALE)
    c['cos_k'] = lay(cs)
    c['sin_k'] = lay(sn)
    p = np.arange(128)
    i = p[None, :]
    j = p[:, None]
    c['dpos'] = np.maximum(i - j, 0).astype(np.float32)
    c['indf'] = (i >= j).astype(np.float32)
    c['dneg'] = np.maximum(j - i, 0).astype(np.float32)
    c['indb'] = (j >= i).astype(np.float32)
    c['ptab'] = np.stack([127 - p, p, p + 1, 128 - p], 1).astype(np.float32)
    ch = np.arange(128)
    phi = 2 * np.pi * np.outer(ch, ch) / 128
    c['dft_c'] = np.concatenate([np.cos(phi), np.sin(phi)], 1) / np.sqrt(128)
    c['dft_c'] = c['dft_c'].astype(ml_dtypes.bfloat16)
    tl = np.arange(2048)
    th = 2 * np.pi * ((np.outer(tl, tl)) % 2048) / 2048
    c['dft_tl_c'] = (np.cos(th) / np.sqrt(2048)).astype(ml_dtypes.bfloat16)
    c['dft_tl_s'] = (-np.sin(th) / np.sqrt(2048)).astype(ml_dtypes.bfloat16)
    tc = np.arange(256)
    th = 2 * np.pi * ((np.outer(tc, tc)) % 256) / 256
    c['dft_tc_c'] = (np.cos(th) / np.sqrt(256)).astype(ml_dtypes.bfloat16)
    c['dft_tc_s'] = (-np.sin(th) / np.sqrt(256)).astype(ml_dtypes.bfloat16)
    c['lowS'] = (j > i).astype(np.float32)
    c['upS'] = (j < i).astype(np.float32)
    c['onesf'] = np.ones((128, 128), np.float32)
    c['thr'] = np.tile((256.0 * np.arange(18))[None, :], (128, 1)).astype(np.float32)
    c['bvals'] = np.tile(np.arange(50.0)[None, :], (128, 1)).astype(np.float32)
    c['pcol'] = np.arange(128.0).reshape(128, 1).astype(np.float32)
    return c


CONST_SPECS = {
    'ident': ([128, 128], BF16), 'identf': ([128, 128], F32),
    'cos_q': ([128, 16, 64], F32), 'sin_q': ([128, 16, 64], F32),
    'cos_k': ([128, 16, 64], F32), 'sin_k': ([128, 16, 64], F32),
    'dpos': ([128, 128], F32), 'indf': ([128, 128], F32), 'dneg': ([128, 128], F32), 'indb': ([128, 128], F32),
    'ptab': ([128, 4], F32), 'dft_c': ([128, 256], BF16),
    'dft_tl_c': ([2048, 2048], BF16), 'dft_tl_s': ([2048, 2048], BF16),
    'dft_tc_c': ([256, 256], BF16), 'dft_tc_s': ([256, 256], BF16),
}

IN_SPECS = {
    'x_all': ([TT, D], F32), 'c2t': ([128, 16], F32),
    'norm1_w': ([2, D], F32), 'norm2_w': ([2, D], F32),
    'ada_w': ([2, D, 6 * D], F32), 'ada_b': ([2, 6 * D], F32),
    'even_w_in': ([1, D, 2560], F32), 'ret_decay_logit': ([1, 8], F32), 'ret_gn_w': ([1, 512], F32),
    'even_w_out': ([1, D, D], F32),
    'router_w': ([2, D, 36], F32), 'router_b': ([2, 36], F32),
    'moe_w_gate': ([2, 32, D, 512], F32), 'moe_w_up': ([2, 32, D, 512], F32), 'moe_w_down': ([2, 32, 512, D], F32),
    'final_norm_w': ([D], F32),
}


SPARSE = True


class Ctx:
    pass


class LazyInputs(dict):
    def __init__(self, kb):
        super().__init__()
        self.kb = kb

    def __missing__(self, k):
        shp, dt = IN_SPECS[k] if k in IN_SPECS else CONST_SPECS[k]
        t = self.kb.dram(k, shp, dt, kind="ExternalInput")
        self[k] = t
        return t


def build(stop_after=None, dbg=()):
    nc = bass.Bass("TRN2", target_bir_lowering=False)
    st = ExitStack()
    with st:
        kb = KB(nc, st)
        g = Ctx()
        g.kb = kb
        g.I = LazyInputs(kb)
        g.out = kb.dram("out", [2048, D], F32, kind="ExternalOutput")
        g.X = kb.dram("Xs", [TT, D], F32)
        g.dbg = {}
        g.regcache = {}
        g.dbg_want = dbg
        g.pT = [kb.ps("pT%d" % i, [128, 1024], BF16) for i in range(2)]
        g.pA = [kb.ps("pA%d" % i, [128, 512], F32) for i in range(6)]
        g.pTi = 0
        g.pAi = 0
        g.ident = kb.sb("ident", [128, 128], BF16)
        kb.dma(g.ident[:], g.I['ident'][:])
        g.identf = kb.sb("identf", [128, 128], F32)
        kb.dma(g.identf[:], g.I['identf'][:])
        g.stop_after = stop_after
        layer0(g)
        if stop_after == 'mix0':
            finish_debug(g)
        else:
            (moe_sparse if SPARSE else moe_stage)(g, 0, False)
            if stop_after == 'l0':
                finish_debug(g)
            else:
                layer1(g)
                if stop_after == 'mix1':
                    finish_debug(g)
                else:
                    (moe_sparse if SPARSE else moe_stage)(g, 1, True)
        kb.emit()
        nc._used_inputs = list(g.I.keys())
        nc._dbg = list(g.dbg.keys())
    return nc


def nextA(g):
    g.pAi = (g.pAi + 1) % len(g.pA)
    return g.pA[g.pAi]


def nextT(g):
    g.pTi = (g.pTi + 1) % len(g.pT)
    return g.pT[g.pTi]


def dump(g, name, view, shape, dt=F32):
    if name not in g.dbg_want:
        return
    kb = g.kb
    d = kb.dram("dbg_" + name, shape, dt, kind="ExternalOutput")
    g.dbg[name] = d
    kb.dma(d[:], view, is_output=True)


def finish_debug(g):
    kb = g.kb
    with kb.phase():
        t = kb.sb("fin_t", [128, D], F32)
        dc = kb.dram("dbg_Xc", [256, D], F32, kind="ExternalOutput")
        for n in range(NTC):
            kb.dma(t[:], g.X[n * 128:(n + 1) * 128, :])
            kb.dma(dc[n * 128:(n + 1) * 128, :], t[:], is_output=True)
        for n in range(NTL):
            kb.dma(t[:], g.X[(n + 2) * 128:(n + 3) * 128, :])
            kb.dma(g.out[n * 128:(n + 1) * 128, :], t[:], is_output=True)


def bcast_row(kb, dst, dram_view):
    v = View(dram_view.tile, dram_view.ap.partition_broadcast(128))
    kb.dma(dst, v)


def compute_mod(g, L, half, modt):
    kb = g.kb
    with kb.phase():
        c2 = kb.sb("c2", [128, 16], F32)
        c2s = kb.sb("c2s", [128, 16], BF16)
        crep = kb.sb("crep", [128, 16, 128], BF16)
        kb.dma(c2[:], g.I['c2t'][:])
        kb.act(c2s[:], c2[:], AF.Silu)
        for j in range(16):
            kb.copy(crep[:, j, :], bc(c2s[:, j:j + 1], [128, 128]), part=True)
        wb = [kb.sb("adaw%d" % i, [128, 8, 512], BF16) for i in range(2)]
        bb = [kb.sb("adab%d" % i, [128, 512], F32) for i in range(2)]
        nw = kb.sb("nw", [128, D], F32)
        nwname = 'norm1_w' if half == 0 else 'norm2_w'
        bcast_row(kb, nw[:], g.I[nwname][L, :])
        for blk in range(6):
            c0 = half * 3072 + blk * 512
            w = wb[blk % 2]
            b = bb[blk % 2]
            kb.dma(w[:], rr(g.I['ada_w'][L, :, c0:c0 + 512], "(k p) c -> p k c", p=128), eng='pool')
            bcast_row(kb, b[:], g.I['ada_b'][L, c0:c0 + 512])
            for r in range(2):
                ps = nextA(g)
                for k in range(8):
                    kb.mm(ps[:, :], crep[:, r * 8 + k, :], w[:, k, :], start=(k == 0), stop=(k == 7))
                kb.tt(modt[r][:, blk * 512:(blk + 1) * 512], ps[:, :], b[:], ALU.add, part=True)
        for r in range(2):
            kb.stt(modt[r][:, 1024:2048], modt[r][:, 1024:2048], 1.0, nw[:], ALU.add, ALU.mult)


def norm_mod_tile(g, xt, modr, hb, ss, tmp):
    kb = g.kb
    if isinstance(ss, list):
        g.nmi = getattr(g, 'nmi', 0) + 1
        ss = ss[g.nmi % len(ss)]
        tmp = tmp[g.nmi % len(tmp)]
    kb.memset(ss[:, 0:1], 0.0)
    kb.act(tmp[:], xt, AF.Square, accum=ss[:, 0:1])
    kb.ts(ss[:, 1:2], ss[:, 0:1], 1.0 / D, EPS, ALU.mult, ALU.add)
    kb.act(ss[:, 2:3], ss[:, 1:2], AF.Sqrt)
    kb.recip(ss[:, 3:4], ss[:, 2:3])
    kb.stt(tmp[:], xt, ss[:, 3:4], modr[:, 1024:2048], ALU.mult, ALU.mult)
    kb.tt(hb, tmp[:], modr[:, 0:1024], ALU.add)


def transpose_into(g, dst3, src_bf, nchunk, eng='act'):
    kb = g.kb
    pt = nextT(g)
    for k in range(nchunk):
        kb.transpose(pt[:, k * 128:(k + 1) * 128], View(src_bf.tile, src_bf.ap[:, k * 128:(k + 1) * 128]), g.ident[:])
    kb.copy(dst3, rr(pt[:, 0:nchunk * 128], "p (k t) -> p k t", k=nchunk), eng=eng, part=True)


def layer0(g):
    kb = g.kb
    I = g.I
    L = 0
    with kb.phase():
        mixT = kb.sb("mixT", [128, 8, TT], BF16)
        SGs = kb.dram("SGs", [TT, 512], BF16)
        FTs = kb.dram("FTs", [4, 128, TT], BF16)
        modt = [kb.sb("modt%d" % r, [128, 3072], F32) for r in range(2)]
        compute_mod(g, L, 0, modt)
        with kb.phase():
            qT = kb.sb("qT", [128, 4, TT], BF16)
            kT = kb.sb("kT", [128, 4, TT], BF16)
            k_tm = kb.sb("k_tm", [128, NT, 512], BF16)
            v_tm = kb.sb("v_tm", [128, NT, 512], BF16)
            with kb.phase():
                hT = mixT
                sgt = [kb.sb("sgt%d" % i, [128, 512], BF16) for i in range(2)]
                xt = [kb.sb("xt%d" % i, [128, D], F32) for i in range(2)]
                tmp = [kb.sb("tmp_%d" % i, [128, D], F32) for i in range(2)]
                hb_l = [kb.sb("hb_%d" % i, [128, D], BF16) for i in range(2)]
                ss = [kb.sb("ss_%d" % i, [128, 4], F32) for i in range(2)]
                def mk_norm(n):
                    def gen(s):
                        kb.dma(xt[s][:], I['x_all'][n * 128:(n + 1) * 128, :])
                        yield
                        yield from norm_mod_gen(g, xt[s][:], modt[0 if n >= NTC else 1], hb_l[s][:], ss[s], tmp[s])
                        yield from transpose_gen(g, s, hT[:, :, n * 128:(n + 1) * 128], hb_l[s][:], 8)
                    return gen
                run_chains([mk_norm(n) for n in range(NT)], 2)
                wb = [kb.sb("winb%d" % i, [128, 8, 512], BF16) for i in range(2)]
                rope_c = kb.sb("rope_c", [128, 16, 64], F32)
                rope_s = kb.sb("rope_s", [128, 16, 64], F32)
                pf2 = [kb.sb("pf%d" % i, [128, 512], F32) for i in range(2)]
                ra2 = [kb.sb("ra%d" % i, [128, 4, 64], F32) for i in range(4)]
                rb2 = [kb.sb("rb%d" % i, [128, 4, 64], F32) for i in range(4)]
                pb2 = [kb.sb("pb%d" % i, [128, 512], BF16) for i in range(2)]
                for cb in range(5):
                    w = wb[cb % 2]
                    kb.dma(w[:], rr(I['even_w_in'][0, :, cb * 512:(cb + 1) * 512], "(k p) c -> p k c", p=128), eng='pool')
                    if cb < 2:
                        kb.dma(rope_c[:], I['cos_q' if cb == 0 else 'cos_k'][:])
                        kb.dma(rope_s[:], I['sin_q' if cb == 0 else 'sin_k'][:])
                    if cb < 4:
                        def mk_proj(n, cb=cb, w=w):
                            def gen(s):
                                ps = nextA_s(g, s)
                                for k in range(8):
                                    kb.mm(ps[:, :], hT[:, k, n * 128:(n + 1) * 128], w[:, k, :], start=(k == 0), stop=(k == 7))
                                yield
                                if cb in (0, 1):
                                    pf, pb = pf2[s], pb2[s]
                                    ra, rb = ra2[s * 2], rb2[s * 2]
                                    ra_, rb_ = ra2[s * 2 + 1], rb2[s * 2 + 1]
                                    dstb = pb[:] if cb == 0 else k_tm[:, n, :]
                                    if n >= NTC:
                                        nl = n - NTC
                                        kb.copy(pf[:], ps[:, :], eng='act')
                                        yield
                                        p3 = rr(pf[:], "p (h e) -> p h e", h=4)
                                        x1 = View(pf, p3.ap[:, :, 0:64])
                                        x2 = View(pf, p3.ap[:, :, 64:128])
                                        cosb = bc(rope_c[:, nl:nl + 1, :], [128, 4, 64])
                                        sinb = bc(rope_s[:, nl:nl + 1, :], [128, 4, 64])
                                        d3 = rr(dstb, "p (h e) -> p h e", h=4)
                                        o1 = View(dstb.tile, d3.ap[:, :, 0:64])
                                        o2 = View(dstb.tile, d3.ap[:, :, 64:128])
                                        kb.tt(ra[:], x1, cosb, ALU.mult)
                                        kb.tt(ra_[:], x1, sinb, ALU.mult, eng='pool')
                                        yield
                                        kb.tt(rb[:], x2, sinb, ALU.mult)
                                        kb.tt(rb_[:], x2, cosb, ALU.mult, eng='pool')
                                        yield
                                        kb.tt(o1, ra[:], rb[:], ALU.subtract, part=True)
                                        kb.tt(o2, ra_[:], rb_[:], ALU.add, part=True, eng='pool')
                                        yield
                                    else:
                                        if cb == 0:
                                            kb.act(dstb, ps[:, :], AF.Copy, scale=SCALE)
                                        else:
                                            kb.copy(dstb, ps[:, :], eng='act')
                                        yield
                                    dT = qT if cb == 0 else kT
                                    yield from transpose_gen(g, s, dT[:, :, n * 128:(n + 1) * 128], dstb, 4, eng='dve')
                                elif cb == 2:
                                    kb.copy(v_tm[:, n, :], ps[:, :], eng='act', part=True)
                                    yield
                                else:
                                    kb.act(sgt[s][:], ps[:, :], AF.Silu)
                                    yield
                                    kb.dma(SGs[n * 128:(n + 1) * 128, :], sgt[s][:], part=True)
                                    yield
                            return gen
                        run_chains([mk_proj(n) for n in range(NT)], 2)
                    else:
                        for gi in range(4):
                            for (t0, tl) in TBLK:
                                ps = nextA(g)
                                for k in range(8):
                                    kb.mm(ps[:, 0:tl], w[:, k, gi * 128:(gi + 1) * 128], hT[:, k, t0:t0 + tl], start=(k == 0), stop=(k == 7))
                                ft_ = sgt[(gi + t0 // 512) % 2]
                                kb.copy(ft_[:, 0:tl], ps[:, 0:tl], eng='act')
                                kb.dma(FTs[gi, :, t0:t0 + tl], ft_[:, 0:tl], part=True)
            dump(g, 'qT', qT[:, 0, 256:768], [128, 512], BF16)
            dump(g, 'v_tm', v_tm[:, 2, :], [128, 512], BF16)
            retention(g, qT, kT, k_tm, v_tm, SGs, mixT)
        fourier(g, FTs, mixT)
        dump(g, 'mixT_r', mixT[:, 0, 256:768], [128, 512], BF16)
        dump(g, 'mixT_f', mixT[:, 4, 256:768], [128, 512], BF16)
        with kb.phase():
            wo = kb.sb("wo", [128, 8, D], BF16)
            kb.dma(wo[:], rr(I['even_w_out'][0, :, :], "(k p) c -> p k c", p=128), eng='pool')
            xt = [kb.sb("xt4_%d" % i, [128, D], F32) for i in range(2)]
            yt_l = [kb.sb("yt4_%d" % i, [128, D], F32) for i in range(2)]
            def _mk(n):
                def gen(s):
                    x_ = xt[s]
                    kb.dma(x_[:], I['x_all'][n * 128:(n + 1) * 128, :])
                    yield
                    mr = modt[0 if n >= NTC else 1]
                    for dh in range(2):
                        ps = nextA_s(g, s)
                        for k in range(8):
                            kb.mm(ps[:, :], mixT[:, k, n * 128:(n + 1) * 128], wo[:, k, dh * 512:(dh + 1) * 512], start=(k == 0), stop=(k == 7))
                        kb.tt(yt_l[s][:, dh * 512:(dh + 1) * 512], ps[:, :], mr[:, 2048 + dh * 512:2048 + (dh + 1) * 512], ALU.mult, part=True)
                        yield
                    kb.tt(x_[:], x_[:], yt_l[s][:], ALU.add)
                    yield
                    kb.dma(g.X[n * 128:(n + 1) * 128, :], x_[:])
                    yield
                    yield
                return gen
            run_chains([_mk(n) for n in range(NT)], 2)


def retention(g, qT, kT, k_tm, v_tm, SGs, mixT):
    kb = g.kb
    I = g.I
    with kb.phase():
        dl = kb.sb("dl", [128, 8], F32)
        lg = kb.sb("lg", [128, 8], F32)
        bcast_row(kb, dl[:], I['ret_decay_logit'][0, :])
        kb.act(lg[:], dl[:], AF.Exp, scale=-1.0)
        kb.ts(lg[:], lg[:], 1.0, None, ALU.add)
        kb.act(lg[:], lg[:], AF.Ln)
        kb.ts(lg[:], lg[:], -1.0, None, ALU.mult)
        consts = {nm: kb.sb("rc_" + nm, [128, 128], F32) for nm in ('dpos', 'indf', 'dneg', 'indb')}
        for nm in consts:
            kb.dma(consts[nm][:], I[nm][:])
        ptab = kb.sb("ptab", [128, 4], F32)
        kb.dma(ptab[:], I['ptab'][:])
        MT = kb.sb("MT", [128, 4, 128], F32)
        mtmp = kb.sb("mtmp", [128, 128], F32)
        pv = kb.sb("pv", [128, 4, 4], F32)
        g128 = kb.sb("g128", [128, 2, 512], F32)
        for h in range(4):
            kb.act(MT[:, h, :], consts['dpos'][:], AF.Exp, scale=lg[:, h:h + 1], part=True)
            kb.tt(MT[:, h, :], MT[:, h, :], consts['indf'][:], ALU.mult, part=True)
            kb.act(mtmp[:], consts['dneg'][:], AF.Exp, scale=lg[:, 4 + h:5 + h])
            kb.tt(mtmp[:], mtmp[:], consts['indb'][:], ALU.mult)
            kb.tt(MT[:, h, :], MT[:, h, :], mtmp[:], ALU.add, part=True)
            kb.act(pv[:, 0, h:h + 1], ptab[:, 0:1], AF.Exp, scale=lg[:, h:h + 1], part=True)
            kb.act(pv[:, 1, h:h + 1], ptab[:, 1:2], AF.Exp, scale=lg[:, 4 + h:5 + h], part=True)
            kb.act(pv[:, 2, h:h + 1], ptab[:, 2:3], AF.Exp, scale=lg[:, h:h + 1], part=True)
            kb.act(pv[:, 3, h:h + 1], ptab[:, 3:4], AF.Exp, scale=lg[:, 4 + h:5 + h], part=True)
            for d in range(2):
                kb.act(mtmp[:, 0:1], lg[:, d * 4 + h:d * 4 + h + 1], AF.Exp, scale=128.0)
                kb.copy(g128[:, d, h * 128:(h + 1) * 128], bc(mtmp[:, 0:1], [128, 128]), part=True)
        S_all = [kb.sb("S_all%d" % d, [128, NT, 512], BF16) for d in range(2)]
        S = kb.sb("S_run", [128, 512], F32)
        S2 = kb.sb("S_tmp", [128, 512], F32)
        vt = kb.sb("vtail", [128, 512], BF16)
        for d in range(2):
            order = list(range(NT)) if d == 0 else [1, 0] + list(range(NT - 1, 1, -1))
            kb.memset(S[:], 0.0)
            for idx, n in enumerate(order):
                kb.copy(S_all[d][:, n, :], S[:], eng='act', part=True)
                if idx == NT - 1:
                    break
                for h in range(4):
                    kb.ts(vt[:, h * 128:(h + 1) * 128], v_tm[:, n, h * 128:(h + 1) * 128], pv[:, d, h:h + 1], None, ALU.mult, part=True)
                ps = nextA(g)
                for h in range(4):
                    kb.mm(ps[:, h * 128:(h + 1) * 128], k_tm[:, n, h * 128:(h + 1) * 128], vt[:, h * 128:(h + 1) * 128], start=True, stop=True)
                kb.tt(S2[:], S[:], g128[:, d, :], ALU.mult)
                kb.tt(S[:], S2[:], ps[:, :], ALU.add)
        A_l = [kb.sb("A_bf%d" % i, [128, 4, 128], BF16) for i in range(2)]
        o_l = [kb.sb("o_ret%d" % i, [128, 4, 128], F32) for i in range(2)]
        o2_l = [kb.sb("o_ret2_%d" % i, [128, 4, 128], F32) for i in range(2)]
        sq_l = [kb.sb("o_sq%d" % i, [128, 4, 128], F32) for i in range(2)]
        st_l = [kb.sb("gn_st%d" % i, [128, 6, 4], F32) for i in range(2)]
        yb_l = [kb.sb("y_ret%d" % i, [128, 512], BF16) for i in range(2)]
        gnw = kb.sb("gnw", [128, 512], F32)
        sgl = [kb.sb("sgl%d" % i, [128, 512], BF16) for i in range(2)]
        bcast_row(kb, gnw[:], I['ret_gn_w'][0, :])
        def _mk(n):
            def gen(s):
                tsl = slice(n * 128, (n + 1) * 128)
                A, o, o2, sq, st_, yb = A_l[s], o_l[s], o2_l[s], sq_l[s], st_l[s], yb_l[s]
                kb.dma(sgl[s][:], SGs[tsl, :])
                yield
                pS = nextA_s(g, s)
                for h in range(4):
                    kb.mm(pS[:, h * 128:(h + 1) * 128], kT[:, h, tsl], qT[:, h, tsl], start=True, stop=True)
                kb.tt(A[:], rr(pS[:, :], "p (h i) -> p h i", h=4), MT[:], ALU.mult)
                yield
                pI = nextA_s(g, s)
                pF = nextA_s(g, s)
                pB = nextA_s(g, s)
                for h in range(4):
                    hs = slice(h * 128, (h + 1) * 128)
                    kb.mm(pI[:, hs], A[:, h, :], v_tm[:, n, hs], start=True, stop=True)
                    kb.mm(pF[:, hs], qT[:, h, tsl], S_all[0][:, n, hs], start=True, stop=True)
                    kb.mm(pB[:, hs], qT[:, h, tsl], S_all[1][:, n, hs], start=True, stop=True)
                kb.copy(rr(o2[:], "p h e -> p (h e)"), pI[:, :], eng='act')
                yield
                for h in range(4):
                    hs = slice(h * 128, (h + 1) * 128)
                    kb.stt(o2[:, h, :], pF[:, hs], pv[:, 2, h:h + 1], o2[:, h, :], ALU.mult, ALU.add, part=True)
                    yield
                for h in range(4):
                    hs = slice(h * 128, (h + 1) * 128)
                    kb.stt(o[:, h, :], pB[:, hs], pv[:, 3, h:h + 1], o2[:, h, :], ALU.mult, ALU.add, part=True)
                    yield
                if n == 2:
                    dump(g, 'o_ret', o[:], [128, 4, 128], F32)
                kb.reduce(st_[:, 0, :], o[:], ALU.add)
                yield
                kb.tt(sq[:], o[:], o[:], ALU.mult)
                yield
                kb.reduce(st_[:, 1, :], sq[:], ALU.add)
                yield
                kb.ts(st_[:, 2, :], st_[:, 0, :], 1.0 / 128, None, ALU.mult)
                yield
                kb.tt(st_[:, 3, :], st_[:, 2, :], st_[:, 2, :], ALU.mult)
                yield
                kb.stt(st_[:, 4, :], st_[:, 1, :], 1.0 / 128, st_[:, 3, :], ALU.mult, ALU.subtract)
                yield
                kb.ts(st_[:, 4, :], st_[:, 4, :], EPS, None, ALU.add)
                yield
                kb.act(st_[:, 5, :], st_[:, 4, :], AF.Sqrt)
                yield
                kb.recip(st_[:, 3, :], st_[:, 5, :])
                yield
                for h in range(4):
                    kb.ts(sq[:, h, :], o[:, h, :], st_[:, 2, h:h + 1], st_[:, 3, h:h + 1], ALU.subtract, ALU.mult, part=True)
                    yield
                sqf = rr(sq[:], "p h e -> p (h e)")
                kb.tt(sqf, sqf, gnw[:], ALU.mult)
                yield
                kb.tt(yb[:], sqf, sgl[s][:], ALU.mult)
                yield
                yield from transpose_gen(g, s, mixT[:, 0:4, tsl], yb[:], 4, eng='act')
                yield
            return gen
        run_chains([_mk(n) for n in range(NT)], 2)


def fourier(g, FTs, mixT):
    kb = g.kb
    I = g.I
    with kb.phase():
        cs = kb.sb("dftc", [128, 256], BF16)
        kb.dma(cs[:], I['dft_c'][:])
        fc = kb.sb("fc", [128, NT, 512], BF16)
        fs = kb.sb("fs", [128, NT, 512], BF16)
        ftl = [kb.sb("ftl%d" % i, [128, 4, 128], BF16) for i in range(2)]
        for n in range(NT):
            tsl = slice(n * 128, (n + 1) * 128)
            fT = ftl[n % 2]
            kb.dma(fT[:], rr(FTs[:, :, tsl], "g c t -> c g t"))
            for gp in range(2):
                ps = nextA(g)
                for gg in range(2):
                    gi = gp * 2 + gg
                    kb.mm(ps[:, gg * 256:(gg + 1) * 256], fT[:, gi, :], cs[:], start=True, stop=True)
                p3 = rr(ps[:, :], "p (g two c) -> p g two c", g=2, two=2)
                kb.copy(rr(fc[:, n, gp * 256:(gp + 1) * 256], "p (g c) -> p g c", g=2), View(ps, p3.ap[:, :, 0, :]), eng='act', part=True)
                kb.copy(rr(fs[:, n, gp * 256:(gp + 1) * 256], "p (g c) -> p g c", g=2), View(ps, p3.ap[:, :, 1, :]), eng='dve', part=True)
        tcc = kb.sb("tcc", [128, 2, 256], BF16)
        tcs = kb.sb("tcs", [128, 2, 256], BF16)
        kb.dma(tcc[:], rr(I['dft_tc_c'][:, :], "(k p) c -> p k c", p=128))
        kb.dma(tcs[:], rr(I['dft_tc_s'][:, :], "(k p) c -> p k c", p=128))
        for gi in range(4):
            gs = slice(gi * 128, (gi + 1) * 128)
            ps = nextA(g)
            for tc in range(2):
                kb.mm(ps[:, 0:256], fc[:, tc, gs], tcc[:, tc, :], start=(tc == 0), stop=False)
                kb.mm(ps[:, 0:256], fs[:, tc, gs], tcs[:, tc, :], start=False, stop=(tc == 1))
            kb.copy(mixT[:, 4 + gi, 0:256], ps[:, 0:256], eng='act', part=True)
        tlc = kb.sb("tlc", [128, 16, 512], BF16)
        tls = kb.sb("tls", [128, 16, 512], BF16)
        for tb in range(4):
            kb.dma(tlc[:], rr(I['dft_tl_c'][:, tb * 512:(tb + 1) * 512], "(k p) c -> p k c", p=128))
            kb.dma(tls[:], rr(I['dft_tl_s'][:, tb * 512:(tb + 1) * 512], "(k p) c -> p k c", p=128))
            for gi in range(4):
                gs = slice(gi * 128, (gi + 1) * 128)
                ps = nextA(g)
                for tc in range(16):
                    kb.mm(ps[:, :], fc[:, 2 + tc, gs], tlc[:, tc, :], start=(tc == 0), stop=False)
                    kb.mm(ps[:, :], fs[:, 2 + tc, gs], tls[:, tc, :], start=False, stop=(tc == 15))
                kb.copy(mixT[:, 4 + gi, 256 + tb * 512:256 + (tb + 1) * 512], ps[:, :], eng='act', part=True)


def prep_shared(inputs):
    sh = {}
    f = lambda a: np.ascontiguousarray(np.asarray(a, dtype=np.float32))
    for k in ('norm1_w', 'norm2_w', 'ada_w', 'ada_b', 'even_w_in', 'ret_gn_w', 'even_w_out',
              'moe_w_gate', 'moe_w_up', 'moe_w_down', 'final_norm_w'):
        if k in inputs:
            sh[k] = f(inputs[k])
    sh['ret_decay_logit'] = f(inputs['ret_decay_logit']).reshape(1, 8)
    if 'odd_w_in' in inputs:
        sh['odd_w_in'] = f(inputs['odd_w_in'])
        sh['odd_w_out'] = f(inputs['odd_w_out'])
        sh['cw'] = f(np.asarray(inputs['dn_conv_w'])[0].reshape(3, 12, 128).transpose(2, 1, 0))
        sh['dn_a_log'] = f(inputs['dn_a_log']).reshape(1, 8)
        sh['dn_dt_bias'] = f(inputs['dn_dt_bias']).reshape(1, 8)
        sh['dn_norm_w'] = f(inputs['dn_norm_w'])
        sh['wsT'] = f(np.asarray(inputs['sgu_w'])[0].transpose(2, 0, 1))
        sh['sgb'] = f(np.asarray(inputs['sgu_b'])[0].T)
    sh['router_w'] = f(np.concatenate([inputs['router_g_w'], inputs['router_e_w']], -1))
    sh['router_b'] = f(np.concatenate([inputs['router_g_b'], inputs['router_e_b']], -1))
    sh.update(host_consts())
    return sh


def prep_core(inputs, sh, b, used):
    m = {}
    for k in used:
        if k == 'x_all':
            m[k] = np.ascontiguousarray(np.concatenate([inputs['ctx'][b], inputs['x'][b]], 0).astype(np.float32))
        elif k == 'c2t':
            c2 = np.stack([inputs['c'][b], inputs['c_ctx']], 0).astype(np.float32)
            m[k] = np.ascontiguousarray(c2.reshape(2, 8, 128).transpose(2, 0, 1).reshape(128, 16))
        else:
            m[k] = sh[k]
    return m


def ubc(v, axis, shape):
    return View(v.tile, v.ap.unsqueeze(axis).to_broadcast(list(shape)))


def moe_stage(g, L, last):
    kb = g.kb
    I = g.I
    tiles = list(range(NTC, NT)) if last else list(range(NT))
    blocks = [(256, 512), (768, 512), (1280, 512), (1792, 512)] if last else \
             [(0, 512), (512, 512), (1024, 512), (1536, 512), (2048, 256)]
    with kb.phase():
        modt = [kb.sb("modm%d" % r, [128, 3072], F32) for r in range(2)]
        compute_mod(g, L, 1, modt)
        h2T = kb.sb("h2T", [128, 8, TT], BF16)
        Gall = kb.sb("Gall", [128, NT, 32], F32)
        acc = {n: kb.sb("acc%d" % n, [128, D], F32) for n in tiles}
        with kb.phase():
            rw = kb.sb("rw", [128, 8, 36], F32)
            kb.dma(rw[:], rr(I['router_w'][L, :, :], "(k p) c -> p k c", p=128))
            rb = kb.sb("rbias", [128, 36], F32)
            bcast_row(kb, rb[:], I['router_b'][L, :])
            xt = [kb.sb("xm%d" % i, [128, D], F32) for i in range(2)]
            tmp = kb.sb("tmpm", [128, D], F32)
            hf = kb.sb("hf", [128, D], F32)
            hb = kb.sb("hbm", [128, D], BF16)
            hTf = kb.sb("hTf", [128, 8, 128], F32)
            ss = kb.sb("ssm", [128, 4], F32)
            lg = kb.sb("lgt", [128, 36], F32)
            s8 = kb.sb("s8", [128, 16], F32)
            oh4 = kb.sb("oh4", [128, 4], F32)
            t48 = kb.sb("t48", [128, 4, 8], F32)
            sel = kb.sb("sel8", [128, 8], F32)
            msk = kb.sb("msk8", [128, 8], F32)
            oh1 = kb.sb("oh1", [128, 8], F32)
            oh2 = kb.sb("oh2", [128, 8], F32)
            G8 = kb.sb("G8", [128, 8], F32)
            e4 = kb.sb("e4", [128, 4], F32)
            for n in tiles:
                x_ = xt[n % 2]
                kb.dma(x_[:], g.X[n * 128:(n + 1) * 128, :])
                norm_mod_tile(g, x_[:], modt[0 if n >= NTC else 1], hf[:], ss, tmp)
                kb.copy(hb[:], hf[:], eng='act')
                transpose_into(g, h2T[:, :, n * 128:(n + 1) * 128], hb[:], 8)
                for half in range(2):
                    pt = nextA(g)
                    for k in range(4):
                        kk = half * 4 + k
                        kb.transpose(pt[:, k * 128:(k + 1) * 128], hf[:, kk * 128:(kk + 1) * 128], g.identf[:])
                    kb.copy(hTf[:, half * 4:(half + 1) * 4, :], rr(pt[:, :], "p (k t) -> p k t", k=4), eng='act', part=True)
                ps = nextA(g)
                for k in range(8):
                    kb.mm(ps[:, 0:36], hTf[:, k, :], rw[:, k, :], start=(k == 0), stop=(k == 7))
                kb.tt(lg[:], ps[:, 0:36], rb[:], ALU.add)
                kb.reduce(s8[:, 0:1], lg[:, 0:4], ALU.max)
                kb.ts(oh4[:], lg[:, 0:4], s8[:, 0:1], None, ALU.is_ge)
                kb.ts(s8[:, 1:2], s8[:, 0:1], -1.0, None, ALU.mult)
                kb.memset(s8[:, 2:3], 0.0)
                kb.act(e4[:], lg[:, 0:4], AF.Exp, bias=s8[:, 1:2], accum=s8[:, 2:3])
                kb.recip(s8[:, 3:4], s8[:, 2:3])
                kb.tt(t48[:], rr(lg[:, 4:36], "p (g e) -> p g e", g=4), ubc(oh4[:], 2, [128, 4, 8]), ALU.mult)
                kb.reduce(sel[:], rr(t48[:], "p g e -> p e g"), ALU.add)
                kb.reduce(s8[:, 4:5], sel[:], ALU.max)
                kb.ts(oh1[:], sel[:], s8[:, 4:5], None, ALU.is_ge)
                kb.stt(msk[:], oh1[:], -1e30, sel[:], ALU.mult, ALU.add)
                kb.reduce(s8[:, 5:6], msk[:], ALU.max)
                kb.ts(oh2[:], msk[:], s8[:, 5:6], None, ALU.is_ge)
                kb.tt(s8[:, 6:7], s8[:, 5:6], s8[:, 4:5], ALU.subtract)
                kb.act(s8[:, 7:8], s8[:, 6:7], AF.Exp)
                kb.ts(s8[:, 8:9], s8[:, 7:8], 1.0, None, ALU.add)
                kb.recip(s8[:, 9:10], s8[:, 8:9])
                kb.tt(s8[:, 10:11], s8[:, 9:10], s8[:, 3:4], ALU.mult)
                kb.tt(s8[:, 11:12], s8[:, 10:11], s8[:, 7:8], ALU.mult)
                kb.ts(G8[:], oh1[:], s8[:, 10:11], None, ALU.mult)
                kb.stt(G8[:], oh2[:], s8[:, 11:12], G8[:], ALU.mult, ALU.add)
                kb.tt(rr(Gall[:, n, :], "p (g e) -> p g e", g=4), ubc(G8[:], 1, [128, 4, 8]), ubc(oh4[:], 2, [128, 4, 8]), ALU.mult, part=True)
                kb.memset(acc[n][:], 0.0, eng='pool')
        dump(g, 'Gall', Gall[:, 2, :], [128, 32], F32)
        with kb.phase():
            wg = [kb.sb("wg%d" % i, [128, 8, 512], BF16) for i in range(2)]
            wu = [kb.sb("wu%d" % i, [128, 8, 512], BF16) for i in range(2)]
            wd = [kb.sb("wd%d" % i, [128, 4, D], BF16) for i in range(2)]
            actT = [kb.sb("actT%d" % i, [128, 4, 512], BF16) for i in range(2)]
            sgm = [kb.sb("sgm%d" % i, [128, 512], BF16) for i in range(2)]
            cnt = 0
            for e in range(32):
                wg_, wu_, wd_ = wg[e % 2], wu[e % 2], wd[e % 2]
                kb.dma(wg_[:], rr(I['moe_w_gate'][L, e, :, :], "(k p) c -> p k c", p=128), eng='pool')
                kb.dma(wu_[:], rr(I['moe_w_up'][L, e, :, :], "(k p) c -> p k c", p=128), eng='pool')
                kb.dma(wd_[:], rr(I['moe_w_down'][L, e, :, :], "(k p) c -> p k c", p=128), eng='pool')
                for bi, (t0, tl) in enumerate(blocks):
                    aT = actT[bi % 2]
                    for hc in range(4):
                        hs = slice(hc * 128, (hc + 1) * 128)
                        pg_ = nextA(g)
                        for k in range(8):
                            kb.mm(pg_[:, 0:tl], wg_[:, k, hs], h2T[:, k, t0:t0 + tl], start=(k == 0), stop=(k == 7))
                        pu_ = nextA(g)
                        for k in range(8):
                            kb.mm(pu_[:, 0:tl], wu_[:, k, hs], h2T[:, k, t0:t0 + tl], start=(k == 0), stop=(k == 7))
                        sg_ = sgm[cnt % 2]
                        cnt += 1
                        kb.act(sg_[:, 0:tl], pg_[:, 0:tl], AF.Silu)
                        kb.tt(aT[:, hc, 0:tl], sg_[:, 0:tl], pu_[:, 0:tl], ALU.mult, part=True)
                    for tsi in range(tl // 128):
                        n = t0 // 128 + tsi
                        for dh in range(2):
                            py = nextA(g)
                            for hc in range(4):
                                kb.mm(py[:, :], aT[:, hc, tsi * 128:(tsi + 1) * 128], wd_[:, hc, dh * 512:(dh + 1) * 512], start=(hc == 0), stop=(hc == 3))
                            a_ = acc[n][:, dh * 512:(dh + 1) * 512]
                            kb.stt(a_, py[:, :], Gall[:, n, e:e + 1], a_, ALU.mult, ALU.add)
        with kb.phase():
            xt = [kb.sb("xf%d" % i, [128, D], F32) for i in range(2)]
            tmp = kb.sb("tmpf", [128, D], F32)
            ss = kb.sb("ssf", [128, 4], F32)
            if last:
                fnw = kb.sb("fnw", [128, D], F32)
                bcast_row(kb, fnw[:], I['final_norm_w'][:])
            for n in tiles:
                x_ = xt[n % 2]
                kb.dma(x_[:], g.X[n * 128:(n + 1) * 128, :])
                mr = modt[0 if n >= NTC else 1]
                kb.tt(tmp[:], acc[n][:], mr[:, 2048:3072], ALU.mult)
                kb.tt(x_[:], x_[:], tmp[:], ALU.add)
                if not last:
                    kb.dma(g.X[n * 128:(n + 1) * 128, :], x_[:])
                else:
                    kb.memset(ss[:, 0:1], 0.0)
                    kb.act(tmp[:], x_[:], AF.Square, accum=ss[:, 0:1])
                    kb.ts(ss[:, 1:2], ss[:, 0:1], 1.0 / D, EPS, ALU.mult, ALU.add)
                    kb.act(ss[:, 2:3], ss[:, 1:2], AF.Sqrt)
                    kb.recip(ss[:, 3:4], ss[:, 2:3])
                    kb.stt(x_[:], x_[:], ss[:, 3:4], fnw[:], ALU.mult, ALU.mult)
                    kb.dma(g.out[(n - NTC) * 128:(n - NTC + 1) * 128, :], x_[:], is_output=True)


IN_SPECS.update({
    'odd_w_in': ([1, D, 3088], F32), 'cw': ([128, 12, 3], F32), 'dn_a_log': ([1, 8], F32), 'dn_dt_bias': ([1, 8], F32),
    'dn_norm_w': ([1, 128], F32), 'wsT': ([128, 4, 128], F32), 'sgb': ([128, 4], F32), 'odd_w_out': ([1, D, D], F32),
})
CONST_SPECS.update({'lowS': ([128, 128], F32), 'upS': ([128, 128], F32), 'onesf': ([128, 128], F32)})


def layer1(g):
    kb = g.kb
    I = g.I
    L = 1
    with kb.phase():
        MIXs = kb.dram("MIXs1", [8, 128, TT], BF16)
        g1k = kb.sb("g1k", [128, D], F32)
        SGs = kb.dram("SGs1", [TT, 512], BF16)
        with kb.phase():
            qT = kb.sb("qT1", [128, 4, TT], BF16)
            kT = kb.sb("kT1", [128, 4, TT], BF16)
            k_tm = kb.sb("k_tm1", [128, NT, 512], BF16)
            v_tm = kb.sb("v_tm1", [128, NT, 512], BF16)
            glog = kb.sb("glog", [128, NT, 8], F32)
            beta = kb.sb("beta", [128, NT, 8], F32)
            hph = kb.phase()
            hph.__enter__()
            hT = kb.sb("hT1", [128, 8, TT], BF16)
            modt = [kb.sb("modt1_%d" % r, [128, 3072], F32) for r in range(2)]
            compute_mod(g, L, 0, modt)
            kb.copy(g1k[:], modt[0][:, 2048:3072], eng='pool')
            with kb.phase():
                xt = [kb.sb("xt1_%d" % i, [128, D], F32) for i in range(2)]
                tmp = [kb.sb("tmp1_%d" % i, [128, D], F32) for i in range(2)]
                hb_l = [kb.sb("hb1_%d" % i, [128, D], BF16) for i in range(2)]
                ss = [kb.sb("ss1_%d" % i, [128, 4], F32) for i in range(2)]
                def _mk(n):
                    def gen(s):
                        x_ = xt[s]
                        kb.dma(x_[:], g.X[n * 128:(n + 1) * 128, :])
                        yield
                        hb = hb_l[s]
                        yield from norm_mod_gen(g, x_[:], modt[0 if n >= NTC else 1], hb[:], ss[s], tmp[s])
                        yield from transpose_gen(g, s, hT[:, :, n * 128:(n + 1) * 128], hb[:], 8)
                        yield
                    return gen
                run_chains([_mk(n) for n in range(NT)], 2)
            with kb.phase():
                wb = [kb.sb("w1b%d" % i, [128, 8, 512], BF16) for i in range(2)]
                cw = kb.sb("cw", [128, 12, 3], F32)
                kb.dma(cw[:], I['cw'][:])
                onesf = kb.sb("onesf", [128, 128], F32)
                kb.dma(onesf[:], I['onesf'][:])
                raw2 = [kb.sb("raw%d" % i, [128, TT], F32) for i in range(2)]
                y2 = [kb.sb("yconv%d" % i, [128, TT], F32) for i in range(2)]
                sq = kb.sb("sqc", [128, TT], F32)
                vb = kb.sb("vbf", [128, TT], BF16)
                for cb in range(3):
                    w = wb[cb % 2]
                    kb.dma(w[:], rr(I['odd_w_in'][0, :, cb * 512:(cb + 1) * 512], "(k p) c -> p k c", p=128), eng='pool')
                    for hh in range(4):
                        ch = cb * 4 + hh
                        raw, y = raw2[ch % 2], y2[ch % 2]
                        for (t0, tl) in TBLK:
                            ps = nextA(g)
                            for k in range(8):
                                kb.mm(ps[:, 0:tl], w[:, k, hh * 128:(hh + 1) * 128], hT[:, k, t0:t0 + tl], start=(k == 0), stop=(k == 7))
                            kb.copy(raw[:, t0:t0 + tl], ps[:, 0:tl], eng='act', part=True)
                        kb.ts(y[:], raw[:], cw[:, ch, 1:2], None, ALU.mult)
                        for (a, b_) in ((1, 256), (257, TT)):
                            kb.stt(y[:, a:b_], raw[:, a - 1:b_ - 1], cw[:, ch, 0:1], y[:, a:b_], ALU.mult, ALU.add)
                        for (a, b_) in ((0, 255), (256, TT - 1)):
                            kb.stt(y[:, a:b_], raw[:, a + 1:b_ + 1], cw[:, ch, 2:3], y[:, a:b_], ALU.mult, ALU.add)
                        kb.act(y[:], y[:], AF.Silu)
                        if cb < 2:
                            kb.tt(sq[:], y[:], y[:], ALU.mult)
                            for (t0, tl) in TBLK:
                                ps = nextA(g)
                                kb.mm(ps[:, 0:tl], onesf[:], sq[:, t0:t0 + tl], start=True, stop=True)
                                kb.ts(raw[:, t0:t0 + tl], ps[:, 0:tl], EPS, None, ALU.add, part=True)
                            kb.act(raw[:], raw[:], AF.Sqrt)
                            kb.recip(raw[:], raw[:])
                            dstT = qT if cb == 0 else kT
                            if cb == 0:
                                kb.stt(dstT[:, hh, :], y[:], SCALE, raw[:], ALU.mult, ALU.mult, part=True)
                            else:
                                kb.tt(dstT[:, hh, :], y[:], raw[:], ALU.mult, part=True)
                        else:
                            kb.copy(vb[:], y[:], eng='act')
                        if cb >= 1:
                            srcT = kT[:, hh, :] if cb == 1 else vb[:]
                            dst_tm = k_tm if cb == 1 else v_tm
                            for n0 in range(0, NT, 6):
                                pt = nextT(g)
                                for q in range(6):
                                    n = n0 + q
                                    kb.transpose(pt[:, q * 128:(q + 1) * 128], View(srcT.tile, srcT.ap[:, n * 128:(n + 1) * 128]), g.ident[:])
                                kb.copy(dst_tm[:, n0:n0 + 6, hh * 128:(hh + 1) * 128], rr(pt[:, 0:768], "p (n e) -> p n e", n=6), eng='dve', part=True)
            with kb.phase():
                wz = kb.sb("wz", [128, 8, 512], BF16)
                wab = kb.sb("wab", [128, 8, 16], BF16)
                wu = kb.sb("wu1", [128, 8, 512], BF16)
                wsx = kb.sb("ws1", [128, 8, 512], BF16)
                kb.dma(wz[:], rr(I['odd_w_in'][0, :, 1536:2048], "(k p) c -> p k c", p=128), eng='pool')
                kb.dma(wab[:], rr(I['odd_w_in'][0, :, 2048:2064], "(k p) c -> p k c", p=128), eng='pool')
                kb.dma(wu[:], rr(I['odd_w_in'][0, :, 2064:2576], "(k p) c -> p k c", p=128), eng='pool')
                kb.dma(wsx[:], rr(I['odd_w_in'][0, :, 2576:3088], "(k p) c -> p k c", p=128), eng='pool')
                alog = kb.sb("alog", [128, 8], F32)
                dtb = kb.sb("dtb", [128, 8], F32)
                bcast_row(kb, alog[:], I['dn_a_log'][0, :])
                bcast_row(kb, dtb[:], I['dn_dt_bias'][0, :])
                kb.act(alog[:], alog[:], AF.Exp)
                kb.ts(alog[:], alog[:], -1.0, None, ALU.mult)
                wsT = kb.sb("wsT", [128, 4, 128], F32)
                wsTb = kb.sb("wsTb", [128, 4, 128], BF16)
                kb.dma(wsT[:], I['wsT'][:])
                kb.copy(wsTb[:], wsT[:], eng='act')
                sgb = kb.sb("sgb", [128, 4], F32)
                kb.dma(sgb[:], I['sgb'][:])
                sgt = [kb.sb("sgt1_%d" % i, [128, 512], BF16) for i in range(2)]
                ab_l = [kb.sb("ab%d" % i, [128, 16], F32) for i in range(2)]
                ug_l = [kb.sb("ug%d" % i, [128, 4, 128], F32) for i in range(2)]
                sgl_l = [kb.sb("sgl1_%d" % i, [128, 4, 128], F32) for i in range(2)]
                sq2_l = [kb.sb("sq2_%d" % i, [128, 4, 128], F32) for i in range(2)]
                vn_l = [kb.sb("vn%d" % i, [128, 4, 128], BF16) for i in range(2)]
                st_l = [kb.sb("sgu_st%d" % i, [128, 6, 4], F32) for i in range(2)]
                ob_l = [kb.sb("sgu_ob%d" % i, [128, 512], BF16) for i in range(2)]
                sgo = [kb.sb("sgo%d" % i, [128, 4, 128], BF16) for i in range(2)]
                def _mk(n):
                    def gen(s):
                        tsl = slice(n * 128, (n + 1) * 128)
                        ab, ug, sgl, sq2, vn, st_, ob = ab_l[s], ug_l[s], sgl_l[s], sq2_l[s], vn_l[s], st_l[s], ob_l[s]
                        ps = nextA_s(g, s)
                        for k in range(8):
                            kb.mm(ps[:, 0:16], hT[:, k, tsl], wab[:, k, :], start=(k == 0), stop=(k == 7))
                        kb.copy(ab[:], ps[:, 0:16], eng='act')
                        yield
                        kb.act(beta[:, n, :], ab[:, 8:16], AF.Sigmoid, part=True)
                        yield
                        kb.tt(ab[:, 0:8], ab[:, 0:8], dtb[:], ALU.add)
                        yield
                        kb.act(ab[:, 0:8], ab[:, 0:8], AF.Exp)
                        yield
                        kb.act(ab[:, 0:8], ab[:, 0:8], AF.Ln, bias=1.0)
                        yield
                        kb.tt(glog[:, n, :], ab[:, 0:8], alog[:], ALU.mult, part=True)
                        yield
                        if n < NTC:
                            return
                        ps = nextA_s(g, s)
                        for k in range(8):
                            kb.mm(ps[:, :], hT[:, k, tsl], wz[:, k, :], start=(k == 0), stop=(k == 7))
                        kb.act(sgt[s][:], ps[:, :], AF.Silu)
                        yield
                        kb.dma(SGs[tsl, :], sgt[s][:], part=True)
                        yield
                        pu = nextA_s(g, s)
                        for k in range(8):
                            kb.mm(pu[:, :], hT[:, k, tsl], wu[:, k, :], start=(k == 0), stop=(k == 7))
                        kb.act(rr(ug[:], "p g c -> p (g c)"), pu[:, :], AF.Gelu)
                        yield
                        pv_ = nextA_s(g, s)
                        for k in range(8):
                            kb.mm(pv_[:, :], hT[:, k, tsl], wsx[:, k, :], start=(k == 0), stop=(k == 7))
                        kb.act(rr(sgl[:], "p g c -> p (g c)"), pv_[:, :], AF.Gelu)
                        yield
                        kb.reduce(st_[:, 0, :], sgl[:], ALU.add)
                        yield
                        kb.tt(sq2[:], sgl[:], sgl[:], ALU.mult)
                        yield
                        kb.reduce(st_[:, 1, :], sq2[:], ALU.add)
                        yield
                        kb.ts(st_[:, 2, :], st_[:, 0, :], 1.0 / 128, None, ALU.mult)
                        yield
                        kb.tt(st_[:, 3, :], st_[:, 2, :], st_[:, 2, :], ALU.mult)
                        yield
                        kb.stt(st_[:, 4, :], st_[:, 1, :], 1.0 / 128, st_[:, 3, :], ALU.mult, ALU.subtract)
                        yield
                        kb.ts(st_[:, 4, :], st_[:, 4, :], EPS, None, ALU.add)
                        yield
                        kb.act(st_[:, 5, :], st_[:, 4, :], AF.Sqrt)
                        yield
                        kb.recip(st_[:, 3, :], st_[:, 5, :])
                        yield
                        for gi in range(4):
                            kb.ts(vn[:, gi, :], sgl[:, gi, :], st_[:, 2, gi:gi + 1], st_[:, 3, gi:gi + 1], ALU.subtract, ALU.mult, part=True)
                            yield
                        pm = nextA_s(g, s)
                        for gi in range(4):
                            kb.mm(pm[:, gi * 128:(gi + 1) * 128], wsTb[:, gi, :], vn[:, gi, :], start=True, stop=True)
                        for gi in range(4):
                            kb.stt(ob[:, gi * 128:(gi + 1) * 128], pm[:, gi * 128:(gi + 1) * 128], sgb[:, gi:gi + 1], ug[:, gi, :], ALU.add, ALU.mult, part=True)
                            yield
                        so_ = sgo[s]
                        yield from transpose_gen(g, s, so_[:], ob[:], 4, eng='act')
                        kb.dma(rr(MIXs[4:8, :, tsl], "k c t -> c k t"), so_[:], part=True)
                        yield
                        yield
                    return gen
                run_chains([_mk(n) for n in range(NT)], 2)
            hph.__exit__(None, None, None)
            deltanet2(g, qT, kT, k_tm, v_tm, glog, beta, SGs, MIXs)
        with kb.phase():
            wo = kb.sb("wo1", [128, 8, D], BF16)
            kb.dma(wo[:], rr(I['odd_w_out'][0, :, :], "(k p) c -> p k c", p=128), eng='pool')
            xt = [kb.sb("xt14_%d" % i, [128, D], F32) for i in range(2)]
            yt_l = [kb.sb("yt14_%d" % i, [128, D], F32) for i in range(2)]
            mxl = [kb.sb("mxl%d" % i, [128, 8, 128], BF16) for i in range(2)]
            def _mk(n):
                def gen(s):
                    tsl = slice(n * 128, (n + 1) * 128)
                    x_ = xt[s]
                    kb.dma(x_[:], g.X[tsl, :])
                    yield
                    mx = mxl[s]
                    kb.dma(mx[:], rr(MIXs[:, :, tsl], "k c t -> c k t"))
                    yield
                    for dh in range(2):
                        ps = nextA_s(g, s)
                        for k in range(8):
                            kb.mm(ps[:, :], mx[:, k, :], wo[:, k, dh * 512:(dh + 1) * 512], start=(k == 0), stop=(k == 7))
                        kb.tt(yt_l[s][:, dh * 512:(dh + 1) * 512], ps[:, :], g1k[:, dh * 512:(dh + 1) * 512], ALU.mult, part=True)
                        yield
                    kb.tt(x_[:], x_[:], yt_l[s][:], ALU.add)
                    yield
                    kb.dma(g.X[tsl, :], x_[:])
                    yield
                    yield
                return gen
            run_chains([_mk(n) for n in range(NTC, NT)], 2)


def deltanet(g, qT, kT, k_tm, v_tm, glog, beta, SGs, mixT):
    kb = g.kb
    I = g.I
    with kb.phase():
        cm = {nm: kb.sb("dc_" + nm, [128, 128], F32) for nm in ('indf', 'indb', 'lowS', 'upS')}
        for nm in cm:
            kb.dma(cm[nm][:], I[nm][:])
        identf = g.identf
        o_acc = kb.sb("o_acc", [128, NTL, 512], F32)
        F = lambda nm: kb.sb(nm, [128, 128], F32)
        B = lambda nm: kb.sb(nm, [128, 128], BF16)
        grep, grow, t1, dec, decS, Lm, Nm, decT = F("grep"), F("grow"), F("t1"), F("dec"), F("decS"), F("Lm"), F("Nm"), F("decT")
        X = [F("X0"), F("X1")]
        Y = [F("Y0"), F("Y1")]
        P = [F("P0"), F("P1")]
        PT = [F("PT0"), F("PT1")]
        TTb, rhsV, rhsK, wTb, vnew, attnT, ktail, Sb = B("TTb"), B("rhsV"), B("rhsK"), B("wTb"), B("vnew"), B("attnT"), B("ktail"), B("Sb")
        u_sb, otmp = F("u_sb"), F("otmp")
        gcol = kb.sb("gcol", [128, 8], F32)
        eg = kb.sb("eg", [128, 8], F32)
        sc = kb.sb("dsc", [128, 8], F32)
        S = [[kb.sb("S%d_%d" % (d, h), [128, 128], F32) for h in range(4)] for d in range(2)]
        for d in range(2):
            order = list(range(NT)) if d == 0 else [1, 0] + list(range(NT - 1, 1, -1))
            mI = cm['indb'] if d == 0 else cm['indf']
            mS = cm['lowS'] if d == 0 else cm['upS']
            U = cm['indf'] if d == 0 else cm['indb']
            li = 127 if d == 0 else 0
            for h in range(4):
                kb.memset(S[d][h][:], 0.0)
            for idx, n in enumerate(order):
                tsl = slice(n * 128, (n + 1) * 128)
                is_out = n >= NTC
                last_chunk = idx == NT - 1
                pg = nextA(g)
                kb.mm(pg[:, 0:4], U[:], glog[:, n, d * 4:(d + 1) * 4], start=True, stop=True)
                kb.copy(gcol[:, 0:4], pg[:, 0:4], eng='act')
                kb.act(eg[:, 0:4], gcol[:, 0:4], AF.Exp)
                for h in range(4):
                    c = d * 4 + h
                    hs = slice(h * 128, (h + 1) * 128)
                    kb.copy(grep[:], bc(glog[:, n, c:c + 1], [128, 128]))
                    p1 = nextA(g)
                    kb.mm(p1[:, 0:128], grep[:], U[:], start=True, stop=True)
                    kb.copy(grow[:], p1[:, 0:128], eng='act')
                    kb.ts(t1[:], grow[:], gcol[:, h:h + 1], 0.0, ALU.subtract, ALU.max)
                    kb.act(t1[:], t1[:], AF.Exp, scale=-1.0)
                    kb.tt(dec[:], t1[:], mI[:], ALU.mult, eng='pool')
                    kb.tt(decS[:], t1[:], mS[:], ALU.mult)
                    p2 = nextA(g)
                    kb.mm(p2[:, 0:128], kT[:, h, tsl], kT[:, h, tsl], start=True, stop=True)
                    kb.stt(Lm[:], p2[:, 0:128], beta[:, n, c:c + 1], decS[:], ALU.mult, ALU.mult)
                    p3 = nextA(g)
                    kb.transpose(p3[:, 0:128], Lm[:], identf[:])
                    kb.transpose(p3[:, 128:256], dec[:], identf[:])
                    kb.copy(Nm[:], p3[:, 0:128], eng='act')
                    kb.copy(decT[:], p3[:, 128:256], eng='act')
                    kb.tt(P[0][:], identf[:], Lm[:], ALU.subtract)
                    kb.tt(PT[0][:], identf[:], Nm[:], ALU.subtract, eng='pool')
                    Xc, Yc = Lm, Nm
                    pi = 0
                    for m in range(6):
                        lastm = m == 5
                        pa = nextA(g)
                        kb.mm(pa[:, 0:128], Xc[:], Yc[:], start=True, stop=True)
                        if not lastm:
                            kb.mm(pa[:, 128:256], Yc[:], Xc[:], start=True, stop=True)
                        Yn, Xn = Y[m % 2], X[m % 2]
                        kb.copy(Yn[:], pa[:, 0:128], eng='act')
                        if not lastm:
                            kb.copy(Xn[:], pa[:, 128:256], eng='act')
                        pb_ = nextA(g)
                        kb.mm(pb_[:, 0:128], P[pi][:], Yn[:], start=True, stop=True)
                        if not lastm:
                            kb.mm(pb_[:, 128:256], PT[pi][:], Xn[:], start=True, stop=True)
                        kb.tt(PT[1 - pi][:], PT[pi][:], pb_[:, 0:128], ALU.add)
                        if not lastm:
                            kb.tt(P[1 - pi][:], P[pi][:], pb_[:, 128:256], ALU.add)
                        pi = 1 - pi
                        Xc, Yc = Xn, Yn
                    kb.copy(TTb[:], PT[pi][:], eng='act')
                    kb.ts(rhsV[:], v_tm[:, n, hs], beta[:, n, c:c + 1], None, ALU.mult)
                    kb.tt(sc[:, 0:1], beta[:, n, c:c + 1], eg[:, h:h + 1], ALU.mult)
                    kb.ts(rhsK[:], k_tm[:, n, hs], sc[:, 0:1], None, ALU.mult)
                    p4 = nextA(g)
                    kb.mm(p4[:, 0:128], TTb[:], rhsV[:], start=True, stop=True)
                    kb.mm(p4[:, 128:256], rhsK[:], TTb[:], start=True, stop=True)
                    kb.copy(u_sb[:], p4[:, 0:128], eng='act')
                    kb.copy(wTb[:], p4[:, 128:256], eng='act')
                    kb.copy(Sb[:], S[d][h][:], eng='act')
                    p5 = nextA(g)
                    kb.mm(p5[:, 0:128], wTb[:], Sb[:], start=True, stop=True)
                    kb.tt(vnew[:], u_sb[:], p5[:, 0:128], ALU.subtract)
                    if is_out:
                        p6 = nextA(g)
                        kb.mm(p6[:, 0:128], kT[:, h, tsl], qT[:, h, tsl], start=True, stop=True)
                        kb.tt(attnT[:], p6[:, 0:128], decT[:], ALU.mult)
                        p7 = nextA(g)
                        kb.mm(p7[:, 0:128], attnT[:], vnew[:], start=True, stop=True)
                        kb.mm(p7[:, 128:256], qT[:, h, tsl], Sb[:], start=True, stop=True)
                        kb.copy(otmp[:], p7[:, 0:128], eng='act')
                        kb.stt(otmp[:], p7[:, 128:256], eg[:, h:h + 1], otmp[:], ALU.mult, ALU.add)
                        oa = o_acc[:, n - NTC, hs]
                        if d == 0:
                            kb.copy(oa, otmp[:], eng='pool', part=True)
                        else:
                            kb.tt(oa, oa, otmp[:], ALU.add, eng='pool')
                    if not last_chunk:
                        kb.act(sc[:, 1:2], gcol[:, h:h + 1], AF.Exp, scale=-1.0, bias=grow[:, li:li + 1])
                        kb.ts(ktail[:], k_tm[:, n, hs], sc[:, 1:2], None, ALU.mult)
                        kb.act(sc[:, 2:3], grow[:, li:li + 1], AF.Exp)
                        p8 = nextA(g)
                        kb.mm(p8[:, 0:128], ktail[:], vnew[:], start=True, stop=True)
                        kb.stt(S[d][h][:], S[d][h][:], sc[:, 2:3], p8[:, 0:128], ALU.mult, ALU.add)
        nwb = kb.sb("dnw", [128, 128], F32)
        bcast_row(kb, nwb[:], I['dn_norm_w'][0, :])
        sq = kb.sb("dsq", [128, 4, 128], F32)
        st_ = kb.sb("dst", [128, 3, 4], F32)
        yb = kb.sb("dyb", [128, 512], BF16)
        zl = [kb.sb("zl%d" % i, [128, 512], BF16) for i in range(2)]
        for nl in range(NTL):
            n = nl + NTC
            tsl = slice(n * 128, (n + 1) * 128)
            kb.dma(zl[nl % 2][:], SGs[tsl, :])
            o3 = rr(o_acc[:, nl, :], "p (h e) -> p h e", h=4)
            kb.tt(sq[:], o3, o3, ALU.mult)
            kb.reduce(st_[:, 0, :], sq[:], ALU.add)
            kb.ts(st_[:, 1, :], st_[:, 0, :], 1.0 / 128, EPS, ALU.mult, ALU.add)
            kb.act(st_[:, 2, :], st_[:, 1, :], AF.Sqrt)
            kb.recip(st_[:, 1, :], st_[:, 2, :])
            for h in range(4):
                kb.stt(sq[:, h, :], View(o_acc, o3.ap[:, h, :]), st_[:, 1, h:h + 1], nwb[:], ALU.mult, ALU.mult, part=True)
            kb.tt(yb[:], rr(sq[:], "p h e -> p (h e)"), zl[nl % 2][:], ALU.mult)
            transpose_into(g, mixT[:, 0:4, tsl], yb[:], 4, eng='act')


def deltanet2(g, qT, kT, k_tm, v_tm, glog, beta, SGs, MIXs):
    kb = g.kb
    I = g.I
    with kb.phase():
        cm = {nm: kb.sb("dc_" + nm, [128, 128], F32) for nm in ('indf', 'indb', 'lowS', 'upS')}
        for nm in cm:
            kb.dma(cm[nm][:], I[nm][:])
        identf = g.identf
        o_acc = [kb.sb("o_acc%d" % i, [128, 512], F32) for i in range(NTL)]
        for i in range(NTL):
            kb.memset(o_acc[i][:], 0.0, eng='pool')

        def chain(d, h):
            c = d * 4 + h
            nm = "c%d_" % c
            F = lambda s: kb.sb(nm + s, [128, 128], F32)
            B = lambda s: kb.sb(nm + s, [128, 128], BF16)
            grep, grow, t1, dec, decS, Lm, Nm, decT = F("grep"), F("grow"), F("t1"), F("dec"), F("decS"), F("Lm"), F("Nm"), F("decT")
            X = [F("X0"), F("X1")]
            Y = [F("Y0"), F("Y1")]
            P = [F("P0"), F("P1")]
            PT = [F("PT0"), F("PT1")]
            TTb, rhsV, rhsK, wTb, vnew, attnT, ktail, Sb = B("TTb"), B("rhsV"), B("rhsK"), B("wTb"), B("vnew"), B("attnT"), B("ktail"), B("Sb")
            u_sb, otmp = t1, grep
            sc = kb.sb(nm + "sc", [128, 8], F32)
            S = F("S")
            order = list(range(NT)) if d == 0 else [1, 0] + list(range(NT - 1, 1, -1))
            mI = cm['indb'] if d == 0 else cm['indf']
            mS = cm['lowS'] if d == 0 else cm['upS']
            U = cm['indf'] if d == 0 else cm['indb']
            li = 127 if d == 0 else 0
            hs = slice(h * 128, (h + 1) * 128)
            bank = g.pA[c] if c < 6 else F32Bank(g.pT[c - 6])
            kb.memset(S[:], 0.0)
            yield
            for idx, n in enumerate(order):
                tsl = slice(n * 128, (n + 1) * 128)
                is_out = n >= NTC
                last_chunk = idx == NT - 1
                bcol = beta[:, n, c:c + 1]
                pg = bank
                kb.mm(pg[:, 0:1], U[:], glog[:, n, c:c + 1], start=True, stop=True)
                kb.copy(grep[:], bc(glog[:, n, c:c + 1], [128, 128]))
                kb.mm(pg[:, 128:256], grep[:], U[:], start=True, stop=True)
                yield
                kb.copy(sc[:, 0:1], pg[:, 0:1], eng='act')
                kb.copy(grow[:], pg[:, 128:256], eng='act')
                yield
                kb.act(sc[:, 1:2], sc[:, 0:1], AF.Exp)
                kb.ts(t1[:], grow[:], sc[:, 0:1], 0.0, ALU.subtract, ALU.max)
                yield
                kb.act(t1[:], t1[:], AF.Exp, scale=-1.0)
                yield
                kb.tt(dec[:], t1[:], mI[:], ALU.mult, eng='pool')
                kb.tt(decS[:], t1[:], mS[:], ALU.mult)
                p2 = bank
                kb.mm(p2[:, 0:128], kT[:, h, tsl], kT[:, h, tsl], start=True, stop=True)
                yield
                kb.stt(Lm[:], p2[:, 0:128], bcol, decS[:], ALU.mult, ALU.mult)
                yield
                p3 = bank
                kb.transpose(p3[:, 0:128], Lm[:], identf[:])
                kb.transpose(p3[:, 128:256], dec[:], identf[:])
                yield
                kb.copy(Nm[:], p3[:, 0:128], eng='act')
                kb.copy(decT[:], p3[:, 128:256], eng='act')
                kb.tt(P[0][:], identf[:], Lm[:], ALU.subtract)
                yield
                kb.tt(PT[0][:], identf[:], Nm[:], ALU.subtract, eng='pool')
                Xc, Yc = Lm, Nm
                pi = 0
                for m in range(6):
                    lastm = m == 5
                    pa = bank
                    kb.mm(pa[:, 0:128], Xc[:], Yc[:], start=True, stop=True)
                    if not lastm:
                        kb.mm(pa[:, 128:256], Yc[:], Xc[:], start=True, stop=True)
                    yield
                    Yn, Xn = Y[m % 2], X[m % 2]
                    kb.copy(Yn[:], pa[:, 0:128], eng='act')
                    if not lastm:
                        kb.copy(Xn[:], pa[:, 128:256], eng='dve')
                    yield
                    pb_ = bank
                    kb.mm(pb_[:, 0:128], P[pi][:], Yn[:], start=True, stop=True)
                    if not lastm:
                        kb.mm(pb_[:, 128:256], PT[pi][:], Xn[:], start=True, stop=True)
                    yield
                    kb.tt(PT[1 - pi][:], PT[pi][:], pb_[:, 0:128], ALU.add)
                    if not lastm:
                        kb.tt(P[1 - pi][:], P[pi][:], pb_[:, 128:256], ALU.add)
                    yield
                    pi = 1 - pi
                    Xc, Yc = Xn, Yn
                kb.copy(TTb[:], PT[pi][:], eng='act')
                kb.ts(rhsV[:], v_tm[:, n, hs], bcol, None, ALU.mult, eng='pool')
                kb.tt(sc[:, 2:3], bcol, sc[:, 1:2], ALU.mult)
                yield
                kb.ts(rhsK[:], k_tm[:, n, hs], sc[:, 2:3], None, ALU.mult)
                kb.copy(Sb[:], S[:], eng='act')
                yield
                p4 = bank
                kb.mm(p4[:, 0:128], TTb[:], rhsV[:], start=True, stop=True)
                kb.mm(p4[:, 128:256], rhsK[:], TTb[:], start=True, stop=True)
                yield
                kb.copy(u_sb[:], p4[:, 0:128], eng='act')
                kb.copy(wTb[:], p4[:, 128:256], eng='dve')
                yield
                p5 = bank
                kb.mm(p5[:, 0:128], wTb[:], Sb[:], start=True, stop=True)
                yield
                kb.tt(vnew[:], u_sb[:], p5[:, 0:128], ALU.subtract)
                yield
                if is_out:
                    p6 = bank
                    kb.mm(p6[:, 0:128], kT[:, h, tsl], qT[:, h, tsl], start=True, stop=True)
                    yield
                    kb.tt(attnT[:], p6[:, 0:128], decT[:], ALU.mult)
                    yield
                    p7 = bank
                    kb.mm(p7[:, 0:128], attnT[:], vnew[:], start=True, stop=True)
                    kb.mm(p7[:, 128:256], qT[:, h, tsl], Sb[:], start=True, stop=True)
                    yield
                    kb.copy(otmp[:], p7[:, 0:128], eng='act')
                    yield
                    kb.stt(otmp[:], p7[:, 128:256], sc[:, 1:2], otmp[:], ALU.mult, ALU.add)
                    yield
                    oa = o_acc[n - NTC][:, hs]
                    kb.tt(oa, oa, otmp[:], ALU.add, eng='pool')
                    yield
                if not last_chunk:
                    kb.act(sc[:, 3:4], sc[:, 0:1], AF.Exp, scale=-1.0, bias=grow[:, li:li + 1])
                    kb.act(sc[:, 4:5], grow[:, li:li + 1], AF.Exp)
                    yield
                    kb.ts(ktail[:], k_tm[:, n, hs], sc[:, 3:4], None, ALU.mult)
                    yield
                    p8 = bank
                    kb.mm(p8[:, 0:128], ktail[:], vnew[:], start=True, stop=True)
                    yield
                    kb.stt(S[:], S[:], sc[:, 4:5], p8[:, 0:128], ALU.mult, ALU.add)
                    yield

        gens = [chain(d, h) for d in range(2) for h in range(4)]
        while gens:
            for gen in list(gens):
                try:
                    next(gen)
                except StopIteration:
                    gens.remove(gen)
        nwb = kb.sb("dnw", [128, 128], F32)
        bcast_row(kb, nwb[:], I['dn_norm_w'][0, :])
        sq = kb.sb("dsq", [128, 4, 128], F32)
        st_ = kb.sb("dst", [128, 3, 4], F32)
        yb = kb.sb("dyb", [128, 512], BF16)
        zl = [kb.sb("zl%d" % i, [128, 512], BF16) for i in range(2)]
        yo = [kb.sb("dyo%d" % i, [128, 4, 128], BF16) for i in range(2)]
        for nl in range(NTL):
            n = nl + NTC
            tsl = slice(n * 128, (n + 1) * 128)
            kb.dma(zl[nl % 2][:], SGs[tsl, :])
            o3 = rr(o_acc[nl][:], "p (h e) -> p h e", h=4)
            kb.tt(sq[:], o3, o3, ALU.mult)
            kb.reduce(st_[:, 0, :], sq[:], ALU.add)
            kb.ts(st_[:, 1, :], st_[:, 0, :], 1.0 / 128, EPS, ALU.mult, ALU.add)
            kb.act(st_[:, 2, :], st_[:, 1, :], AF.Sqrt)
            kb.recip(st_[:, 1, :], st_[:, 2, :])
            for h in range(4):
                kb.stt(sq[:, h, :], View(o_acc[nl], o3.ap[:, h, :]), st_[:, 1, h:h + 1], nwb[:], ALU.mult, ALU.mult, part=True)
            kb.tt(yb[:], rr(sq[:], "p h e -> p (h e)"), zl[nl % 2][:], ALU.mult)
            transpose_into(g, yo[nl % 2][:], yb[:], 4, eng='act')
            kb.dma(rr(MIXs[0:4, :, tsl], "k c t -> c k t"), yo[nl % 2][:], part=True)


class F32Bank:
    def __init__(self, tile):
        self.tile = tile

    def __getitem__(self, idx):
        return View(self.tile, self.tile.h[:, :].bitcast(F32)[idx])


CONST_SPECS.update({'thr': ([128, 18], F32), 'bvals': ([128, 50], F32), 'pcol': ([128, 1], F32)})


def moe_sparse(g, L, last):
    kb = g.kb
    I = g.I
    tiles = list(range(NTC, NT)) if last else list(range(NT))
    BR = 256
    NB_OVF = len(tiles) - 1
    NBLK = 32 + NB_OVF
    OVF_BASE = 32 * BR
    with kb.phase():
        modt = [kb.sb("modm%d" % r, [128, 3072], F32) for r in range(2)]
        compute_mod(g, L, 1, modt)
        h2all = kb.sb("h2all", [128, NT, D], BF16)
        Gall = kb.sb("Gall", [128, NT, 32], F32)
        kb.memset(Gall[:], 0.0, eng='pool')
        idx_i = kb.sb("idx_i", [128, 2, NT], I32)
        gts = kb.sb("gts", [128, 2, NT], F32)
        widx_i = kb.sb("widx_i", [128, NB_OVF], I32)
        ROWS = kb.dram("ROWS%d" % L, [NBLK * BR, D], BF16)
        YROWS = kb.dram("YROWS%d" % L, [NBLK * BR, D], F32)
        with kb.phase():
            rw = kb.sb("rw", [128, 8, 36], F32)
            kb.dma(rw[:], rr(I['router_w'][L, :, :], "(k p) c -> p k c", p=128))
            rb = kb.sb("rbias", [128, 36], F32)
            bcast_row(kb, rb[:], I['router_b'][L, :])
            xt = [kb.sb("xm%d" % i, [128, D], F32) for i in range(2)]
            tmp = [kb.sb("tmpm%d" % i, [128, D], F32) for i in range(2)]
            hf_l = [kb.sb("hf%d" % i, [128, D], F32) for i in range(2)]
            hTf = kb.sb("hTf", [128, 8, 128], F32)
            ss = [kb.sb("ssm%d" % i, [128, 4], F32) for i in range(2)]
            hTf_l = [kb.sb("hTf%d" % i, [128, 8, 128], F32) for i in range(2)]
            lgall = kb.sb("lgall", [128, NT, 36], F32)
            kb.memset(lgall[:], 0.0, eng='pool')
            s8 = kb.sb("s8", [128, 16], F32)
            oh4 = kb.sb("oh4", [128, 4], F32)
            t48 = kb.sb("t48", [128, 4, 8], F32)
            sel8 = kb.sb("sel8", [128, 8], F32)
            msk = kb.sb("msk8", [128, 8], F32)
            oh1 = kb.sb("oh1", [128, 8], F32)
            oh2 = kb.sb("oh2", [128, 8], F32)
            G8 = kb.sb("G8", [128, 8], F32)
            e4 = kb.sb("e4", [128, 4], F32)
            def _mk(n):
                def gen(s):
                    x_ = xt[s]
                    kb.dma(x_[:], g.X[n * 128:(n + 1) * 128, :])
                    yield
                    hf, hTf = hf_l[s], hTf_l[s]
                    yield from norm_mod_gen(g, x_[:], modt[0 if n >= NTC else 1], hf[:], ss[s], tmp[s])
                    kb.copy(h2all[:, n, :], hf[:], eng='act', part=True)
                    yield
                    for half in range(2):
                        pt = nextA_s(g, s)
                        for k in range(4):
                            kk = half * 4 + k
                            kb.transpose(pt[:, k * 128:(k + 1) * 128], hf[:, kk * 128:(kk + 1) * 128], g.identf[:])
                        kb.copy(hTf[:, half * 4:(half + 1) * 4, :], rr(pt[:, :], "p (k t) -> p k t", k=4), eng='act', part=True)
                        yield
                    ps = nextA_s(g, s)
                    for k in range(8):
                        kb.mm(ps[:, 0:36], hTf[:, k, :], rw[:, k, :], start=(k == 0), stop=(k == 7))
                    kb.tt(lgall[:, n, :], ps[:, 0:36], rb[:], ALU.add, part=True)
                    yield
                    yield
                return gen
            run_chains([_mk(n) for n in tiles], 2)
            N3 = lambda nm, k: kb.sb(nm, [128, NT, k], F32)
            N2 = lambda nm: kb.sb(nm, [128, NT], F32)
            lg4 = lgall[:, :, 0:4]
            m4, s4, pgr, m1, m2, dm, ee, g1_, g2_ = N2("m4"), N2("s4"), N2("pgr"), N2("m1"), N2("m2"), N2("dm"), N2("ee"), N2("g1_"), N2("g2_")
            oh4, d4, sel8, oh1, oh2, msk, G8 = N3("oh4", 4), N3("d4", 4), N3("sel8", 8), N3("oh1", 8), N3("oh2", 8), N3("msk", 8), N3("G8", 8)
            t48 = kb.sb("t48", [128, NT, 4, 8], F32)
            kb.reduce(m4[:], lg4, ALU.max)
            kb.tt(oh4[:], lg4, ubc(m4[:], 2, [128, NT, 4]), ALU.is_ge)
            kb.tt(d4[:], lg4, ubc(m4[:], 2, [128, NT, 4]), ALU.subtract)
            kb.act(d4[:], d4[:], AF.Exp)
            kb.reduce(s4[:], d4[:], ALU.add)
            kb.recip(pgr[:], s4[:])
            kb.tt(t48[:], rr(lgall[:, :, 4:36], "p n (g e) -> p n g e", g=4), ubc(oh4[:], 3, [128, NT, 4, 8]), ALU.mult)
            kb.reduce(sel8[:], rr(t48[:], "p n g e -> p n e g"), ALU.add)
            kb.reduce(m1[:], sel8[:], ALU.max)
            kb.tt(oh1[:], sel8[:], ubc(m1[:], 2, [128, NT, 8]), ALU.is_ge)
            kb.stt(msk[:], oh1[:], -1e30, sel8[:], ALU.mult, ALU.add)
            kb.reduce(m2[:], msk[:], ALU.max)
            kb.tt(oh2[:], msk[:], ubc(m2[:], 2, [128, NT, 8]), ALU.is_ge)
            kb.tt(dm[:], m2[:], m1[:], ALU.subtract)
            kb.act(ee[:], dm[:], AF.Exp)
            kb.ts(dm[:], ee[:], 1.0, None, ALU.add)
            kb.recip(g1_[:], dm[:])
            kb.tt(g1_[:], g1_[:], pgr[:], ALU.mult)
            kb.tt(g2_[:], g1_[:], ee[:], ALU.mult)
            kb.tt(G8[:], oh1[:], ubc(g1_[:], 2, [128, NT, 8]), ALU.mult)
            kb.tt(msk[:], oh2[:], ubc(g2_[:], 2, [128, NT, 8]), ALU.mult)
            kb.tt(G8[:], G8[:], msk[:], ALU.add)
            kb.tt(rr(Gall[:], "p n (g e) -> p n g e", g=4), ubc(G8[:], 2, [128, NT, 4, 8]), ubc(oh4[:], 3, [128, NT, 4, 8]), ALU.mult)
            if last:
                kb.memset(Gall[:, 0:NTC, :], 0.0)
        with kb.phase():
            cst = {nm: kb.sb("ms_" + nm, shp, F32) for nm, shp in (('upS', [128, 128]), ('onesf', [128, 128]), ('thr', [128, 18]), ('bvals', [128, 50]), ('pcol', [128, 1]))}
            for nm in cst:
                kb.dma(cst[nm][:], I[nm][:])
            A3 = lambda nm: kb.sb(nm, [128, NT, 32], F32)
            sel, rank, dest, mlo, mhi, eq = A3("sel"), A3("rank"), A3("dest"), A3("mlo"), A3("mhi"), A3("eq")
            base = kb.sb("base", [128, 32], F32)
            kb.ts(sel[:], Gall[:], 0.0, None, ALU.is_gt)
            kb.memset(rank[:], 0.0, eng='pool')
            kb.memset(base[:], 0.0)
            for n in tiles:
                ps = nextA(g)
                kb.mm(ps[:, 0:32], cst['upS'][:], sel[:, n, :], start=True, stop=True)
                kb.mm(ps[:, 32:64], cst['onesf'][:], sel[:, n, :], start=True, stop=True)
                kb.tt(rank[:, n, :], ps[:, 0:32], base[:], ALU.add, part=True)
                kb.tt(base[:], base[:], ps[:, 32:64], ALU.add)
            cmp = kb.sb("cmpk", [128, 32, 17], F32)
            nb = kb.sb("nbk", [128, 32], F32)
            kb.tt(cmp[:], ubc(base[:], 2, [128, 32, 17]), ubc(cst['thr'][:, 1:18], 1, [128, 32, 17]), ALU.is_gt)
            kb.reduce(nb[:], cmp[:], ALU.add)
            cum = [kb.sb("cum%d" % i, [128, 32], F32) for i in range(2)]
            kb.copy(cum[0][:], nb[:])
            ci = 0
            for s in (1, 2, 4, 8, 16):
                kb.copy(cum[1 - ci][:], cum[ci][:])
                kb.tt(cum[1 - ci][:, s:32], cum[ci][:, s:32], cum[ci][:, 0:32 - s], ALU.add)
                ci = 1 - ci
            start = kb.sb("startb", [128, 32], F32)
            st256 = kb.sb("st256", [128, 32], F32)
            kb.tt(start[:], cum[ci][:], nb[:], ALU.subtract)
            e256 = kb.sb("e256", [128, 32], F32)
            kb.ts(e256[:], cst['bvals'][:, 0:32], float(BR), None, ALU.mult)
            kb.ts(st256[:], start[:], float(BR), float(OVF_BASE - BR), ALU.mult, ALU.add)
            kb.tt(st256[:], st256[:], e256[:], ALU.subtract)
            kb.ts(dest[:], rank[:], float(BR), None, ALU.is_ge)
            kb.tt(dest[:], dest[:], ubc(st256[:], 1, [128, NT, 32]), ALU.mult)
            kb.tt(dest[:], dest[:], rank[:], ALU.add)
            kb.tt(dest[:], dest[:], ubc(e256[:], 1, [128, NT, 32]), ALU.add)
            kb.ts(eq[:], sel[:], -1e9, 1e9, ALU.mult, ALU.add)
            kb.tt(mlo[:], dest[:], eq[:], ALU.add)
            kb.tt(mhi[:], dest[:], sel[:], ALU.mult)
            ixf = kb.sb("ixf", [128, 2, NT], F32)
            kb.reduce(ixf[:, 0, :], mlo[:], ALU.min)
            kb.reduce(ixf[:, 1, :], mhi[:], ALU.max)
            kb.copy(idx_i[:], ixf[:])
            kb.tt(eq[:], mlo[:], ubc(ixf[:, 0, :], 2, [128, NT, 32]), ALU.is_equal)
            kb.tt(eq[:], eq[:], Gall[:], ALU.mult)
            kb.reduce(gts[:, 0, :], eq[:], ALU.add)
            kb.tt(eq[:], mhi[:], ubc(ixf[:, 1, :], 2, [128, NT, 32]), ALU.is_equal)
            kb.tt(eq[:], eq[:], Gall[:], ALU.mult)
            kb.reduce(gts[:, 1, :], eq[:], ALU.add)
            cmpb = kb.sb("cmpb", [128, NB_OVF, 32], F32)
            ebf = kb.sb("ebf", [128, NB_OVF], F32)
            kb.tt(cmpb[:], ubc(start[:], 1, [128, NB_OVF, 32]), ubc(cst['bvals'][:, 0:NB_OVF], 2, [128, NB_OVF, 32]), ALU.is_le)
            kb.reduce(ebf[:], cmpb[:], ALU.add)
            kb.ts(ebf[:], ebf[:], -1.0 + 32.0 * L, 128.0, ALU.add, ALU.mult)
            kb.ts(ebf[:], ebf[:], cst['pcol'][:, 0:1], None, ALU.add)
            oobf = kb.sb("oobf", [128, NB_OVF], F32)
            kb.ts(oobf[:], cst['bvals'][:, 0:NB_OVF], cum[ci][:, 31:32], 1.0e7, ALU.is_ge, ALU.mult)
            kb.tt(ebf[:], ebf[:], oobf[:], ALU.add)
            kb.copy(widx_i[:], ebf[:])
            dump(g, 'idx_i', idx_i[:], [128, 2, NT], I32)
            dump(g, 'widx_i', widx_i[:], [128, NB_OVF], I32)
            dump(g, 'gts', gts[:], [128, 2, NT], F32)
        rows_ap = ROWS[:, :].ap
        for n in tiles:
            for j in range(2):
                iap = h2all[:, n, :].ap
                xap = idx_i[:, j, n:n + 1].ap

                def scat(e, iap=iap, xap=xap):
                    return e.indirect_dma_start(out=rows_ap, out_offset=bass.IndirectOffsetOnAxis(ap=xap, axis=0), in_=iap, in_offset=None)
                kb.dma_custom('pool', scat, sbt=h2all, reads=[h2all[:, n, :], idx_i[:]], writes=[], pwrites=[ROWS[:, :]])
        with kb.phase():
            Wg2 = I['moe_w_gate'][:, :, :, :].ap.rearrange("l e (p k) c -> (l e p) (k c)", k=8)
            Wu2 = I['moe_w_up'][:, :, :, :].ap.rearrange("l e (p k) c -> (l e p) (k c)", k=8)
            Wd2 = I['moe_w_down'][:, :, :, :].ap.rearrange("l e (p k) c -> (l e p) (k c)", k=4)
            wg = [kb.sb("wg%d" % i, [128, 8, 512], BF16) for i in range(2)]
            wu = [kb.sb("wu%d" % i, [128, 8, 512], BF16) for i in range(2)]
            wd = [kb.sb("wd%d" % i, [128, 4, D], BF16) for i in range(2)]
            xr = [kb.sb("xr%d" % i, [128, D], BF16) for i in range(4)]
            xT = [kb.sb("xT%d" % i, [128, 8, BR], BF16) for i in range(2)]
            aT = [kb.sb("aT%d" % i, [128, 4, BR], BF16) for i in range(2)]
            sgm = [kb.sb("sgm%d" % i, [128, BR], BF16) for i in range(2)]
            yo = [kb.sb("yo%d" % i, [128, D], F32) for i in range(2)]
            sgm4 = [kb.sb("sgmx%d" % i, [128, BR], BF16) for i in range(4)]
            yo4 = [kb.sb("yox%d" % i, [128, D], F32) for i in range(4)]

            def mk_block(b):
                def gen(s):
                    wg_, wu_, wd_ = wg[s], wu[s], wd[s]
                    if b < 32:
                        kb.dma(wg_[:], rr(I['moe_w_gate'][L, b, :, :], "(p k) c -> p k c", k=8), eng='pool')
                        kb.dma(wu_[:], rr(I['moe_w_up'][L, b, :, :], "(p k) c -> p k c", k=8), eng='pool')
                        kb.dma(wd_[:], rr(I['moe_w_down'][L, b, :, :], "(p k) c -> p k c", k=4), eng='pool')
                    xap = widx_i[:, max(b - 32, 0):max(b - 32, 0) + 1].ap
                    for (dst, src) in (((wg_, Wg2), (wu_, Wu2), (wd_, Wd2)) if b >= 32 else ()):
                        oap = dst[:].ap.rearrange("p k c -> p (k c)")

                        def gat(e, oap=oap, src=src, xap=xap):
                            if 'bc' not in g.regcache:
                                g.regcache['bc'] = e.to_reg(2 * 32 * 128 - 1)
                            return e.indirect_dma_start(out=oap, out_offset=None, in_=src, in_offset=bass.IndirectOffsetOnAxis(ap=xap, axis=0),
                                                        bounds_check=g.regcache['bc'], oob_is_err=False)
                        kb.dma_custom('pool', gat, sbt=dst, reads=[widx_i[:]], writes=[dst[:]])
                    xT_ = xT[s]
                    for i in range(2):
                        xr_ = xr[s * 2 + i]
                        kb.dma(xr_[:], ROWS[b * BR + i * 128:b * BR + (i + 1) * 128, :])
                    yield
                    for i in range(2):
                        xr_ = xr[s * 2 + i]
                        pt = g.pT[s]
                        x3 = xr_[:].ap.rearrange("t (p k) -> t k p", k=8)
                        for k in range(8):
                            kb.transpose(pt[:, k * 128:(k + 1) * 128], View(xr_, x3[:, k, :]), g.ident[:])
                        kb.copy(xT_[:, :, i * 128:(i + 1) * 128], rr(pt[:, :], "p (k t) -> p k t", k=8), eng=('act' if i == 0 else 'dve'), part=True)
                        yield
                    aT_ = aT[s]
                    for hc in range(4):
                        pg_ = nextA_s(g, s)
                        for k in range(8):
                            lw = View(wg_, wg_[:, k, :].ap.rearrange("p (m f) -> p f m", f=4)[:, hc, :])
                            kb.mm(pg_[:, 0:BR], lw, xT_[:, k, :], start=(k == 0), stop=(k == 7))
                        pu_ = nextA_s(g, s)
                        for k in range(8):
                            lw = View(wu_, wu_[:, k, :].ap.rearrange("p (m f) -> p f m", f=4)[:, hc, :])
                            kb.mm(pu_[:, 0:BR], lw, xT_[:, k, :], start=(k == 0), stop=(k == 7))
                        sg_ = sgm4[s * 2 + hc % 2]
                        kb.act(sg_[:], pg_[:, 0:BR], AF.Silu)
                        kb.tt(aT_[:, hc, :], sg_[:], pu_[:, 0:BR], ALU.mult, part=True)
                        yield
                    for i in range(2):
                        yo_ = yo4[s * 2 + i]
                        for dh in range(2):
                            py = nextA_s(g, s)
                            for hc in range(4):
                                kb.mm(py[:, :], aT_[:, hc, i * 128:(i + 1) * 128], wd_[:, hc, dh * 512:(dh + 1) * 512], start=(hc == 0), stop=(hc == 3))
                            kb.copy(yo_[:, dh * 512:(dh + 1) * 512], py[:, :], eng=('act' if dh == 0 else 'dve'), part=True)
                        kb.dma(YROWS[b * BR + i * 128:b * BR + (i + 1) * 128, :], yo_[:], part=True)
                        yield
                return gen
            run_chains([mk_block(b) for b in range(NBLK)], 2)
        with kb.phase():
            xt = [kb.sb("xf%d" % i, [128, D], F32) for i in range(2)]
            yl = [kb.sb("yl%d" % i, [128, D], F32) for i in range(2)]
            yh = [kb.sb("yh%d" % i, [128, D], F32) for i in range(2)]
            tmp_l = [kb.sb("tmpf%d" % i, [128, D], F32) for i in range(2)]
            ss_l = [kb.sb("ssf%d" % i, [128, 4], F32) for i in range(2)]
            yrows_ap = YROWS[:, :].ap
            if last:
                fnw = kb.sb("fnw", [128, D], F32)
                bcast_row(kb, fnw[:], I['final_norm_w'][:])
            def _mk(n):
                def gen(s):
                    x_ = xt[s]
                    kb.dma(x_[:], g.X[n * 128:(n + 1) * 128, :])
                    yield
                    bufs = (yl[s], yh[s])
                    for j in range(2):
                        oap = bufs[j][:].ap
                        xap = idx_i[:, j, n:n + 1].ap

                        def gat2(e, oap=oap, xap=xap):
                            return e.indirect_dma_start(out=oap, out_offset=None, in_=yrows_ap, in_offset=bass.IndirectOffsetOnAxis(ap=xap, axis=0))
                        kb.dma_custom('pool', gat2, sbt=bufs[j], reads=[idx_i[:], YROWS[:, :]], writes=[bufs[j][:]])
                        yield
                    mr = modt[0 if n >= NTC else 1]
                    kb.ts(tmp_l[s][:], bufs[0][:], gts[:, 0, n:n + 1], None, ALU.mult)
                    yield
                    kb.stt(tmp_l[s][:], bufs[1][:], gts[:, 1, n:n + 1], tmp_l[s][:], ALU.mult, ALU.add)
                    yield
                    kb.tt(tmp_l[s][:], tmp_l[s][:], mr[:, 2048:3072], ALU.mult)
                    yield
                    kb.tt(x_[:], x_[:], tmp_l[s][:], ALU.add)
                    yield
                    if not last:
                        kb.dma(g.X[n * 128:(n + 1) * 128, :], x_[:])
                        yield
                    else:
                        kb.memset(ss_l[s][:, 0:1], 0.0)
                        yield
                        kb.act(tmp_l[s][:], x_[:], AF.Square, accum=ss_l[s][:, 0:1])
                        yield
                        kb.ts(ss_l[s][:, 1:2], ss_l[s][:, 0:1], 1.0 / D, EPS, ALU.mult, ALU.add)
                        yield
                        kb.act(ss_l[s][:, 2:3], ss_l[s][:, 1:2], AF.Sqrt)
                        yield
                        kb.recip(ss_l[s][:, 3:4], ss_l[s][:, 2:3])
                        yield
                        kb.stt(x_[:], x_[:], ss_l[s][:, 3:4], fnw[:], ALU.mult, ALU.mult)
                        yield
                        kb.dma(g.out[(n - NTC) * 128:(n - NTC + 1) * 128, :], x_[:], is_output=True)
                        yield
                    yield
                return gen
            run_chains([_mk(n) for n in tiles], 2)


def run_chains(makers, width):
    makers = list(makers)
    nxt = 0
    active = {}
    free = list(range(width))
    while nxt < len(makers) or active:
        while free and nxt < len(makers):
            s = free.pop(0)
            active[s] = makers[nxt](s)
            nxt += 1
        for s in sorted(active):
            try:
                next(active[s])
            except StopIteration:
                del active[s]
                free.append(s)


def nextA_s(g, s, nslots=2):
    per = len(g.pA) // nslots
    d = g.__dict__.setdefault('pAs', {})
    d[s] = (d.get(s, -1) + 1) % per
    return g.pA[s * per + d[s]]


def norm_mod_gen(g, xt, modr, hb, ss, tmp):
    kb = g.kb
    kb.memset(ss[:, 0:1], 0.0)
    kb.act(tmp[:], xt, AF.Square, accum=ss[:, 0:1])
    yield
    kb.ts(ss[:, 1:2], ss[:, 0:1], 1.0 / D, EPS, ALU.mult, ALU.add)
    yield
    kb.act(ss[:, 2:3], ss[:, 1:2], AF.Sqrt)
    yield
    kb.recip(ss[:, 3:4], ss[:, 2:3])
    yield
    kb.stt(tmp[:], xt, ss[:, 3:4], modr[:, 1024:2048], ALU.mult, ALU.mult)
    yield
    kb.tt(hb, tmp[:], modr[:, 0:1024], ALU.add)
    yield


def transpose_gen(g, s, dst3, src_bf, nchunk, eng='act'):
    kb = g.kb
    pt = g.pT[s]
    for k in range(nchunk):
        kb.transpose(pt[:, k * 128:(k + 1) * 128], View(src_bf.tile, src_bf.ap[:, k * 128:(k + 1) * 128]), g.ident[:])
    yield
    kb.copy(dst3, rr(pt[:, 0:nchunk * 128], "p (k t) -> p k t", k=nchunk), eng=eng, part=True)
    yield


def kernel(**inputs):
    inputs = {k: np.asarray(v) for k, v in inputs.items()}
    nc = build()
    sh = prep_shared(inputs)
    maps = [prep_core(inputs, sh, b, nc._used_inputs) for b in range(8)]
    res = run_bass_kernel_spmd(nc, maps, core_ids=list(range(8)))
    out = np.stack([np.asarray(r["out"], dtype=np.float32) for r in res.results], 0)
    return out
```
